# Optimizing a Trainium2 kernel written in Bass

```python
import jax, jax.numpy as jnp
from jax import lax
import numpy as np

D_MODEL = 1024
BATCH = 2
SEQ = 16384
DEPTH = 2

GRID_W = 64
CTX_LEN = 256
EPS = 1e-6
MASK_VALUE = -1e30
TINY = 1e-30

HG_HEADS = 4
HG_KDIM = 128
HG_VDIM = 128
HG_KW = HG_HEADS * HG_KDIM
HG_VW = HG_HEADS * HG_VDIM
HG_CHUNK = 64

HEAD_DIM = 64
ATT_HEADS = 8
ATT_KV = 2
SWA_HEADS = 8
SWA_KV = 2
WINDOW = 128
Q_BLOCK = 128
ROPE_THETA = 10000.0
ATTN_SCALE = HEAD_DIM ** -0.5
N_BRANCH = 3

N_GROUPS = 4
EXP_PER_GROUP = 8
N_EXPERTS = N_GROUPS * EXP_PER_GROUP
TOP_K = 2
D_EXPERT = 512
MOE_BLOCK = 128

IN_SIZES = (HG_KW, HG_KW, HG_KW, HG_VW, HG_VW,
            ATT_HEADS * HEAD_DIM, ATT_KV * HEAD_DIM, ATT_KV * HEAD_DIM,
            SWA_HEADS * HEAD_DIM, SWA_KV * HEAD_DIM, SWA_KV * HEAD_DIM,
            N_BRANCH * D_MODEL)
D_IN = sum(IN_SIZES)

kernel_name = "hybrid_flow_backbone_block"


def rms_norm(x, g):
    xf = x.astype(jnp.float32)
    y = xf * lax.rsqrt(jnp.mean(xf * xf, axis=-1, keepdims=True) + EPS)
    return y.astype(x.dtype) * g


def split_projection(p):
    idx, acc = [], 0
    for s in IN_SIZES[:-1]:
        acc += s
        idx.append(acc)
    return jnp.split(p, idx, axis=-1)


def split_heads(a, n_heads):
    return a.reshape(a.shape[0], a.shape[1], n_heads, HEAD_DIM)


def axial_rope(n_tokens, dtype):
    n_rows = n_tokens // GRID_W
    row = jnp.repeat(jnp.arange(n_rows), GRID_W).astype(jnp.float32)
    col = jnp.tile(jnp.arange(GRID_W), n_rows).astype(jnp.float32)
    axis_pairs = HEAD_DIM // 4
    inv = ROPE_THETA ** (-jnp.arange(axis_pairs, dtype=jnp.float32) / axis_pairs)
    ang = jnp.concatenate([row[:, None] * inv, col[:, None] * inv], axis=-1)
    return jnp.cos(ang).astype(dtype)[:, None, :], jnp.sin(ang).astype(dtype)[:, None, :]


def apply_rope(x, cos, sin):
    x1, x2 = jnp.split(x, 2, axis=-1)
    return jnp.concatenate([x1 * cos - x2 * sin, x2 * cos + x1 * sin], axis=-1)


def gla_chunk_scan(q, k, v, log_f, s0):
    b, l, h, _ = q.shape
    n = l // HG_CHUNK

    def chunks(a):
        return a.astype(jnp.float32).reshape(b, n, HG_CHUNK, h, a.shape[-1]).transpose(1, 0, 3, 2, 4)

    lower = jnp.tril(jnp.ones((HG_CHUNK, HG_CHUNK), bool))[None, None, :, :, None]

    def step(state, inp):
        qc, kc, vc, gc = inp
        g_cum = jnp.cumsum(gc, axis=2)
        diff = jnp.where(lower, g_cum[:, :, :, None, :] - g_cum[:, :, None, :, :], 0.0)
        rel = jnp.where(lower, jnp.exp(diff), 0.0)
        scores = jnp.einsum('bhtk,bhsk,bhtsk->bhts', qc, kc, rel)
        out = (jnp.einsum('bhts,bhsv->bhtv', scores, vc)
               + jnp.einsum('bhtk,bhkv->bhtv', qc * jnp.exp(g_cum), state))
        g_end = g_cum[:, :, -1:, :]
        state = (jnp.exp(g_end[:, :, 0, :, None]) * state
                 + jnp.einsum('bhsk,bhsv->bhkv', kc * jnp.exp(g_end - g_cum), vc))
        return state, out

    state, out = lax.scan(step, s0, (chunks(q), chunks(k), chunks(v), chunks(log_f)))
    out = out.transpose(1, 0, 3, 2, 4).reshape(b, l, h, v.shape[-1])
    return out.astype(v.dtype), state


def hgrn2_branch(parts_l, parts_c, lower_bound, out_norm_g, ctx_out):
    lb = lower_bound.reshape(HG_HEADS, HG_KDIM)

    def prep(q, f_fwd, f_bwd, i):
        bsz, n = q.shape[:2]
        heads = lambda a: a.reshape(bsz, n, HG_HEADS, -1)
        qh = heads(jax.nn.silu(q)) * HG_KDIM ** -0.5

        def decay(f):
            sig = jax.nn.sigmoid(heads(f).astype(jnp.float32))
            forget = lb + (1.0 - lb) * sig
            log_forget = jnp.log(jnp.maximum(forget, TINY))
            key = (1.0 - lb) * (1.0 - sig)
            return key, log_forget
        return qh, heads(i), decay(f_fwd), decay(f_bwd)

    flip = lambda a: jnp.flip(a, axis=1)
    s0 = jnp.zeros((parts_l[0].shape[0], HG_HEADS, HG_KDIM, HG_VDIM), jnp.float32)
    q_c, v_c, (kf_c, gf_c), (kb_c, gb_c) = prep(*parts_c[:4])
    q_l, v_l, (kf_l, gf_l), (kb_l, gb_l) = prep(*parts_l[:4])
    of_c, state_f = gla_chunk_scan(q_c, kf_c, v_c, gf_c, s0)
    ob_c, state_b = gla_chunk_scan(flip(q_c), flip(kb_c), flip(v_c), flip(gb_c), s0)
    of_l, _ = gla_chunk_scan(q_l, kf_l, v_l, gf_l, state_f)
    ob_l, _ = gla_chunk_scan(flip(q_l), flip(kb_l), flip(v_l), flip(gb_l), state_b)

    def readout(o, gate):
        return rms_norm(o, out_norm_g).reshape(gate.shape) * jax.nn.silu(gate)

    out_l = readout(of_l + flip(ob_l), parts_l[4])
    out_c = readout(of_c + flip(ob_c), parts_c[4]) if ctx_out else None
    return out_l, out_c


def dense_attention_branch(q_l, k_l, v_l, q_c, k_c, v_c, q_norm_g, k_norm_g, cos, sin, ctx_out):
    bsz, s = q_l.shape[:2]
    group = ATT_HEADS // ATT_KV
    ql = apply_rope(rms_norm(split_heads(q_l, ATT_HEADS), q_norm_g), cos, sin)
    kl = apply_rope(rms_norm(split_heads(k_l, ATT_KV), k_norm_g), cos, sin)
    kc = rms_norm(split_heads(k_c, ATT_KV), k_norm_g)
    vl, vc = split_heads(v_l, ATT_KV), split_heads(v_c, ATT_KV)
    k_all = jnp.concatenate([kc, kl], axis=1)
    v_all = jnp.concatenate([vc, vl], axis=1)

    def attend(q, keys, vals):
        qg = q.reshape(q.shape[0], q.shape[1], ATT_KV, group, HEAD_DIM)
        logits = jnp.einsum('bqhgd,bkhd->bhgqk', qg, keys).astype(jnp.float32) * ATTN_SCALE
        p = jax.nn.softmax(logits, axis=-1).astype(vals.dtype)
        o = jnp.einsum('bhgqk,bkhd->bqhgd', p, vals)
        return o.reshape(q.shape[0], q.shape[1], ATT_HEADS * HEAD_DIM)

    def block(n):
        return attend(lax.dynamic_slice_in_dim(ql, n * Q_BLOCK, Q_BLOCK, axis=1), k_all, v_all)

    out_l = lax.map(block, jnp.arange(s // Q_BLOCK)).transpose(1, 0, 2, 3).reshape(bsz, s, -1)
    out_c = attend(rms_norm(split_heads(q_c, ATT_HEADS), q_norm_g), kc, vc) if ctx_out else None
    return out_l, out_c


def window_attention_branch(q_l, k_l, v_l, q_c, k_c, v_c, sink, cos, sin, ctx_out):
    bsz, s = q_l.shape[:2]
    group = SWA_HEADS // SWA_KV
    band = Q_BLOCK + 2 * WINDOW
    ql = apply_rope(split_heads(q_l, SWA_HEADS), cos, sin)
    kl = apply_rope(split_heads(k_l, SWA_KV), cos, sin)
    vl = split_heads(v_l, SWA_KV)
    kc, vc = split_heads(k_c, SWA_KV), split_heads(v_c, SWA_KV)
    pad = ((0, 0), (WINDOW, WINDOW), (0, 0), (0, 0))
    kp, vp = jnp.pad(kl, pad), jnp.pad(vl, pad)
    sink_logit = sink.astype(jnp.float32).reshape(1, SWA_KV, group, 1, 1)
    offs_q = jnp.arange(Q_BLOCK)
    offs_k = jnp.arange(band) - WINDOW

    def attend(q, keys, vals, mask):
        qg = q.reshape(q.shape[0], q.shape[1], SWA_KV, group, HEAD_DIM)
        logits = [jnp.einsum('bqhgd,bkhd->bhgqk', qg, kk).astype(jnp.float32) * ATTN_SCALE for kk in keys]
        if mask is not None:
            logits[-1] = jnp.where(mask, logits[-1], MASK_VALUE)
        sink_col = jnp.broadcast_to(sink_logit, logits[0].shape[:-1] + (1,))
        p = jax.nn.softmax(jnp.concatenate(logits + [sink_col], axis=-1), axis=-1)
        outs, lo = [], 0
        for vv in vals:
            hi = lo + vv.shape[1]
            outs.append(jnp.einsum('bhgqk,bkhd->bqhgd', p[..., lo:hi].astype(vv.dtype), vv))
            lo = hi
        return sum(outs).reshape(q.shape[0], q.shape[1], SWA_HEADS * HEAD_DIM)

    def block(n):
        start = n * Q_BLOCK
        qb = lax.dynamic_slice_in_dim(ql, start, Q_BLOCK, axis=1)
        kb = lax.dynamic_slice_in_dim(kp, start, band, axis=1)
        vb = lax.dynamic_slice_in_dim(vp, start, band, axis=1)
        key_pos = start + offs_k
        mask = ((jnp.abs(offs_k[None, :] - offs_q[:, None]) <= WINDOW)
                & (key_pos >= 0)[None, :] & (key_pos < s)[None, :])
        return attend(qb, [kc, kb], [vc, vb], mask)

    out_l = lax.map(block, jnp.arange(s // Q_BLOCK)).transpose(1, 0, 2, 3).reshape(bsz, s, -1)
    out_c = attend(split_heads(q_c, SWA_HEADS), [kc], [vc], None) if ctx_out else None
    return out_l, out_c


def hier_moe(h, w_group, w_router, w_gate, w_up, w_down):
    n_tok, d = h.shape
    n_assign = n_tok * TOP_K
    grp_prob = jax.nn.softmax((h @ w_group).astype(jnp.float32), axis=-1)
    grp_w, grp_idx = lax.top_k(grp_prob, 1)
    exp_logits = (h @ w_router).astype(jnp.float32).reshape(n_tok, N_GROUPS, EXP_PER_GROUP)
    in_grp = jnp.take_along_axis(exp_logits, grp_idx[:, :, None], axis=1)[:, 0]
    top_logit, top_j = lax.top_k(in_grp, TOP_K)
    weight = (jax.nn.softmax(top_logit, axis=-1) * grp_w).reshape(-1)
    expert = (grp_idx * EXP_PER_GROUP + top_j).reshape(-1)
    token = jnp.repeat(jnp.arange(n_tok), TOP_K)
    order = jnp.argsort(expert, stable=True)
    e_s, t_s, w_s = expert[order], token[order], weight[order]
    counts = jnp.bincount(expert, length=N_EXPERTS)
    padded = (counts + MOE_BLOCK - 1) // MOE_BLOCK * MOE_BLOCK
    pad_end = jnp.cumsum(padded)
    pad_start = pad_end - padded
    start = jnp.cumsum(counts) - counts
    dest = pad_start[e_s] + jnp.arange(n_assign) - start[e_s]
    n_blocks = -(-(n_assign + N_EXPERTS * (MOE_BLOCK - 1)) // MOE_BLOCK)
    rows = jnp.zeros((n_blocks * MOE_BLOCK, d), h.dtype).at[dest].set(h[t_s])
    block_expert = jnp.minimum(jnp.searchsorted(pad_end, jnp.arange(n_blocks) * MOE_BLOCK, side='right'),
                               N_EXPERTS - 1)

    def expert_block(args):
        xb, e = args
        return (jax.nn.silu(xb @ w_gate[e]) * (xb @ w_up[e])) @ w_down[e]

    out_rows = lax.map(expert_block, (rows.reshape(n_blocks, MOE_BLOCK, d), block_expert)).reshape(-1, d)
    return jnp.zeros_like(h).at[t_s].add(out_rows[dest] * w_s[:, None].astype(h.dtype))


def trunk_layer(xl, xc, mod_l, mod_c, cos, sin, norm1_g, norm2_g, w_in, lower_bound, hgrn_g,
                q_norm_g, k_norm_g, sink, w_br_a, w_br_b, w_br_c, w_out,
                w_group, w_router, w_gate, w_up, w_down, ctx_out):
    d = xl.shape[-1]
    sh1, sc1, ga1, sh2, sc2, ga2 = jnp.split(mod_l[:, None, :], 6, axis=-1)
    csh1, csc1, cga1, csh2, csc2, cga2 = jnp.split(mod_c, 6, axis=-1)

    pl = split_projection((rms_norm(xl, norm1_g) * (1 + sc1) + sh1) @ w_in)
    pc = split_projection((rms_norm(xc, norm1_g) * (1 + csc1) + csh1) @ w_in)
    a_l, a_c = hgrn2_branch(pl[0:5], pc[0:5], lower_bound, hgrn_g, ctx_out)
    b_l, b_c = dense_attention_branch(*pl[5:8], *pc[5:8], q_norm_g, k_norm_g, cos, sin, ctx_out)
    c_l, c_c = window_attention_branch(*pl[8:11], *pc[8:11], sink, cos, sin, ctx_out)

    def merge(a, bb, cc, gate_logits):
        g_a, g_b, g_c = jnp.split(jax.nn.sigmoid(gate_logits), N_BRANCH, axis=-1)
        return (g_a * (a @ w_br_a) + g_b * (bb @ w_br_b) + g_c * (cc @ w_br_c)) @ w_out

    xl = xl + ga1 * merge(a_l, b_l, c_l, pl[11])
    if ctx_out:
        xc = xc + cga1 * merge(a_c, b_c, c_c, pc[11])

    hl = (rms_norm(xl, norm2_g) * (1 + sc2) + sh2).reshape(-1, d)
    if ctx_out:
        hc = (rms_norm(xc, norm2_g) * (1 + csc2) + csh2).reshape(-1, d)
        y = hier_moe(jnp.concatenate([hc, hl], axis=0), w_group, w_router, w_gate, w_up, w_down)
        n_c = hc.shape[0]
        xc = xc + cga2 * y[:n_c].reshape(xc.shape)
        y_l = y[n_c:]
    else:
        y_l = hier_moe(hl, w_group, w_router, w_gate, w_up, w_down)
    xl = xl + ga2 * y_l.reshape(xl.shape)
    return xl, xc


def setup_inputs(seed: int = 0) -> dict:
    key = jax.random.key(seed)
    ks = jax.random.split(key, 24)
    d = D_MODEL

    def nrm(k, shape, scale):
        return jax.random.normal(k, shape, jnp.float32) * scale

    return {
        "x": nrm(ks[0], (BATCH, SEQ, d), 1.0),
        "c": nrm(ks[1], (BATCH, d), 1.0),
        "ctx": nrm(ks[2], (BATCH, CTX_LEN, d), 1.0),
        "c_ctx": nrm(ks[3], (d,), 1.0),
        "w_mod": nrm(ks[4], (DEPTH, d, 6 * d), 0.5 * d ** -0.5),
        "b_mod": nrm(ks[5], (DEPTH, 6 * d), 0.02),
        "norm1_g": 1.0 + nrm(ks[6], (DEPTH, d), 0.02),
        "norm2_g": 1.0 + nrm(ks[7], (DEPTH, d), 0.02),
        "w_in": nrm(ks[8], (DEPTH, d, D_IN), d ** -0.5),
        "hgrn_lb_logits": nrm(ks[9], (DEPTH, HG_KW), 0.5),
        "hgrn_out_norm_g": 1.0 + nrm(ks[10], (DEPTH, HG_VDIM), 0.02),
        "attn_q_norm_g": 1.0 + nrm(ks[11], (DEPTH, HEAD_DIM), 0.02),
        "attn_k_norm_g": 1.0 + nrm(ks[12], (DEPTH, HEAD_DIM), 0.02),
        "swa_sink": nrm(ks[13], (DEPTH, SWA_HEADS), 1.0),
        "w_branch_a": nrm(ks[14], (DEPTH, HG_VW, d), HG_VW ** -0.5),
        "w_branch_b": nrm(ks[15], (DEPTH, ATT_HEADS * HEAD_DIM, d), (ATT_HEADS * HEAD_DIM) ** -0.5),
        "w_branch_c": nrm(ks[16], (DEPTH, SWA_HEADS * HEAD_DIM, d), (SWA_HEADS * HEAD_DIM) ** -0.5),
        "w_out": nrm(ks[17], (DEPTH, d, d), d ** -0.5),
        "w_group": nrm(ks[18], (DEPTH, d, N_GROUPS), d ** -0.5),
        "w_router": nrm(ks[19], (DEPTH, d, N_EXPERTS), d ** -0.5),
        "w_exp_gate": nrm(ks[20], (DEPTH, N_EXPERTS, d, D_EXPERT), d ** -0.5),
        "w_exp_up": nrm(ks[21], (DEPTH, N_EXPERTS, d, D_EXPERT), d ** -0.5),
        "w_exp_down": nrm(ks[22], (DEPTH, N_EXPERTS, D_EXPERT, d), D_EXPERT ** -0.5),
        "final_norm_g": 1.0 + nrm(ks[23], (d,), 0.02),
    }


def reference(x, c, ctx, c_ctx, w_mod, b_mod, norm1_g, norm2_g, w_in, hgrn_lb_logits, hgrn_out_norm_g,
              attn_q_norm_g, attn_k_norm_g, swa_sink, w_branch_a, w_branch_b, w_branch_c, w_out,
              w_group, w_router, w_exp_gate, w_exp_up, w_exp_down, final_norm_g):
    cos, sin = axial_rope(x.shape[1], x.dtype)
    lb_p = jax.nn.softmax(hgrn_lb_logits.astype(jnp.float32), axis=0)
    lower_bounds = jnp.cumsum(lb_p, axis=0) - lb_p[0]
    xl, xc = x, ctx
    for layer in range(DEPTH):
        mod_l = jax.nn.silu(c) @ w_mod[layer] + b_mod[layer]
        mod_c = jax.nn.silu(c_ctx) @ w_mod[layer] + b_mod[layer]
        xl, xc = trunk_layer(xl, xc, mod_l, mod_c, cos, sin, norm1_g[layer], norm2_g[layer], w_in[layer],
                             lower_bounds[layer], hgrn_out_norm_g[layer], attn_q_norm_g[layer],
                             attn_k_norm_g[layer], swa_sink[layer], w_branch_a[layer], w_branch_b[layer],
                             w_branch_c[layer], w_out[layer], w_group[layer], w_router[layer],
                             w_exp_gate[layer], w_exp_up[layer], w_exp_down[layer],
                             ctx_out=layer < DEPTH - 1)
    return rms_norm(xl, final_norm_g)
```

```python
import numpy as np
import ml_dtypes
import concourse.bass as bass
import concourse.mybir as mybir
from concourse.bass_utils import run_bass_kernel_spmd

F32 = mybir.dt.float32
BF16 = mybir.dt.bfloat16
AF = mybir.ActivationFunctionType
ALU = mybir.AluOpType
AX = mybir.AxisListType
NPBF = ml_dtypes.bfloat16

ENGS = ["sync", "scalar", "vector", "gpsimd", "tensor"]
EPOCH = 12000
DMA_SLOTS = 6
DMA_EPOCH = 700

D = 1024
CTXT = 2
EPS = 1e-6
TINY = 1e-30


class Prog:
    def __init__(self, nc):
        self.nc = nc
        self.ops = {e: [] for e in ENGS}
        self.ccnt = {e: 0 for e in ENGS}
        self.dcnt = {e: 0 for e in ENGS}
        self.tok_w = {}
        self.tok_r = {}
        self.seen = {e: {} for e in ENGS}
        self.sems = {}
        self.nsem = 0
        self.uid = 0

    def _sem(self, key):
        if key not in self.sems:
            self.sems[key] = self.nc.alloc_semaphore(name="s%d" % self.nsem)
            self.nsem += 1
        return self.sems[key]

    def _ev_sem(self, ev):
        kind, eng, idx = ev
        if kind == "c":
            ep = (idx - 1) // EPOCH
            return ("c", eng, ep), idx - ep * EPOCH
        slot = idx % DMA_SLOTS
        use = idx // DMA_SLOTS
        ep = use // DMA_EPOCH
        return ("d", eng, slot, ep), 16 * (use - ep * DMA_EPOCH + 1)

    def add(self, eng, fn, reads=(), writes=(), dma=False):
        deps = set()
        for t in reads:
            w = self.tok_w.get(t)
            if w is not None:
                deps.add(w)
        for t in writes:
            w = self.tok_w.get(t)
            if w is not None:
                deps.add(w)
            for r in self.tok_r.get(t, ()):
                deps.add(r)
        if dma:
            i = self.dcnt[eng]
            self.dcnt[eng] += 1
            ev = ("d", eng, i)
            if i >= DMA_SLOTS:
                deps.add(("d", eng, i - DMA_SLOTS))
        else:
            self.ccnt[eng] += 1
            ev = ("c", eng, self.ccnt[eng])
        need = {}
        for d in deps:
            if d == ev:
                continue
            if d[0] == "c" and d[1] == eng == "tensor" and not dma:
                continue
            k, v = self._ev_sem(d)
            if self.seen[eng].get(k, 0) >= v:
                continue
            if need.get(k, 0) < v:
                need[k] = v
        for k, v in need.items():
            self.seen[eng][k] = v
        self.ops[eng].append((fn, sorted(need.items(), key=str), self._ev_sem(ev), dma))
        for t in reads:
            self.tok_r.setdefault(t, []).append(ev)
        for t in writes:
            self.tok_w[t] = ev
            self.tok_r[t] = []
        return ev

    def final_waits(self, eng="sync"):
        need = {}
        for e in ENGS:
            if self.ccnt[e] and e != eng:
                k, v = self._ev_sem(("c", e, self.ccnt[e]))
                need[k] = v
            n = self.dcnt[e]
            for i in range(max(0, n - DMA_SLOTS), n):
                k, v = self._ev_sem(("d", e, i))
                need[k] = max(need.get(k, 0), v)
        self.ops[eng].append((None, sorted(need.items(), key=str), None, False))

    def emit(self):
        for e in ENGS:
            for (fn, waits, inc, dma) in self.ops[e]:
                for k, v in waits:
                    self._sem(k)
                if inc is not None:
                    self._sem(inc[0])
        with self.nc.Block() as block:
            for e in ENGS:
                if not self.ops[e]:
                    continue

                def body(engh, e=e):
                    for (fn, waits, inc, dma) in self.ops[e]:
                        for k, v in waits:
                            engh.wait_ge(self.sems[k], v)
                        if fn is None:
                            continue
                        ins = fn(engh)
                        ins.then_inc(self.sems[inc[0]], 16 if dma else 1)

                getattr(block, e)(body)

    def sb(self, name, shape, dt):
        return self.nc.alloc_sbuf_tensor(name, list(shape), dt)

    def ps(self, name, shape, dt=F32):
        return self.nc.alloc_psum_tensor(name, list(shape), dt)

    def dma(self, out, in_, r, w, q=None):
        if q is None:
            q = "sync" if (self.dcnt["sync"] <= self.dcnt["gpsimd"]) else "gpsimd"
        self.add(q, lambda e: e.dma_start(out=out, in_=in_), r, w, dma=True)

    def act(self, out, in_, func, r, w, scale=1.0, bias=0.0, accum=None):
        if accum is None:
            self.add("scalar", lambda e: e.activation(out=out, in_=in_, func=func, scale=scale, bias=bias), r, w)
        else:
            self.add("scalar", lambda e: e.activation(out=out, in_=in_, func=func, scale=scale, bias=bias,
                                                      accum_out=accum), r, w)

    def tt(self, out, a, b, op, r, w, eng="vector"):
        self.add(eng, lambda e: e.tensor_tensor(out=out, in0=a, in1=b, op=op), r, w)

    def ts(self, out, a, s1, op0, r, w, s2=None, op1=None, eng="vector"):
        if op1 is None:
            self.add(eng, lambda e: e.tensor_scalar(out=out, in0=a, scalar1=s1, scalar2=None, op0=op0), r, w)
        else:
            self.add(eng, lambda e: e.tensor_scalar(out=out, in0=a, scalar1=s1, scalar2=s2, op0=op0, op1=op1), r, w)

    def stt(self, out, a, s, b, op0, op1, r, w):
        self.add("vector", lambda e: e.scalar_tensor_tensor(out=out, in0=a, scalar=s, in1=b, op0=op0, op1=op1), r, w)

    def cp(self, out, in_, r, w, eng="vector"):
        if eng == "scalar":
            self.add(eng, lambda e: e.activation(out=out, in_=in_, func=AF.Copy), r, w)
        else:
            self.add(eng, lambda e: e.tensor_copy(out=out, in_=in_), r, w)

    def mm(self, out, lhsT, rhs, start, stop, r, w):
        self.add("tensor", lambda e: e.matmul(out, lhsT=lhsT, rhs=rhs, start=start, stop=stop), r, w)

    def tr(self, out, in_, ident, r, w):
        self.add("tensor", lambda e: e.transpose(out=out, in_=in_, identity=ident), r, w)

    def ident(self, name, dt):
        t = self.sb(name, [128, 128], dt)
        self.add("gpsimd", lambda e: e.memset(t[:], 0.0), (), [name])
        self.add("gpsimd", lambda e: e.affine_select(out=t[:], in_=t[:], compare_op=ALU.not_equal, fill=1.0,
                                                     base=0, pattern=[[-1, 128]], channel_multiplier=1), [name], [name])
        return t


def _run(nc, in_maps):
    res = run_bass_kernel_spmd(nc, in_maps, core_ids=list(range(len(in_maps))))
    return res.results


def emit_mod(P, nc, cc, wmod, bmod, ncols, pfx, wst, wtoks):
    scT = P.sb(pfx + "scT", [128, 8, 2], F32)
    P.dma(scT[:], cc.rearrange("(c p) j -> p c j", p=128), (), ["scT"])
    P.act(scT[:], scT[:], AF.Silu, ["scT"], ["scT"])
    rows = P.sb(pfx + "rows", [1, 2, ncols], F32)
    brow = P.sb(pfx + "brow", [1, ncols], F32)
    P.dma(brow[:], bmod.rearrange("(o n) -> o n", o=1), (), ["brow"])
    pm = P.ps(pfx + "pm", [1, 2, 512])
    for g in range(ncols // 512):
        w = wst[g % 2]
        wt = wtoks[g % 2]
        P.dma(w[:], wmod[:, g * 512:(g + 1) * 512].rearrange("(c p) n -> p c n", p=128), (), [wt])
        for j in range(2):
            for c in range(8):
                P.mm(pm[:, j, :], scT[:, c, j:j + 1], w[:, c, :], c == 0, c == 7, [wt, "scT"], ["pm"])
        for j in range(2):
            P.tt(rows[:, j, g * 512:(g + 1) * 512], pm[:, j, :], brow[:, g * 512:(g + 1) * 512], ALU.add,
                 ["pm", "brow"], ["modrow"])
    return rows


def row_to_cols(P, nc, row_ap, n, cols_out, one11, pcol, r, w):
    k = n // 128
    for c in range(k):
        P.mm(pcol[:, c:c + 1], row_ap[:, c * 128:(c + 1) * 128], one11, True, True, r, ["pcol"])
    P.cp(cols_out, pcol[:, 0:k], ["pcol"], w)


def row_bcast(P, nc, row_ap, n, out_tile, ones1, pb, r, w):
    for g in range(0, n, 512):
        m = min(512, n - g)
        P.mm(pb[:, 0:m], ones1, row_ap[:, g:g + m], True, True, r, ["pb"])
        P.cp(out_tile[:, g:g + m], pb[:, 0:m], ["pb"], w)


def emit_norm_T(P, nc, xt, xtok, i, scal, bias, ident, hT, hTtok, pfx, ptr, sq, ss, xh):
    b = i % 2
    P.act(sq[b][:], xt, AF.Square, [xtok], [pfx + "sq%d" % b, pfx + "ss%d" % b], accum=ss[b][:])
    P.act(ss[b][:], ss[b][:], AF.Sqrt, [pfx + "ss%d" % b], [pfx + "ss%d" % b], scale=1.0 / D, bias=EPS)
    P.add("vector", lambda e: e.reciprocal(out=ss[b][:], in_=ss[b][:]), [pfx + "ss%d" % b], [pfx + "ss%d" % b])
    P.ts(xh[b][:], xt, ss[b][:, 0:1], ALU.mult, [xtok, pfx + "ss%d" % b], [pfx + "xh%d" % b])
    for half in range(2):
        pt = ptr[half]
        ptk = pfx + "ptr%d" % half
        for c4 in range(4):
            c = half * 4 + c4
            P.tr(pt[:, c4, :], xh[b][:, c * 128:(c + 1) * 128], ident[:], [pfx + "xh%d" % b, "identf"], [ptk])
        for c4 in range(4):
            c = half * 4 + c4
            P.act(hT[:, c, :], pt[:, c4, :], AF.Identity, [ptk, "modcols"], [hTtok],
                  scale=scal[:, c:c + 1], bias=bias[:, c:c + 1])


def build_PA(NT, layer):
    NTOK = NT * 128
    nc = bass.Bass("TRN2", target_bir_lowering=False)
    dt_in = lambda n, s, d=F32: nc.dram_tensor(n, list(s), d, kind="ExternalInput").ap()
    dt_out = lambda n, s, d: nc.dram_tensor(n, list(s), d, kind="ExternalOutput").ap()
    x = dt_in("x", [NTOK, D])
    cc = dt_in("cc", [D, 2])
    wmod = dt_in("wmod", [D, 2048])
    bmod = dt_in("bmod", [2048])
    n1g = dt_in("n1g", [D])
    win = dt_in("win", [D, 4096])
    lbl = dt_in("lbl", [2, 512])
    aqg = dt_in("aqg", [64])
    akg = dt_in("akg", [64])
    cosd = dt_in("cos", [NTOK, 32])
    sind = dt_in("sin", [NTOK, 32])
    QS = dt_out("QS", [NTOK, 512], BF16)
    KKf = dt_out("KKf", [NTOK, 512], BF16)
    KKb = dt_out("KKb", [NTOK, 512], BF16)
    LFf = dt_out("LFf", [NTOK, 512], F32)
    LFb = dt_out("LFb", [NTOK, 512], F32)
    VH = dt_out("VH", [NTOK, 512], BF16)
    OG = dt_out("OG", [NTOK, 512], F32)
    QA = dt_out("QA", [NTOK, 512], BF16)
    QW = dt_out("QW", [NTOK, 512], BF16)
    KV4 = dt_out("KV4", [NTOK, 512], BF16)

    P = Prog(nc)
    identf = P.ident("identf", F32)
    one1 = P.sb("one1", [1, 128], F32)
    P.add("vector", lambda e: e.memset(one1[:], 1.0), (), ["one1"])
    Wb = P.sb("Wb", [128, 8, 4096], BF16)
    wst = [P.sb("wstA%d" % i, [128, 8, 512], F32) for i in range(2)]
    src_groups = [(0, 512), (512, 512), (1024, 512), (1536, 512), (2048, 512), (2560, 512), (3328, 512),
                  (3072, 128), (3840, 128), (3200, 128), (3968, 128)]
    dst = 0
    for gi, (s0, n) in enumerate(src_groups):
        w = wst[gi % 2]
        wt = "wstA%d" % (gi % 2)
        P.dma(w[:, :, 0:n], win[:, s0:s0 + n].rearrange("(c p) n -> p c n", p=128), (), [wt])
        P.cp(Wb[:, :, dst:dst + n], w[:, :, 0:n], [wt], ["Wb"], eng="gpsimd")
        dst += n
    rows = emit_mod(P, nc, cc, wmod, bmod, 2048, "A", wst, ["wstA0", "wstA1"])
    g1row = P.sb("g1row", [1, D], F32)
    P.dma(g1row[:], n1g.rearrange("(o n) -> o n", o=1), (), ["g1row"])
    scrow = P.sb("scrow", [1, 2, D], F32)
    for j in range(2):
        P.stt(scrow[:, j, :], rows[:, j, 1024:2048], 1.0, g1row[:], ALU.add, ALU.mult, ["modrow", "g1row"], ["scrow"])
    pcol = P.ps("pcol", [128, 16])
    one11 = one1[:, 0:1]
    scal = [P.sb("scal%d" % j, [128, 8], F32) for j in range(2)]
    bias = [P.sb("bias%d" % j, [128, 8], F32) for j in range(2)]
    for j in range(2):
        row_to_cols(P, nc, scrow[:, j, :], D, scal[j][:], one11, pcol, ["scrow", "one1"], ["modcols"])
        row_to_cols(P, nc, rows[:, j, 0:1024], D, bias[j][:], one11, pcol, ["modrow", "one1"], ["modcols"])
    lbB = P.sb("lbB", [128, 512], F32)
    omlbB = P.sb("omlbB", [128, 512], F32)
    e0 = P.sb("e0", [128, 512], F32)
    e1 = P.sb("e1", [128, 512], F32)
    P.dma(e0[:], lbl[0, :].partition_broadcast(128), (), ["e0"])
    P.dma(e1[:], lbl[1, :].partition_broadcast(128), (), ["e1"])
    P.act(e0[:], e0[:], AF.Exp, ["e0"], ["e0"])
    P.act(e1[:], e1[:], AF.Exp, ["e1"], ["e1"])
    P.tt(lbB[:], e0[:], e1[:], ALU.add, ["e0", "e1"], ["lbB"])
    P.add("vector", lambda e: e.reciprocal(out=lbB[:], in_=lbB[:]), ["lbB"], ["lbB"])
    P.tt(e0[:], e0[:], lbB[:], ALU.mult, ["e0", "lbB"], ["e0"])
    P.tt(e1[:], e1[:], lbB[:], ALU.mult, ["e1", "lbB"], ["e1"])
    if layer == 0:
        P.tt(lbB[:], e0[:], e0[:], ALU.subtract, ["e0"], ["lbB"])
    else:
        P.tt(lbB[:], e0[:], e1[:], ALU.add, ["e0", "e1"], ["lbB"])
        P.tt(lbB[:], lbB[:], e0[:], ALU.subtract, ["lbB", "e0"], ["lbB"])
    P.ts(omlbB[:], lbB[:], -1.0, ALU.mult, ["lbB"], ["omlbB"], s2=1.0, op1=ALU.add)
    gq = P.sb("gq", [128, 64], F32)
    gk = P.sb("gk", [128, 64], F32)
    P.dma(gq[:], aqg.partition_broadcast(128), (), ["gq"])
    P.dma(gk[:], akg.partition_broadcast(128), (), ["gk"])

    xt = [P.sb("xt%d" % i, [128, D], F32) for i in range(2)]
    sq = [P.sb("sq%d" % i, [128, D], F32) for i in range(2)]
    ss = [P.sb("ss%d" % i, [128, 1], F32) for i in range(2)]
    xh = [P.sb("xh%d" % i, [128, D], F32) for i in range(2)]
    hT = [P.sb("hT%d" % i, [128, 8, 128], BF16) for i in range(2)]
    cs = [P.sb("cs%d" % i, [128, 2, 32], F32) for i in range(2)]
    ptr = [P.ps("ptr%d" % i, [128, 4, 128]) for i in range(2)]
    pg = [P.ps("pg%d" % i, [128, 512]) for i in range(3)]
    NW = 6
    wk = [[P.sb("wk%d_%d" % (k, i), [128, 512], F32) for i in range(2)] for k in range(NW)]
    ob = [[P.sb("ob%d_%d" % (k, i), [128, 512], BF16) for i in range(2)] for k in range(3)]
    sm = [P.sb("sm%d" % i, [128, 8], F32) for i in range(2)]
    gcount = [0]

    def proj(i, g):
        k = gcount[0] % 3
        gcount[0] += 1
        for c in range(8):
            P.mm(pg[k][:], hT[i % 2][:, c, :], Wb[:, c, g * 512:(g + 1) * 512], c == 0, c == 7,
                 ["hT%d" % (i % 2), "Wb"], ["pg%d" % k])
        return pg[k], "pg%d" % k

    def rope(src, stok, dst_, dtok, H, b, eng="gpsimd"):
        sv = src.rearrange("p (h two d) -> p h two d", h=H, two=2)
        dv = dst_.rearrange("p (h two d) -> p h two d", h=H, two=2)
        cB = cs[b][:, 0, :].unsqueeze(1).to_broadcast([128, H, 32])
        sB = cs[b][:, 1, :].unsqueeze(1).to_broadcast([128, H, 32])
        t1 = wk[4][b][:, 0:H * 32].rearrange("p (h d) -> p h d", h=H)
        t2 = wk[5][b][:, 0:H * 32].rearrange("p (h d) -> p h d", h=H)
        a, t = "wk4_%d" % b, "wk5_%d" % b
        ctk = "cs%d" % b
        P.tt(t1, sv[:, :, 0, :], cB, ALU.mult, [stok, ctk], [a], eng=eng)
        P.tt(t2, sv[:, :, 1, :], sB, ALU.mult, [stok, ctk], [t], eng=eng)
        P.tt(dv[:, :, 0, :], t1, t2, ALU.subtract, [a, t], [dtok], eng=eng)
        P.tt(t1, sv[:, :, 1, :], cB, ALU.mult, [stok, ctk], [a], eng=eng)
        P.tt(t2, sv[:, :, 0, :], sB, ALU.mult, [stok, ctk], [t], eng=eng)
        P.tt(dv[:, :, 1, :], t1, t2, ALU.add, [a, t], [dtok], eng=eng)

    def qknorm(src, stok, H, gB, gtok, b, outw, otok):
        v = src.rearrange("p (h d) -> p h d", h=H)
        tmp = wk[3][b][:, 0:H * 64]
        P.tt(tmp, src, src, ALU.mult, [stok], ["wk3_%d" % b])
        P.add("vector", lambda e: e.tensor_reduce(out=sm[b][:, 0:H], in_=tmp.rearrange("p (h d) -> p h d", h=H),
                                                  axis=AX.X, op=ALU.add), ["wk3_%d" % b], ["sm%d" % b])
        P.act(sm[b][:, 0:H], sm[b][:, 0:H], AF.Sqrt, ["sm%d" % b], ["sm%d" % b], scale=1.0 / 64, bias=EPS)
        P.add("vector", lambda e: e.reciprocal(out=sm[b][:, 0:H], in_=sm[b][:, 0:H]), ["sm%d" % b], ["sm%d" % b])
        ov = outw.rearrange("p (h d) -> p h d", h=H)
        P.tt(ov, v, sm[b][:, 0:H].unsqueeze(2).to_broadcast([128, H, 64]), ALU.mult, [stok, "sm%d" % b], [otok])
        P.tt(ov, ov, gB[:, :].unsqueeze(1).to_broadcast([128, H, 64]), ALU.mult, [otok, gtok], [otok])

    for i in range(NT):
        b = i % 2
        j = 1 if i < CTXT else 0
        rs = slice(i * 128, (i + 1) * 128)
        P.dma(xt[b][:], x[rs, :], (), ["xt%d" % b])
        P.dma(cs[b][:, 0, :], cosd[rs, :], (), ["cs%d" % b])
        P.dma(cs[b][:, 1, :], sind[rs, :], (), ["cs%d" % b])
        emit_norm_T(P, nc, xt[b][:], "xt%d" % b, i, scal[j], bias[j], identf, hT[b], "hT%d" % b, "A", ptr, sq, ss, xh)
        pgt, pk = proj(i, 0)
        P.act(wk[0][b][:], pgt[:], AF.Silu, [pk], ["wk0_%d" % b])
        P.ts(ob[0][b][:], wk[0][b][:], 128.0 ** -0.5, ALU.mult, ["wk0_%d" % b], ["ob0_%d" % b], eng="gpsimd")
        P.dma(QS[rs, :], ob[0][b][:], ["ob0_%d" % b], ["QS"])
        for (g, KKo, LFo) in ((1, KKf, LFf), (2, KKb, LFb)):
            pgt, pk = proj(i, g)
            P.act(wk[0][b][:], pgt[:], AF.Sigmoid, [pk], ["wk0_%d" % b])
            P.tt(wk[1][b][:], wk[0][b][:], omlbB[:], ALU.mult, ["wk0_%d" % b, "omlbB"], ["wk1_%d" % b])
            P.tt(ob[1][b][:], omlbB[:], wk[1][b][:], ALU.subtract, ["wk1_%d" % b, "omlbB"], ["ob1_%d" % b], eng="gpsimd")
            P.stt(wk[2][b][:], wk[1][b][:], TINY, lbB[:], ALU.max, ALU.add, ["wk1_%d" % b, "lbB"], ["wk2_%d" % b])
            P.act(wk[2][b][:], wk[2][b][:], AF.Ln, ["wk2_%d" % b], ["wk2_%d" % b])
            P.dma(KKo[rs, :], ob[1][b][:], ["ob1_%d" % b], ["KK"])
            P.dma(LFo[rs, :], wk[2][b][:], ["wk2_%d" % b], ["LF"])
        pgt, pk = proj(i, 3)
        P.cp(ob[2][b][:], pgt[:], [pk], ["ob2_%d" % b])
        P.dma(VH[rs, :], ob[2][b][:], ["ob2_%d" % b], ["VH"])
        pgt, pk = proj(i, 4)
        P.act(wk[0][b][:], pgt[:], AF.Silu, [pk], ["wk0_%d" % b])
        P.dma(OG[rs, :], wk[0][b][:], ["wk0_%d" % b], ["OG"])
        pgt, pk = proj(i, 5)
        P.act(wk[0][b][:], pgt[:], AF.Copy, [pk], ["wk0_%d" % b])
        qknorm(wk[0][b][:], "wk0_%d" % b, 8, gq, "gq", b, wk[1][b][:], "wk1_%d" % b)
        rope(wk[1][b][:], "wk1_%d" % b, ob[0][b][:], "ob0_%d" % b, 8, b)
        P.dma(QA[rs, :], ob[0][b][:], ["ob0_%d" % b], ["QA"])
        pgt, pk = proj(i, 6)
        P.act(wk[0][b][:], pgt[:], AF.Copy, [pk], ["wk0_%d" % b])
        rope(wk[0][b][:], "wk0_%d" % b, ob[1][b][:], "ob1_%d" % b, 8, b)
        P.dma(QW[rs, :], ob[1][b][:], ["ob1_%d" % b], ["QW"])
        pgt, pk = proj(i, 7)
        P.act(wk[0][b][:], pgt[:], AF.Copy, [pk], ["wk0_%d" % b])
        qknorm(wk[0][b][:, 0:128], "wk0_%d" % b, 2, gk, "gk", b, wk[0][b][:, 0:128], "wk0_%d" % b)
        rope(wk[0][b][:, 0:256], "wk0_%d" % b, ob[2][b][:, 0:256], "ob2_%d" % b, 4, b)
        P.cp(ob[2][b][:, 256:512], wk[0][b][:, 256:512], ["wk0_%d" % b], ["ob2_%d" % b], eng="gpsimd")
        P.dma(KV4[rs, :], ob[2][b][:], ["ob2_%d" % b], ["KV4"])
    P.final_waits("sync")
    P.emit()
    return nc


def hgrn_consts():
    i = np.arange(128)
    ch = i // 64
    blk = i // 32
    out = []
    u = i[:, None]
    t = i[None, :]
    same_ch = (ch[:, None] == ch[None, :])
    r = (blk * 32 + 16)[None, :]
    Mdq = (((u > r) & (u <= t)).astype(np.float32) - ((u > t) & (u <= r)).astype(np.float32))
    Mcq = (same_ch & (u <= t)).astype(np.float32)
    cs = (ch * 64)[None, :]
    Moq_full = ((u >= cs + 32) & (u <= t) & same_ch).astype(np.float32)
    Mok_full = ((u > t) & (u <= cs + 31) & same_ch).astype(np.float32)
    second = np.concatenate([np.arange(32, 64), np.arange(96, 128)])
    first = np.concatenate([np.arange(0, 32), np.arange(64, 96)])
    Mend = (same_ch & (u > t)).astype(np.float32)
    Mdiag = ((blk[:, None] == blk[None, :]) & (u <= t)).astype(np.float32)
    Moff = (same_ch & ((i % 64) < 32)[:, None] & ((i % 64) >= 32)[None, :]).astype(np.float32)
    MCf = np.concatenate([Mdq, Mcq, -Mdq, Moq_full[:, second], Mok_full[:, first]], 1)
    out.append(dict(MC=MCf, Mend=Mend, Mdiag=Mdiag, Moff=Moff))
    fl = lambda M: M[::-1, ::-1].copy()
    Mdq_b, Mcq_b = fl(Mdq), fl(Mcq)
    Moq_b, Mok_b = fl(Moq_full), fl(Mok_full)
    MCb = np.concatenate([Mdq_b, Mcq_b, -Mdq_b, Moq_b[:, first], Mok_b[:, second]], 1)
    out.append(dict(MC=MCb, Mend=fl(Mend), Mdiag=fl(Mdiag), Moff=fl(Moff)))
    return out


def build_PB(NL, parts=(1, 1, 1)):
    NTB = CTXT + NL
    NTOK = NTB * 128
    nc = bass.Bass("TRN2", target_bir_lowering=False)
    dt_in = lambda n, s, d=F32: nc.dram_tensor(n, list(s), d, kind="ExternalInput").ap()
    dt_out = lambda n, s, d: nc.dram_tensor(n, list(s), d, kind="ExternalOutput").ap()
    qs = dt_in("qs", [NTOK, 128], BF16)
    kk = [dt_in("kk%d" % d, [NTOK, 128], BF16) for d in range(2)]
    lf = [dt_in("lf%d" % d, [NTOK, 128]) for d in range(2)]
    vh = dt_in("vh", [NTOK, 128], BF16)
    og = dt_in("og", [NTOK, 128])
    hg = dt_in("hg", [128])
    MCd = [dt_in("MC%d" % d, [128, 512]) for d in range(2)]
    Mendd = [dt_in("Mend%d" % d, [128, 128]) for d in range(2)]
    Mdiagd = [dt_in("Mdiag%d" % d, [128, 128]) for d in range(2)]
    Moffd = [dt_in("Moff%d" % d, [128, 128]) for d in range(2)]
    qa = dt_in("qa", [NTOK, 128], BF16)
    ka = dt_in("ka", [NTOK, 128], BF16)
    va = dt_in("va", [NTOK, 64], BF16)
    qw = dt_in("qw", [NTOK, 128], BF16)
    kw = dt_in("kw", [NTOK, 128], BF16)
    vw = dt_in("vw", [NTOK, 64], BF16)
    sink = dt_in("sink", [2])
    wm = dt_in("wm", [2, 128, 128])
    Ao = dt_out("A", [NTOK, 128], BF16)
    Bo = dt_out("B", [NTOK, 128], BF16)
    Co = dt_out("C", [NTOK, 128], BF16)

    P = Prog(nc)
    identf = P.ident("identf", F32)
    identb = P.sb("identb", [128, 128], BF16)
    P.cp(identb[:], identf[:], ["identf"], ["identb"])

    MC = [P.sb("MCs%d" % d, [128, 512], F32) for d in range(2)]
    Mend = [P.sb("Mends%d" % d, [128, 128], F32) for d in range(2)]
    Mdiag = [P.sb("Mdiags%d" % d, [128, 128], F32) for d in range(2)]
    Moff = [P.sb("Moffs%d" % d, [128, 128], F32) for d in range(2)]
    for d in range(2):
        P.dma(MC[d][:], MCd[d][:, :], (), ["consts"])
        P.dma(Mend[d][:], Mendd[d][:, :], (), ["consts"])
        P.dma(Mdiag[d][:], Mdiagd[d][:, :], (), ["consts"])
        P.dma(Moff[d][:], Moffd[d][:, :], (), ["consts"])
    hgB = P.sb("hgB", [128, 128], F32)
    P.dma(hgB[:], hg.partition_broadcast(128), (), ["consts"])
    Oacc = P.sb("Oacc", [128, NTB, 128], F32)
    Sm = P.sb("Sm", [128, 128], F32)
    Sb = [P.sb("Sb%d" % i, [128, 128], BF16) for i in range(2)]
    lft = [P.sb("lft%d" % i, [128, 128], F32) for i in range(2)]
    kkt = [P.sb("kkt%d" % i, [128, 128], BF16) for i in range(2)]
    qst = [P.sb("qst%d" % i, [128, 128], BF16) for i in range(2)]
    vt = [P.sb("vt%d" % i, [128, 128], BF16) for i in range(2)]
    ogt = [P.sb("ogt%d" % i, [128, 128], F32) for i in range(2)]
    qkT = [P.sb("qkT%d" % i, [128, 2, 128], BF16) for i in range(2)]
    E = [P.sb("E%d" % i, [128, 512], F32) for i in range(2)]
    E2 = [P.sb("E2%d" % i, [128, 128], F32) for i in range(2)]
    Kt = [P.sb("Kt%d" % i, [128, 128], BF16) for i in range(2)]
    QC = [P.sb("QC%d" % i, [128, 6, 128], BF16) for i in range(2)]
    Pm = [P.sb("Pm%d" % i, [128, 2, 128], BF16) for i in range(2)]
    tot = [P.sb("tot%d" % i, [128, 128], F32) for i in range(2)]
    hs = [P.sb("hs%d" % i, [128, 2], F32) for i in range(2)]
    ao = [P.sb("ao%d" % i, [128, 128], BF16) for i in range(2)]
    for i in range(2):
        P.add("gpsimd", lambda e, i=i: e.memset(QC[i][:], 0.0), (), ["QC%d" % i])
    pbb = P.ps("pbb", [128, 8, 128], BF16)
    pbk = [P.ps("pbk%d" % i, [128, 512]) for i in range(6)]
    p_tr = pbb[:, 0:2, :]
    p_ex = pbk[0]
    p_e2 = pbk[1][:, 0:128]
    p_sc = pbk[2][:, 0:256].rearrange("p (c j) -> p c j", c=2)
    p_u = [pbk[3][:, 0:128], pbk[5][:, 0:128]]
    putok = ["p_u", "p_u1"]
    p_o = pbk[4][:, 0:128]
    P.add("vector", lambda e: e.memset(Sm[:], 0.0), (), ["Sm"])

    cnt = [0]

    def hgrn_tile(ti, d, reset):
        b = cnt[0] % 2
        cnt[0] += 1
        B = str(b)
        rs = slice(ti * 128, (ti + 1) * 128)
        if reset:
            P.add("vector", lambda e: e.memset(Sm[:], 0.0), (), ["Sm"])
        P.dma(lft[b][:], lf[d][rs, :], (), ["lft" + B])
        P.dma(kkt[b][:], kk[d][rs, :], (), ["kkt" + B])
        P.dma(qst[b][:], qs[rs, :], (), ["qst" + B])
        P.dma(vt[b][:], vh[rs, :], (), ["vt" + B])
        P.tr(p_tr[:, 0, :], qst[b][:], identb[:], ["qst" + B, "identb"], ["p_tr"])
        P.tr(p_tr[:, 1, :], kkt[b][:], identb[:], ["kkt" + B, "identb"], ["p_tr"])
        P.cp(qkT[b][:], p_tr, ["p_tr"], ["qkT" + B])
        P.mm(p_ex[:], lft[b][:], MC[d][:], True, True, ["lft" + B, "consts"], ["p_ex"])
        P.act(E[b][:], p_ex[:], AF.Exp, ["p_ex"], ["E" + B])
        qT = qkT[b][:, 0, :]
        kT = qkT[b][:, 1, :]
        r = ["qkT" + B, "E" + B]
        w = ["QC" + B]
        P.tt(QC[b][:, 0, :], qT, E[b][:, 0:128], ALU.mult, r, w)
        P.tt(QC[b][:, 1, 0:64], qT[:, 0:64], E[b][:, 128:192], ALU.mult, r, w)
        P.tt(QC[b][:, 2, 64:128], qT[:, 64:128], E[b][:, 192:256], ALU.mult, r, w)
        P.tt(QC[b][:, 3, :], kT, E[b][:, 256:384], ALU.mult, r, w, eng="gpsimd")
        qa_sl, ka_sl = (slice(32, 64), slice(0, 32)) if d == 0 else (slice(0, 32), slice(32, 64))
        v3 = lambda ap: ap.rearrange("p (c j) -> p c j", c=2)
        P.tt(v3(QC[b][:, 4, :])[:, :, qa_sl], v3(qT)[:, :, qa_sl], E[b][:, 384:448].rearrange("p (c j) -> p c j", c=2),
             ALU.mult, r, w, eng="gpsimd")
        P.tt(v3(QC[b][:, 5, :])[:, :, ka_sl], v3(kT)[:, :, ka_sl], E[b][:, 448:512].rearrange("p (c j) -> p c j", c=2),
             ALU.mult, r, w, eng="gpsimd")
        P.mm(p_e2, Mend[d][:], lft[b][:], True, True, ["lft" + B, "consts"], ["p_e2"])
        P.act(E2[b][:], p_e2, AF.Exp, ["p_e2"], ["E2" + B])
        P.tt(Kt[b][:], kkt[b][:], E2[b][:], ALU.mult, ["kkt" + B, "E2" + B], ["Kt" + B])
        P.mm(p_sc[:, 0, :], QC[b][:, 3, :], QC[b][:, 0, :], True, True, ["QC" + B], ["p_sc"])
        P.mm(p_sc[:, 1, :], QC[b][:, 5, :], QC[b][:, 4, :], True, True, ["QC" + B], ["p_sc"])
        P.tt(Pm[b][:, 0, :], p_sc[:, 0, :], Mdiag[d][:], ALU.mult, ["p_sc", "consts"], ["Pm" + B])
        P.tt(Pm[b][:, 1, :], p_sc[:, 1, :], Moff[d][:], ALU.mult, ["p_sc", "consts"], ["Pm" + B])
        for c in range(2):
            P.mm(p_u[c], Kt[b][64 * c:64 * c + 64, :], vt[b][64 * c:64 * c + 64, :], True, True,
                 ["Kt" + B, "vt" + B], [putok[c]])
        order = (0, 1) if d == 0 else (1, 0)
        for c in order:
            dcol = 128 + 64 * c + (63 if d == 0 else 0)
            P.cp(Sb[c][:], Sm[:], ["Sm"], ["Sb%d" % c], eng="scalar")
            P.stt(Sm[:], Sm[:], E[b][:, dcol:dcol + 1], p_u[c], ALU.mult, ALU.add, ["Sm", "E" + B, putok[c]], ["Sm"])
        P.mm(p_o, Pm[b][:, 0, :], vt[b][:], True, False, ["Pm" + B, "vt" + B], ["p_o"])
        P.mm(p_o, Pm[b][:, 1, :], vt[b][:], False, False, ["Pm" + B, "vt" + B], ["p_o"])
        P.mm(p_o, QC[b][:, 1, :], Sb[0][:], False, False, ["QC" + B, "Sb0"], ["p_o"])
        P.mm(p_o, QC[b][:, 2, :], Sb[1][:], False, True, ["QC" + B, "Sb1"], ["p_o"])
        if d == 0:
            P.cp(Oacc[:, ti, :], p_o, ["p_o"], ["Oacc%d" % ti])
        else:
            P.dma(ogt[b][:], og[rs, :], (), ["ogt" + B])
            P.tt(tot[b][:], p_o, Oacc[:, ti, :], ALU.add, ["p_o", "Oacc%d" % ti], ["tot" + B])
            P.act(E2[b][:], tot[b][:], AF.Square, ["tot" + B], ["E2" + B, "hs" + B], accum=hs[b][:, 0:1])
            P.act(hs[b][:, 0:1], hs[b][:, 0:1], AF.Sqrt, ["hs" + B], ["hs" + B], scale=1.0 / 128, bias=EPS)
            P.add("vector", lambda e: e.reciprocal(out=hs[b][:, 0:1], in_=hs[b][:, 0:1]), ["hs" + B], ["hs" + B])
            P.stt(tot[b][:], tot[b][:], hs[b][:, 0:1], hgB[:], ALU.mult, ALU.mult, ["tot" + B, "hs" + B, "consts"], ["tot" + B])
            P.tt(ao[b][:], tot[b][:], ogt[b][:], ALU.mult, ["tot" + B, "ogt" + B], ["ao" + B])
            P.dma(Ao[rs, :], ao[b][:], ["ao" + B], ["Ao"])

    fwd_order = list(range(NTB))
    bwd_order = [1, 0] + list(range(NTB - 1, CTXT - 1, -1))
    if parts[0]:
        for n, ti in enumerate(fwd_order):
            hgrn_tile(ti, 0, n == 0)
        for n, ti in enumerate(bwd_order):
            hgrn_tile(ti, 1, n == 0)

    QT2 = P.sb("QT2", [128, NTOK], BF16)
    KT2 = P.sb("KT2", [128, NTOK], BF16)
    Vx = P.sb("Vx", [128, NTB, 72], BF16)
    ld = [P.sb("ld%d" % i, [128, 8, 128], BF16) for i in range(2)]
    PT = [P.sb("PT%d" % i, [128, 512], BF16) for i in range(3)]
    OT = [P.sb("OT%d" % i, [65, 512], F32) for i in range(2)]
    bo = [P.sb("bo%d" % i, [128, 4, 128], BF16) for i in range(2)]
    rec = [P.sb("rec%d" % i, [128, 1], F32) for i in range(2)]
    wmt = P.sb("wmt", [128, 2, 128], F32)
    P.dma(wmt[:], wm.rearrange("m k q -> k m q"), (), ["consts2"])
    esink = P.sb("esink", [128, 2], F32)
    P.dma(esink[:], sink.partition_broadcast(128), (), ["esink"])
    P.act(esink[:], esink[:], AF.Exp, ["esink"], ["esink"])
    p_s = [pbk[0], pbk[1]]
    p_ot = [pbk[2], pbk[3]]
    p_f = pbk[4]
    pstok = ["p_ex", "p_e2"]
    pottok = ["p_sc", "p_u"]
    st = dict(ld=0, pt=0, s=0, ot=0, bo=0, rec=0)

    def load_T(src, dstT, dtok):
        for t0 in range(0, NTB, 8):
            n = min(8, NTB - t0)
            b = st["ld"] % 2
            st["ld"] += 1
            P.dma(ld[b][:, 0:n, :], src[t0 * 128:(t0 + n) * 128, :].rearrange("(t p) c -> p t c", p=128), (), ["ld%d" % b])
            for k in range(n):
                P.tr(pbb[:, k, :], ld[b][:, k, :], identb[:], ["ld%d" % b, "identb"], ["p_tr"])
            P.cp(dstT[:, t0 * 128:(t0 + n) * 128], pbb[:, 0:n, :].rearrange("p t c -> p (t c)"), ["p_tr"], [dtok])

    def attn_pass(qsrc, ksrc, vsrc, outd, window):
        load_T(qsrc, QT2, "QT2")
        load_T(ksrc, KT2, "KT2")
        P.add("gpsimd", lambda e: e.memset(Vx[:, :, 64:65], 1.0), (), ["Vx"])
        for v0 in range(0, NTB, 32):
            vn = min(32, NTB - v0)
            P.dma(Vx[:, v0:v0 + vn, 0:64], vsrc[v0 * 128:(v0 + vn) * 128, :].rearrange("(t p) c -> p t c", p=128), (), ["Vx"])
        if window:
            groups = [(t, 1) for t in range(NTB)]
        else:
            groups = [(0, CTXT)] + [(t, min(4, NTB - t)) for t in range(CTXT, NTB, 4)]
        for (t0, nt) in groups:
            nq = nt * 128
            q0 = t0 * 128
            if t0 < CTXT:
                kbs = [(kb, None) for kb in range(CTXT)]
            elif window:
                kbs = [(kb, None) for kb in range(CTXT)]
                if t0 - 1 >= CTXT:
                    kbs.append((t0 - 1, 0))
                kbs.append((t0, None))
                if t0 + 1 < NTB:
                    kbs.append((t0 + 1, 1))
            else:
                kbs = [(kb, None) for kb in range(NTB)]
            gb = st["bo"] % 2
            st["bo"] += 1
            for e_ in range(2):
                hp = slice(64 * e_, 64 * e_ + 64)
                ob_ = st["ot"] % 2
                st["ot"] += 1
                for n, (kb, msk) in enumerate(kbs):
                    sb_ = st["s"] % 2
                    st["s"] += 1
                    pb_ = st["pt"] % 3
                    st["pt"] += 1
                    P.mm(p_s[sb_][:, 0:nq], KT2[hp, kb * 128:(kb + 1) * 128], QT2[hp, q0:q0 + nq], True, True,
                         ["KT2", "QT2"], [pstok[sb_]])
                    P.act(PT[pb_][:, 0:nq], p_s[sb_][:, 0:nq], AF.Exp, [pstok[sb_]], ["PT%d" % pb_], scale=0.125)
                    if msk is not None:
                        P.tt(PT[pb_][:, 0:nq], PT[pb_][:, 0:nq], wmt[:, msk, :], ALU.mult, ["PT%d" % pb_, "consts2"],
                             ["PT%d" % pb_], eng="gpsimd")
                    P.mm(p_ot[ob_][0:65, 0:nq], Vx[:, kb, 0:65], PT[pb_][:, 0:nq], n == 0, n == len(kbs) - 1,
                         ["Vx", "PT%d" % pb_], [pottok[ob_]])
                P.cp(OT[ob_][:, 0:nq], p_ot[ob_][0:65, 0:nq], [pottok[ob_]], ["OT%d" % ob_])
                for k in range(nt):
                    rb = st["rec"] % 2
                    st["rec"] += 1
                    P.tr(p_f[:, 0:65], OT[ob_][:, k * 128:(k + 1) * 128], identf[0:65, 0:65], ["OT%d" % ob_, "identf"], ["p_o"])
                    if window:
                        P.ts(rec[rb][:], p_f[:, 64:65], esink[:, e_:e_ + 1], ALU.add, ["p_o", "esink"], ["rec%d" % rb])
                        P.add("vector", lambda e, rb=rb: e.reciprocal(out=rec[rb][:], in_=rec[rb][:]), ["rec%d" % rb], ["rec%d" % rb])
                    else:
                        P.add("vector", lambda e, rb=rb: e.reciprocal(out=rec[rb][:], in_=p_f[:, 64:65]), ["p_o"], ["rec%d" % rb])
                    P.ts(bo[gb][:, k, hp], p_f[:, 0:64], rec[rb][:, 0:1], ALU.mult, ["p_o", "rec%d" % rb], ["bo%d" % gb])
            P.dma(outd[q0:q0 + nq, :].rearrange("(t p) c -> p t c", p=128), bo[gb][:, 0:nt, :], ["bo%d" % gb], ["outd"])

    if parts[1]:
        attn_pass(qa, ka, va, Bo, False)
    if parts[2]:
        attn_pass(qw, kw, vw, Co, True)
    P.final_waits("sync")
    P.emit()
    return nc


def barrier(P):
    for e in ENGS:
        need = {}
        for o in ENGS:
            if o != e and P.ccnt[o]:
                k, v = P._ev_sem(("c", o, P.ccnt[o]))
                need[k] = v
            n = P.dcnt[o]
            for i in range(max(0, n - DMA_SLOTS), n):
                k, v = P._ev_sem(("d", o, i))
                need[k] = max(need.get(k, 0), v)
        need = {k: v for k, v in need.items() if P.seen[e].get(k, 0) < v}
        for k, v in need.items():
            P.seen[e][k] = v
        P.ops[e].append((None, sorted(need.items(), key=str), None, False))


def build_PC(NT, NEXP=32):
    NTOK = NT * 128
    nc = bass.Bass("TRN2", target_bir_lowering=False)
    dt_in = lambda n, s, d=F32: nc.dram_tensor(n, list(s), d, kind="ExternalInput").ap()
    dt_out = lambda n, s, d: nc.dram_tensor(n, list(s), d, kind="ExternalOutput").ap()
    x = dt_in("x", [NTOK, D])
    brd = [dt_in(n, [NTOK, 512], BF16) for n in ("A", "B", "C")]
    cc = dt_in("cc", [D, 2])
    wmod = dt_in("wmod", [D, 6144])
    bmod = dt_in("bmod", [6144])
    n1g = dt_in("n1g", [D])
    n2g = dt_in("n2g", [D])
    fng = dt_in("fng", [D])
    wgt = dt_in("wgt", [D, 3072])
    wbr = [dt_in(n, [512, D]) for n in ("wba", "wbb", "wbc")]
    wout = dt_in("wout", [D, D])
    wr = dt_in("wr", [D, 36])
    wg = dt_in("wg", [NEXP, D, 512])
    wu = dt_in("wu", [NEXP, D, 512])
    wd = dt_in("wd", [NEXP, 512, D])
    Xn = dt_out("Xn", [NTOK, D], F32)
    Yn = dt_out("Yn", [NTOK, D], F32)
    X1 = nc.dram_tensor("X1s", [NTOK, D], F32, kind="Internal").ap()
    H2T = nc.dram_tensor("H2Ts", [128, 8, NTOK], BF16, kind="Internal").ap()
    WR = nc.dram_tensor("WRs", [NTOK, 32], F32, kind="Internal").ap()

    P = Prog(nc)
    identf = P.ident("identf", F32)
    identb = P.sb("identb", [128, 128], BF16)
    P.cp(identb[:], identf[:], ["identf"], ["identb"])
    one1 = P.sb("one1", [1, 128], F32)
    P.add("vector", lambda e: e.memset(one1[:], 1.0), (), ["one1"])
    arena = P.sb("arena", [128, 45056], BF16)
    Wgt = arena[:, 0:24576].rearrange("p (c n) -> p c n", c=8)
    Wbr = arena[:, 24576:36864].rearrange("p (b c n) -> p b c n", b=3, c=4)
    Wout = arena[:, 36864:45056].rearrange("p (c n) -> p c n", c=8)
    wrb = P.sb("wrb", [128, 8, 36], BF16)
    wst = [P.sb("wstC%d" % i, [128, 4, 512], F32) for i in range(2)]
    wsc = [0]

    def load_cast(dst, src_ap, dtok, eng=None):
        b = wsc[0] % 2
        wsc[0] += 1
        P.dma(wst[b][:], src_ap, (), ["wstC%d" % b])
        en = eng or ("gpsimd", "vector")[b]
        P.cp(dst, wst[b][:], ["wstC%d" % b], [dtok], eng=en)

    for g in range(6):
        for h in range(2):
            load_cast(Wgt[:, 4 * h:4 * h + 4, g * 512:(g + 1) * 512],
                      wgt[512 * h:512 * h + 512, g * 512:(g + 1) * 512].rearrange("(c p) n -> p c n", p=128), "Wgt")
    for bi in range(3):
        for h in range(2):
            load_cast(Wbr[:, bi, :, h * 512:(h + 1) * 512], wbr[bi][:, h * 512:(h + 1) * 512].rearrange("(c p) n -> p c n", p=128), "Wbr")
    for g in range(2):
        for h in range(2):
            load_cast(Wout[:, 4 * h:4 * h + 4, g * 512:(g + 1) * 512],
                      wout[512 * h:512 * h + 512, g * 512:(g + 1) * 512].rearrange("(c p) n -> p c n", p=128), "Wout")
    wrs = P.sb("wrs", [128, 8, 36], F32)
    P.dma(wrs[:], wr.rearrange("(c p) n -> p c n", p=128), (), ["wrs"])
    P.cp(wrb[:], wrs[:], ["wrs"], ["wrb"])

    pbk = [P.ps("pbk%d" % i, [128, 512]) for i in range(7)]
    pbb = P.ps("pbb", [128, 8, 128], BF16)
    scT = P.sb("scT", [128, 8, 2], F32)
    P.dma(scT[:], cc.rearrange("(c p) j -> p c j", p=128), (), ["scT"])
    P.act(scT[:], scT[:], AF.Silu, ["scT"], ["scT"])
    rowg = P.sb("rowg", [1, 512], F32)
    browg = P.sb("browg", [1, 512], F32)
    growg = P.sb("growg", [1, 512], F32)
    scal = [[P.sb("scal%d_%d" % (k, j), [128, 8], F32) for j in range(2)] for k in range(2)]
    bias = [[P.sb("bias%d_%d" % (k, j), [128, 8], F32) for j in range(2)] for k in range(2)]
    gaB = [[P.sb("gaB%d_%d" % (k, j), [128, D], F32) for j in range(2)] for k in range(2)]
    one11 = one1[:, 0:1]
    ngs = (n1g, n2g)
    for g in range(12):
        v, hf = g // 2, g % 2
        k, kind = v // 3, v % 3
        P.dma(browg[:], bmod[g * 512:(g + 1) * 512].rearrange("(o n) -> o n", o=1), (), ["browg"])
        if kind == 1:
            P.dma(growg[:], ngs[k][hf * 512:(hf + 1) * 512].rearrange("(o n) -> o n", o=1), (), ["growg"])
        for j in range(2):
            for h in range(2):
                b_ = wsc[0] % 2
                wsc[0] += 1
                P.dma(wst[b_][:], wmod[512 * h:512 * h + 512, g * 512:(g + 1) * 512].rearrange("(c p) n -> p c n", p=128),
                      (), ["wstC%d" % b_])
                for c in range(4):
                    P.mm(pbk[0][0:1, :], scT[:, 4 * h + c, j:j + 1], wst[b_][:, c, :], h == 0 and c == 0, h == 1 and c == 3,
                         ["wstC%d" % b_, "scT"], ["pb0"])
            P.tt(rowg[:], pbk[0][0:1, :], browg[:], ALU.add, ["pb0", "browg"], ["rowg"])
            if kind == 1:
                P.stt(rowg[:], rowg[:], 1.0, growg[:], ALU.add, ALU.mult, ["rowg", "growg"], ["rowg"])
            if kind < 2:
                for c in range(4):
                    P.mm(pbk[1][:, c:c + 1], rowg[:, c * 128:(c + 1) * 128], one11, True, True, ["rowg", "one1"], ["pb1"])
                dstc = (bias if kind == 0 else scal)[k][j]
                P.cp(dstc[:, hf * 4:hf * 4 + 4], pbk[1][:, 0:4], ["pb1"], ["modcols"])
            else:
                P.mm(pbk[1][:, :], one1[:, :], rowg[:], True, True, ["rowg", "one1"], ["pb1"])
                P.cp(gaB[k][j][:, hf * 512:(hf + 1) * 512], pbk[1][:, :], ["pb1"], ["gaB"])
    fngB = P.sb("fngB", [128, D], F32)
    P.dma(fngB[:], fng.partition_broadcast(128), (), ["fngB"])

    xt = P.sb("xt", [128, D], F32)
    ss = [P.sb("ss%d" % i, [128, 1], F32) for i in range(2)]
    xh = [P.sb("xh0", [128, D], F32)] * 2
    hT = [P.sb("hT%d" % i, [128, 8, 128], BF16) for i in range(2)]
    ptr = [pbk[2].rearrange("p (c j) -> p c j", c=4), pbk[3].rearrange("p (c j) -> p c j", c=4)]
    G = P.sb("G", [128, D], F32)
    brt = [P.sb("brt%d" % i, [128, 512], BF16) for i in range(2)]
    brT = [P.sb("brT%d" % i, [128, 4, 128], BF16) for i in range(2)]
    m = P.sb("m", [128, D], F32)
    tmp = P.sb("tmp", [128, D], F32)
    sq = [tmp, tmp]
    mT = P.sb("mT", [128, 8, 128], BF16)
    x1 = P.sb("x1", [128, D], F32)
    Lr = P.sb("Lr", [128, 36], F32)
    Lm = P.sb("Lm", [128, 32], F32)
    k1 = P.sb("k1", [128, 32], F32)
    k2 = P.sb("k2", [128, 32], F32)
    Wt = P.sb("Wt", [128, 32], F32)
    r8 = P.sb("r8", [128, 8], F32)
    g4 = P.sb("g4", [128, 4], F32)
    pen = P.sb("pen", [128, 4], F32)
    mmc = [0]

    def bank():
        k = 4 + (mmc[0] % 3)
        mmc[0] += 1
        return pbk[k], "pb%d" % k

    def norm_T(i, xin, xtok, k, j, hTt, hTtok):
        P.act(sq[0][:], xin, AF.Square, [xtok], ["tmp", "Css"], accum=ss[0][:])
        P.act(ss[0][:], ss[0][:], AF.Sqrt, ["Css"], ["Css"], scale=1.0 / D, bias=EPS)
        P.add("vector", lambda e: e.reciprocal(out=ss[0][:], in_=ss[0][:]), ["Css"], ["Css"])
        P.ts(xh[0][:], xin, ss[0][:, 0:1], ALU.mult, [xtok, "Css"], ["Cxh"])
        for half in range(2):
            ptk = "pb%d" % (2 + half)
            for c4 in range(4):
                c = half * 4 + c4
                P.tr(ptr[half][:, c4, :], xh[0][:, c * 128:(c + 1) * 128], identf[:], ["Cxh", "identf"], [ptk])
            for c4 in range(4):
                c = half * 4 + c4
                P.act(hTt[:, c, :], ptr[half][:, c4, :], AF.Identity, [ptk, "modcols"], [hTtok],
                      scale=scal[k][j][:, c:c + 1], bias=bias[k][j][:, c:c + 1])

    for i in range(NT):
        j = 1 if i < CTXT else 0
        rs = slice(i * 128, (i + 1) * 128)
        P.dma(xt[:], x[rs, :], (), ["xt"])
        norm_T(i, xt[:], "xt", 0, j, hT[0], "hT0")
        for bi in range(3):
            bb = bi % 2
            P.dma(brt[bb][:], brd[bi][rs, :], (), ["brt%d" % bb])
            for c in range(4):
                P.tr(pbb[:, c, :], brt[bb][:, c * 128:(c + 1) * 128], identb[:], ["brt%d" % bb, "identb"], ["pbb"])
            P.cp(brT[bb][:], pbb[:, 0:4, :], ["pbb"], ["brT%d" % bb])
            for hf in range(2):
                pk, tk = bank()
                for c in range(8):
                    P.mm(pk[:], hT[0][:, c, :], Wgt[:, c, bi * 1024 + hf * 512:bi * 1024 + (hf + 1) * 512], c == 0, c == 7,
                         ["hT0", "Wgt"], [tk])
                P.act(G[:, hf * 512:(hf + 1) * 512], pk[:], AF.Sigmoid, [tk], ["G"])
            for hf in range(2):
                pk, tk = bank()
                for c in range(4):
                    P.mm(pk[:], brT[bb][:, c, :], Wbr[:, bi, c, hf * 512:(hf + 1) * 512], c == 0, c == 3,
                         ["brT%d" % bb, "Wbr"], [tk])
                hs_ = slice(hf * 512, (hf + 1) * 512)
                if bi == 0:
                    P.tt(m[:, hs_], pk[:], G[:, hs_], ALU.mult, [tk, "G"], ["m"])
                else:
                    P.tt(tmp[:, hs_], pk[:], G[:, hs_], ALU.mult, [tk, "G"], ["tmp"])
                    P.tt(m[:, hs_], m[:, hs_], tmp[:, hs_], ALU.add, ["m", "tmp"], ["m"], eng="gpsimd")
        for half in range(2):
            ptk = "pb%d" % (2 + half)
            for c4 in range(4):
                c = half * 4 + c4
                P.tr(ptr[half][:, c4, :], m[:, c * 128:(c + 1) * 128], identf[:], ["m", "identf"], [ptk])
            P.cp(mT[:, half * 4:half * 4 + 4, :], ptr[half][:, :, :], [ptk], ["mT"], eng="scalar")
        for hf in range(2):
            pk, tk = bank()
            hs_ = slice(hf * 512, (hf + 1) * 512)
            for c in range(8):
                P.mm(pk[:], mT[:, c, :], Wout[:, c, hs_], c == 0, c == 7, ["mT", "Wout"], [tk])
            P.tt(x1[:, hs_], pk[:], gaB[0][j][:, hs_], ALU.mult, [tk, "gaB"], ["x1"])
            P.tt(x1[:, hs_], x1[:, hs_], xt[:, hs_], ALU.add, ["x1", "xt"], ["x1"], eng="gpsimd")
        P.dma(X1[rs, :], x1[:], ["x1"], ["X1s"])
        norm_T(i, x1[:], "x1", 1, j, hT[1], "hT1")
        P.dma(H2T[:, :, rs], hT[1][:], ["hT1"], ["H2Ts"])
        pk, tk = bank()
        for c in range(8):
            P.mm(pk[:, 0:36], hT[1][:, c, :], wrb[:, c, :], c == 0, c == 7, ["hT1", "wrb"], [tk])
        P.cp(Lr[:], pk[:, 0:36], [tk], ["Lr"])
        R_ = ["Lr", "r8", "g4", "pen", "Lm", "k1", "k2", "Wt"]
        P.add("vector", lambda e: e.tensor_reduce(out=r8[:, 0:1], in_=Lr[:, 0:4], axis=AX.X, op=ALU.max), R_, R_)
        P.ts(g4[:], Lr[:, 0:4], r8[:, 0:1], ALU.is_ge, R_, R_)
        P.ts(r8[:, 1:2], r8[:, 0:1], -1.0, ALU.mult, R_, R_)
        P.act(pen[:], Lr[:, 0:4], AF.Exp, R_, R_, bias=r8[:, 1:2], accum=r8[:, 2:3])
        P.add("vector", lambda e: e.reciprocal(out=r8[:, 2:3], in_=r8[:, 2:3]), R_, R_)
        P.ts(pen[:], g4[:], -1.0, ALU.add, R_, R_, s2=1e30, op1=ALU.mult)
        P.tt(Lm[:].rearrange("p (g j) -> p g j", g=4), Lr[:, 4:36].rearrange("p (g j) -> p g j", g=4),
             pen[:, :].unsqueeze(2).to_broadcast([128, 4, 8]), ALU.add, R_, R_)
        P.add("vector", lambda e: e.tensor_reduce(out=r8[:, 3:4], in_=Lm[:], axis=AX.X, op=ALU.max), R_, R_)
        P.ts(k1[:], Lm[:], r8[:, 3:4], ALU.is_ge, R_, R_)
        P.stt(Lm[:], k1[:], -1e30, Lm[:], ALU.mult, ALU.add, R_, R_)
        P.add("vector", lambda e: e.tensor_reduce(out=r8[:, 4:5], in_=Lm[:], axis=AX.X, op=ALU.max), R_, R_)
        P.ts(k2[:], Lm[:], r8[:, 4:5], ALU.is_ge, R_, R_)
        P.tt(r8[:, 5:6], r8[:, 4:5], r8[:, 3:4], ALU.subtract, R_, R_)
        P.act(r8[:, 5:6], r8[:, 5:6], AF.Exp, R_, R_)
        P.ts(r8[:, 6:7], r8[:, 5:6], 1.0, ALU.add, R_, R_)
        P.add("vector", lambda e: e.reciprocal(out=r8[:, 6:7], in_=r8[:, 6:7]), R_, R_)
        P.tt(r8[:, 7:8], r8[:, 5:6], r8[:, 6:7], ALU.mult, R_, R_)
        P.tt(r8[:, 6:7], r8[:, 6:7], r8[:, 2:3], ALU.mult, R_, R_)
        P.tt(r8[:, 7:8], r8[:, 7:8], r8[:, 2:3], ALU.mult, R_, R_)
        P.ts(Wt[:], k1[:], r8[:, 6:7], ALU.mult, R_, R_)
        P.stt(Wt[:], k2[:], r8[:, 7:8], Wt[:], ALU.mult, ALU.add, R_, R_)
        P.dma(WR[rs, :], Wt[:], R_, ["WRs"])

    barrier(P)
    SBT = 8
    WE = [arena[:, p * 12288:(p + 1) * 12288].rearrange("p (m c n) -> p m c n", m=3, c=8) for p in range(2)]
    h2sb = arena[:, 24576:32768].rearrange("p (c n) -> p c n", c=8)
    AT = [arena[:, 32768 + q * 2048:32768 + (q + 1) * 2048].rearrange("p (c n) -> p c n", c=4) for q in range(2)]
    y = P.sb("y", [128, SBT, D], F32)
    Wsb = P.sb("Wsb", [128, SBT, 32], F32)
    sg = [P.sb("sg%d" % i, [128, 512], F32) for i in range(2)]
    x1r = [m, tmp]
    cntm = dict(at=0, sg=0)
    for s0 in range(0, NT, SBT):
        ns = min(SBT, NT - s0)
        ntk = ns * 128
        P.dma(h2sb[:, :, 0:ntk], H2T[:, :, s0 * 128:s0 * 128 + ntk], ["H2Ts"], ["h2sb"])
        P.dma(Wsb[:, 0:ns, :], WR[s0 * 128:s0 * 128 + ntk, :].rearrange("(t p) e -> p t e", p=128), ["WRs"], ["Wsb"])
        for e_ in range(NEXP):
            p = e_ % 2
            wtok = "WE%d" % p
            for mi, src in enumerate((wg, wu)):
                for h in range(2):
                    load_cast(WE[p][:, mi, 4 * h:4 * h + 4, :], src[e_, 512 * h:512 * h + 512, :].rearrange("(c p) n -> p c n", p=128), wtok)
            for h in range(2):
                load_cast(WE[p][:, 2, :, :].rearrange("p (fc hf) n -> p fc hf n", hf=2)[:, :, h, :],
                          wd[e_, :, h * 512:(h + 1) * 512].rearrange("(c p) n -> p c n", p=128), wtok)
            for g0 in range(0, ns, 4):
                ng = min(4, ns - g0)
                n = ng * 128
                a = cntm["at"] % 2
                cntm["at"] += 1
                for fc in range(4):
                    pg_, tg = bank()
                    for c in range(8):
                        P.mm(pg_[:, 0:n], WE[p][:, 0, c, fc * 128:(fc + 1) * 128], h2sb[:, c, g0 * 128:g0 * 128 + n], c == 0, c == 7,
                             [wtok, "h2sb"], [tg])
                    pu_, tu = bank()
                    for c in range(8):
                        P.mm(pu_[:, 0:n], WE[p][:, 1, c, fc * 128:(fc + 1) * 128], h2sb[:, c, g0 * 128:g0 * 128 + n], c == 0, c == 7,
                             [wtok, "h2sb"], [tu])
                    sb_ = cntm["sg"] % 2
                    cntm["sg"] += 1
                    P.act(sg[sb_][:, 0:n], pg_[:, 0:n], AF.Silu, [tg], ["sg%d" % sb_])
                    P.tt(AT[a][:, fc, 0:n], pu_[:, 0:n], sg[sb_][:, 0:n], ALU.mult, [tu, "sg%d" % sb_], ["AT%d" % a])
                for t in range(ng):
                    ti = g0 + t
                    for hf in range(2):
                        py_, ty = bank()
                        for fc in range(4):
                            P.mm(py_[:], AT[a][:, fc, t * 128:(t + 1) * 128], WE[p][:, 2, fc * 2 + hf, :], fc == 0, fc == 3,
                                 ["AT%d" % a, wtok], [ty])
                        yv = y[:, ti, hf * 512:(hf + 1) * 512]
                        if e_ == 0:
                            P.ts(yv, py_[:], Wsb[:, ti, e_:e_ + 1], ALU.mult, [ty, "Wsb"], ["y%d" % ti])
                        else:
                            P.stt(yv, py_[:], Wsb[:, ti, e_:e_ + 1], yv, ALU.mult, ALU.add, [ty, "Wsb", "y%d" % ti], ["y%d" % ti])
        for t in range(ns):
            i = s0 + t
            j = 1 if i < CTXT else 0
            rs = slice(i * 128, (i + 1) * 128)
            xb = x1r[t % 2]
            xtk = "x1r%d" % (t % 2)
            P.dma(xb[:], X1[rs, :], ["X1s"], [xtk])
            P.tt(y[:, t, :], y[:, t, :], gaB[1][j][:], ALU.mult, ["y%d" % t, "gaB"], ["y%d" % t], eng="gpsimd")
            P.tt(xb[:], xb[:], y[:, t, :], ALU.add, [xtk, "y%d" % t], [xtk])
            P.dma(Xn[rs, :], xb[:], [xtk], ["Xn"])
            P.act(G[:], xb[:], AF.Square, [xtk], ["G", "Css"], accum=ss[0][:])
            P.act(ss[0][:], ss[0][:], AF.Sqrt, ["Css"], ["Css"], scale=1.0 / D, bias=EPS)
            P.add("vector", lambda e: e.reciprocal(out=ss[0][:], in_=ss[0][:]), ["Css"], ["Css"])
            P.stt(G[:], xb[:], ss[0][:, 0:1], fngB[:], ALU.mult, ALU.mult, [xtk, "Css", "fngB"], ["G"])
            P.dma(Yn[rs, :], G[:], ["G"], ["Yn"])
        barrier(P)
    P.final_waits("sync")
    P.emit()
    return nc


_CACHE = {}


def _rope_tables(S):
    pos = np.arange(S)
    row = (pos // 64).astype(np.float32)
    col = (pos % 64).astype(np.float32)
    inv = (10000.0 ** (-np.arange(16, dtype=np.float32) / np.float32(16))).astype(np.float32)
    ang = np.concatenate([row[:, None] * inv, col[:, None] * inv], axis=-1).astype(np.float32)
    return np.cos(ang).astype(np.float32), np.sin(ang).astype(np.float32)


def _prog(key, fn):
    if key not in _CACHE:
        _CACHE[key] = fn()
    return _CACHE[key]


def kernel(x, c, ctx, c_ctx, w_mod, b_mod, norm1_g, norm2_g, w_in, hgrn_lb_logits, hgrn_out_norm_g,
           attn_q_norm_g, attn_k_norm_g, swa_sink, w_branch_a, w_branch_b, w_branch_c, w_out,
           w_group, w_router, w_exp_gate, w_exp_up, w_exp_down, final_norm_g):
    f32 = lambda a: np.ascontiguousarray(np.asarray(a), dtype=np.float32)
    x, c, ctx, c_ctx = f32(x), f32(c), f32(ctx), f32(c_ctx)
    Bn, S, _ = x.shape
    QN = 4
    Lc = S // QN
    NT = CTXT + Lc // 128
    NL = S // 128
    depth = w_in.shape[0]
    cosL, sinL = _rope_tables(S)
    cos_c = np.ones((256, 32), np.float32)
    sin_c = np.zeros((256, 32), np.float32)
    hc = hgrn_consts()
    i = np.arange(128)
    wm = np.stack([(i[:, None] >= i[None, :]), (i[:, None] <= i[None, :])]).astype(np.float32)
    xl, xc = x, ctx
    cores = [(b, j) for b in range(Bn) for j in range(QN)]
    yout = None
    for layer in range(depth):
        xin = [np.concatenate([xc[b], xl[b, j * Lc:(j + 1) * Lc]], 0) for (b, j) in cores]
        ccs = [np.ascontiguousarray(np.stack([c[b], c_ctx], 1)) for (b, j) in cores]
        pa = _prog(("PA", NT, layer), lambda: build_PA(NT, layer))
        wmodA = f32(w_mod[layer][:, :2048])
        winA = f32(w_in[layer][:, :4096])
        ims = []
        for k, (b, j) in enumerate(cores):
            ims.append(dict(x=xin[k], cc=ccs[k], wmod=wmodA, bmod=f32(b_mod[layer][:2048]), n1g=f32(norm1_g[layer]),
                            win=winA, lbl=f32(hgrn_lb_logits), aqg=f32(attn_q_norm_g[layer]), akg=f32(attn_k_norm_g[layer]),
                            cos=np.concatenate([cos_c, cosL[j * Lc:(j + 1) * Lc]], 0),
                            sin=np.concatenate([sin_c, sinL[j * Lc:(j + 1) * Lc]], 0)))
        ra = _run(pa, ims)
        full = {}
        for nm in ("QS", "KKf", "KKb", "LFf", "LFb", "VH", "OG", "QA", "QW", "KV4"):
            full[nm] = [np.concatenate([np.asarray(ra[b * QN][nm])[:256]] +
                                       [np.asarray(ra[b * QN + j][nm])[256:] for j in range(QN)], 0) for b in range(Bn)]
        del ra
        pb = _prog(("PB", NL), lambda: build_PB(NL))
        ims = []
        for b in range(Bn):
            for hp in range(4):
                hs = slice(hp * 128, (hp + 1) * 128)
                kv = hp // 2
                ksl = lambda o: slice(o + kv * 64, o + kv * 64 + 64)
                kv4 = full["KV4"][b]
                d = dict(qs=full["QS"][b][:, hs], kk0=full["KKf"][b][:, hs], kk1=full["KKb"][b][:, hs],
                         lf0=full["LFf"][b][:, hs], lf1=full["LFb"][b][:, hs], vh=full["VH"][b][:, hs],
                         og=full["OG"][b][:, hs], hg=f32(hgrn_out_norm_g[layer]),
                         qa=full["QA"][b][:, hs], ka=np.concatenate([kv4[:, ksl(0)], kv4[:, ksl(0)]], 1), va=kv4[:, ksl(256)],
                         qw=full["QW"][b][:, hs], kw=np.concatenate([kv4[:, ksl(128)], kv4[:, ksl(128)]], 1), vw=kv4[:, ksl(384)],
                         sink=f32(swa_sink[layer][2 * hp:2 * hp + 2]), wm=wm)
                for dd in range(2):
                    for nm in ("MC", "Mend", "Mdiag", "Moff"):
                        d["%s%d" % (nm, dd)] = hc[dd][nm]
                ims.append({k_: np.ascontiguousarray(v) for k_, v in d.items()})
        del full
        rb = _run(pb, ims)
        br = {}
        for nm in ("A", "B", "C"):
            br[nm] = [np.concatenate([np.asarray(rb[b * 4 + hp][nm]) for hp in range(4)], 1) for b in range(Bn)]
        del rb
        pc = _prog(("PC", NT), lambda: build_PC(NT))
        wmodC = f32(w_mod[layer])
        shared = dict(wmod=wmodC, bmod=f32(b_mod[layer]), n1g=f32(norm1_g[layer]), n2g=f32(norm2_g[layer]),
                      fng=f32(final_norm_g), wgt=f32(w_in[layer][:, 4096:7168]), wba=f32(w_branch_a[layer]),
                      wbb=f32(w_branch_b[layer]), wbc=f32(w_branch_c[layer]), wout=f32(w_out[layer]),
                      wr=f32(np.concatenate([np.asarray(w_group[layer]), np.asarray(w_router[layer])], 1)),
                      wg=f32(w_exp_gate[layer]), wu=f32(w_exp_up[layer]), wd=f32(w_exp_down[layer]))
        ims = []
        for k, (b, j) in enumerate(cores):
            d = dict(shared)
            d["x"] = xin[k]
            d["cc"] = ccs[k]
            for nm in ("A", "B", "C"):
                d[nm] = np.ascontiguousarray(np.concatenate([br[nm][b][:256], br[nm][b][256 + j * Lc:256 + (j + 1) * Lc]], 0))
            ims.append(d)
        rc = _run(pc, ims)
        xl = np.stack([np.concatenate([np.asarray(rc[b * QN + j]["Xn"])[256:] for j in range(QN)], 0) for b in range(Bn)])
        xc = np.stack([np.asarray(rc[b * QN]["Xn"])[:256] for b in range(Bn)])
        if layer == depth - 1:
            yout = np.stack([np.concatenate([np.asarray(rc[b * QN + j]["Yn"])[256:] for j in range(QN)], 0) for b in range(Bn)])
        del rc
    return yout.astype(np.float32)
```

```python
import numpy as np
import ml_dtypes
import concourse.bass as bass
import concourse.mybir as mybir
from concourse.bass_utils import run_bass_kernel_spmd

F32 = mybir.dt.float32
BF16 = mybir.dt.bfloat16
AF = mybir.ActivationFunctionType
ALU = mybir.AluOpType
AX = mybir.AxisListType
NPBF = ml_dtypes.bfloat16

ENGS = ["sync", "scalar", "vector", "gpsimd", "tensor"]
EPOCH = 12000
DMA_SLOTS = 6
DMA_EPOCH = 700

D = 1024
CTXT = 2
EPS = 1e-6
TINY = 1e-30


class Prog:
    def __init__(self, nc):
        self.nc = nc
        self.ops = {e: [] for e in ENGS}
        self.ccnt = {e: 0 for e in ENGS}
        self.dcnt = {e: 0 for e in ENGS}
        self.tok_w = {}
        self.tok_r = {}
        self.seen = {e: {} for e in ENGS}
        self.sems = {}
        self.nsem = 0
        self.uid = 0

    def _sem(self, key):
        if key not in self.sems:
            self.sems[key] = self.nc.alloc_semaphore(name="s%d" % self.nsem)
            self.nsem += 1
        return self.sems[key]

    def _ev_sem(self, ev):
        kind, eng, idx = ev
        if kind == "c":
            ep = (idx - 1) // EPOCH
            return ("c", eng, ep), idx - ep * EPOCH
        slot = idx % DMA_SLOTS
        use = idx // DMA_SLOTS
        ep = use // DMA_EPOCH
        return ("d", eng, slot, ep), 16 * (use - ep * DMA_EPOCH + 1)

    def add(self, eng, fn, reads=(), writes=(), dma=False):
        deps = set()
        for t in reads:
            w = self.tok_w.get(t)
            if w is not None:
                deps.add(w)
        for t in writes:
            w = self.tok_w.get(t)
            if w is not None:
                deps.add(w)
            for r in self.tok_r.get(t, ()):
                deps.add(r)
        if dma:
            i = self.dcnt[eng]
            self.dcnt[eng] += 1
            ev = ("d", eng, i)
            if i >= DMA_SLOTS:
                deps.add(("d", eng, i - DMA_SLOTS))
        else:
            self.ccnt[eng] += 1
            ev = ("c", eng, self.ccnt[eng])
        need = {}
        for d in deps:
            if d == ev:
                continue
            if d[0] == "c" and d[1] == eng == "tensor" and not dma:
                continue
            k, v = self._ev_sem(d)
            if self.seen[eng].get(k, 0) >= v:
                continue
            if need.get(k, 0) < v:
                need[k] = v
        for k, v in need.items():
            self.seen[eng][k] = v
        self.ops[eng].append((fn, sorted(need.items(), key=str), self._ev_sem(ev), dma))
        for t in reads:
            self.tok_r.setdefault(t, []).append(ev)
        for t in writes:
            self.tok_w[t] = ev
            self.tok_r[t] = []
        return ev

    def final_waits(self, eng="sync"):
        need = {}
        for e in ENGS:
            if self.ccnt[e] and e != eng:
                k, v = self._ev_sem(("c", e, self.ccnt[e]))
                need[k] = v
            n = self.dcnt[e]
            for i in range(max(0, n - DMA_SLOTS), n):
                k, v = self._ev_sem(("d", e, i))
                need[k] = max(need.get(k, 0), v)
        self.ops[eng].append((None, sorted(need.items(), key=str), None, False))

    def emit(self):
        for e in ENGS:
            for (fn, waits, inc, dma) in self.ops[e]:
                for k, v in waits:
                    self._sem(k)
                if inc is not None:
                    self._sem(inc[0])
        with self.nc.Block() as block:
            for e in ENGS:
                if not self.ops[e]:
                    continue

                def body(engh, e=e):
                    for (fn, waits, inc, dma) in self.ops[e]:
                        for k, v in waits:
                            engh.wait_ge(self.sems[k], v)
                        if fn is None:
                            continue
                        ins = fn(engh)
                        ins.then_inc(self.sems[inc[0]], 16 if dma else 1)

                getattr(block, e)(body)

    def sb(self, name, shape, dt):
        return self.nc.alloc_sbuf_tensor(name, list(shape), dt)

    def ps(self, name, shape, dt=F32):
        return self.nc.alloc_psum_tensor(name, list(shape), dt)

    def dma(self, out, in_, r, w, q=None):
        if q is None:
            q = "sync" if (self.dcnt["sync"] <= self.dcnt["gpsimd"]) else "gpsimd"
        self.add(q, lambda e: e.dma_start(out=out, in_=in_), r, w, dma=True)

    def act(self, out, in_, func, r, w, scale=1.0, bias=0.0, accum=None):
        if accum is None:
            self.add("scalar", lambda e: e.activation(out=out, in_=in_, func=func, scale=scale, bias=bias), r, w)
        else:
            self.add("scalar", lambda e: e.activation(out=out, in_=in_, func=func, scale=scale, bias=bias,
                                                      accum_out=accum), r, w)

    def tt(self, out, a, b, op, r, w, eng="vector"):
        self.add(eng, lambda e: e.tensor_tensor(out=out, in0=a, in1=b, op=op), r, w)

    def ts(self, out, a, s1, op0, r, w, s2=None, op1=None, eng="vector"):
        if op1 is None:
            self.add(eng, lambda e: e.tensor_scalar(out=out, in0=a, scalar1=s1, scalar2=None, op0=op0), r, w)
        else:
            self.add(eng, lambda e: e.tensor_scalar(out=out, in0=a, scalar1=s1, scalar2=s2, op0=op0, op1=op1), r, w)

    def stt(self, out, a, s, b, op0, op1, r, w):
        self.add("vector", lambda e: e.scalar_tensor_tensor(out=out, in0=a, scalar=s, in1=b, op0=op0, op1=op1), r, w)

    def cp(self, out, in_, r, w, eng="vector"):
        if eng == "scalar":
            self.add(eng, lambda e: e.activation(out=out, in_=in_, func=AF.Copy), r, w)
        else:
            self.add(eng, lambda e: e.tensor_copy(out=out, in_=in_), r, w)

    def mm(self, out, lhsT, rhs, start, stop, r, w):
        self.add("tensor", lambda e: e.matmul(out, lhsT=lhsT, rhs=rhs, start=start, stop=stop), r, w)

    def tr(self, out, in_, ident, r, w):
        self.add("tensor", lambda e: e.transpose(out=out, in_=in_, identity=ident), r, w)

    def ident(self, name, dt):
        t = self.sb(name, [128, 128], dt)
        self.add("gpsimd", lambda e: e.memset(t[:], 0.0), (), [name])
        self.add("gpsimd", lambda e: e.affine_select(out=t[:], in_=t[:], compare_op=ALU.not_equal, fill=1.0,
                                                     base=0, pattern=[[-1, 128]], channel_multiplier=1), [name], [name])
        return t


def _run(nc, in_maps):
    res = run_bass_kernel_spmd(nc, in_maps, core_ids=list(range(len(in_maps))))
    return res.results


def emit_mod(P, nc, cc, wmod, bmod, ncols, pfx, wst, wtoks):
    scT = P.sb(pfx + "scT", [128, 8, 2], F32)
    P.dma(scT[:], cc.rearrange("(c p) j -> p c j", p=128), (), ["scT"])
    P.act(scT[:], scT[:], AF.Silu, ["scT"], ["scT"])
    rows = P.sb(pfx + "rows", [1, 2, ncols], F32)
    brow = P.sb(pfx + "brow", [1, ncols], F32)
    P.dma(brow[:], bmod.rearrange("(o n) -> o n", o=1), (), ["brow"])
    pm = P.ps(pfx + "pm", [1, 2, 512])
    for g in range(ncols // 512):
        w = wst[g % 2]
        wt = wtoks[g % 2]
        P.dma(w[:], wmod[:, g * 512:(g + 1) * 512].rearrange("(c p) n -> p c n", p=128), (), [wt])
        for j in range(2):
            for c in range(8):
                P.mm(pm[:, j, :], scT[:, c, j:j + 1], w[:, c, :], c == 0, c == 7, [wt, "scT"], ["pm"])
        for j in range(2):
            P.tt(rows[:, j, g * 512:(g + 1) * 512], pm[:, j, :], brow[:, g * 512:(g + 1) * 512], ALU.add,
                 ["pm", "brow"], ["modrow"])
    return rows


def row_to_cols(P, nc, row_ap, n, cols_out, one11, pcol, r, w):
    k = n // 128
    for c in range(k):
        P.mm(pcol[:, c:c + 1], row_ap[:, c * 128:(c + 1) * 128], one11, True, True, r, ["pcol"])
    P.cp(cols_out, pcol[:, 0:k], ["pcol"], w)


def row_bcast(P, nc, row_ap, n, out_tile, ones1, pb, r, w):
    for g in range(0, n, 512):
        m = min(512, n - g)
        P.mm(pb[:, 0:m], ones1, row_ap[:, g:g + m], True, True, r, ["pb"])
        P.cp(out_tile[:, g:g + m], pb[:, 0:m], ["pb"], w)


def emit_norm_T(P, nc, xt, xtok, i, scal, bias, ident, hT, hTtok, pfx, ptr, sq, ss, xh):
    b = i % 2
    P.act(sq[b][:], xt, AF.Square, [xtok], [pfx + "sq%d" % b, pfx + "ss%d" % b], accum=ss[b][:])
    P.act(ss[b][:], ss[b][:], AF.Sqrt, [pfx + "ss%d" % b], [pfx + "ss%d" % b], scale=1.0 / D, bias=EPS)
    P.add("vector", lambda e: e.reciprocal(out=ss[b][:], in_=ss[b][:]), [pfx + "ss%d" % b], [pfx + "ss%d" % b])
    P.ts(xh[b][:], xt, ss[b][:, 0:1], ALU.mult, [xtok, pfx + "ss%d" % b], [pfx + "xh%d" % b])
    for half in range(2):
        pt = ptr[half]
        ptk = pfx + "ptr%d" % half
        for c4 in range(4):
            c = half * 4 + c4
            P.tr(pt[:, c4, :], xh[b][:, c * 128:(c + 1) * 128], ident[:], [pfx + "xh%d" % b, "identf"], [ptk])
        for c4 in range(4):
            c = half * 4 + c4
            P.act(hT[:, c, :], pt[:, c4, :], AF.Identity, [ptk, "modcols"], [hTtok],
                  scale=scal[:, c:c + 1], bias=bias[:, c:c + 1])


def build_PA(NT, layer):
    NTOK = NT * 128
    nc = bass.Bass("TRN2", target_bir_lowering=False)
    dt_in = lambda n, s, d=F32: nc.dram_tensor(n, list(s), d, kind="ExternalInput").ap()
    dt_out = lambda n, s, d: nc.dram_tensor(n, list(s), d, kind="ExternalOutput").ap()
    x = dt_in("x", [NTOK, D])
    cc = dt_in("cc", [D, 2])
    wmod = dt_in("wmod", [D, 2048])
    bmod = dt_in("bmod", [2048])
    n1g = dt_in("n1g", [D])
    win = dt_in("win", [D, 4096])
    lbl = dt_in("lbl", [2, 512])
    aqg = dt_in("aqg", [64])
    akg = dt_in("akg", [64])
    cosd = dt_in("cos", [NTOK, 32])
    sind = dt_in("sin", [NTOK, 32])
    QS = dt_out("QS", [NTOK, 512], BF16)
    KKf = dt_out("KKf", [NTOK, 512], BF16)
    KKb = dt_out("KKb", [NTOK, 512], BF16)
    LFf = dt_out("LFf", [NTOK, 512], F32)
    LFb = dt_out("LFb", [NTOK, 512], F32)
    VH = dt_out("VH", [NTOK, 512], BF16)
    OG = dt_out("OG", [NTOK, 512], F32)
    QA = dt_out("QA", [NTOK, 512], BF16)
    QW = dt_out("QW", [NTOK, 512], BF16)
    KV4 = dt_out("KV4", [NTOK, 512], BF16)

    P = Prog(nc)
    identf = P.ident("identf", F32)
    one1 = P.sb("one1", [1, 128], F32)
    P.add("vector", lambda e: e.memset(one1[:], 1.0), (), ["one1"])
    Wb = P.sb("Wb", [128, 8, 4096], BF16)
    wst = [P.sb("wstA%d" % i, [128, 8, 512], F32) for i in range(2)]
    src_groups = [(0, 512), (512, 512), (1024, 512), (1536, 512), (2048, 512), (2560, 512), (3328, 512),
                  (3072, 128), (3840, 128), (3200, 128), (3968, 128)]
    dst = 0
    for gi, (s0, n) in enumerate(src_groups):
        P.dma(Wb[:, :, dst:dst + n], win[:, s0:s0 + n].rearrange("(c p) n -> p c n", p=128), (), ["Wb"], q="gpsimd")
        dst += n
    rows = emit_mod(P, nc, cc, wmod, bmod, 2048, "A", wst, ["wstA0", "wstA1"])
    g1row = P.sb("g1row", [1, D], F32)
    P.dma(g1row[:], n1g.rearrange("(o n) -> o n", o=1), (), ["g1row"])
    scrow = P.sb("scrow", [1, 2, D], F32)
    for j in range(2):
        P.stt(scrow[:, j, :], rows[:, j, 1024:2048], 1.0, g1row[:], ALU.add, ALU.mult, ["modrow", "g1row"], ["scrow"])
    pcol = P.ps("pcol", [128, 16])
    one11 = one1[:, 0:1]
    scal = [P.sb("scal%d" % j, [128, 8], F32) for j in range(2)]
    bias = [P.sb("bias%d" % j, [128, 8], F32) for j in range(2)]
    for j in range(2):
        row_to_cols(P, nc, scrow[:, j, :], D, scal[j][:], one11, pcol, ["scrow", "one1"], ["modcols"])
        row_to_cols(P, nc, rows[:, j, 0:1024], D, bias[j][:], one11, pcol, ["modrow", "one1"], ["modcols"])
    lbB = P.sb("lbB", [128, 512], F32)
    omlbB = P.sb("omlbB", [128, 512], F32)
    e0 = P.sb("e0", [128, 512], F32)
    e1 = P.sb("e1", [128, 512], F32)
    P.dma(e0[:], lbl[0, :].partition_broadcast(128), (), ["e0"])
    P.dma(e1[:], lbl[1, :].partition_broadcast(128), (), ["e1"])
    P.act(e0[:], e0[:], AF.Exp, ["e0"], ["e0"])
    P.act(e1[:], e1[:], AF.Exp, ["e1"], ["e1"])
    P.tt(lbB[:], e0[:], e1[:], ALU.add, ["e0", "e1"], ["lbB"])
    P.add("vector", lambda e: e.reciprocal(out=lbB[:], in_=lbB[:]), ["lbB"], ["lbB"])
    P.tt(e0[:], e0[:], lbB[:], ALU.mult, ["e0", "lbB"], ["e0"])
    P.tt(e1[:], e1[:], lbB[:], ALU.mult, ["e1", "lbB"], ["e1"])
    if layer == 0:
        P.tt(lbB[:], e0[:], e0[:], ALU.subtract, ["e0"], ["lbB"])
    else:
        P.tt(lbB[:], e0[:], e1[:], ALU.add, ["e0", "e1"], ["lbB"])
        P.tt(lbB[:], lbB[:], e0[:], ALU.subtract, ["lbB", "e0"], ["lbB"])
    P.ts(omlbB[:], lbB[:], -1.0, ALU.mult, ["lbB"], ["omlbB"], s2=1.0, op1=ALU.add)
    gq = P.sb("gq", [128, 64], F32)
    gk = P.sb("gk", [128, 64], F32)
    P.dma(gq[:], aqg.partition_broadcast(128), (), ["gq"])
    P.dma(gk[:], akg.partition_broadcast(128), (), ["gk"])

    xt = [P.sb("xt%d" % i, [128, D], F32) for i in range(2)]
    sq = [P.sb("sq%d" % i, [128, D], F32) for i in range(2)]
    ss = [P.sb("ss%d" % i, [128, 1], F32) for i in range(2)]
    xh = [P.sb("xh%d" % i, [128, D], F32) for i in range(2)]
    hT = [P.sb("hT%d" % i, [128, 8, 128], BF16) for i in range(2)]
    cs = [P.sb("cs%d" % i, [128, 2, 32], F32) for i in range(2)]
    ptr = [P.ps("ptr%d" % i, [128, 4, 128]) for i in range(2)]
    pg = [P.ps("pg%d" % i, [128, 512]) for i in range(3)]
    NW = 6
    wk = [[P.sb("wk%d_%d" % (k, i), [128, 512], F32) for i in range(2)] for k in range(NW)]
    ob = [[P.sb("ob%d_%d" % (k, i), [128, 512], BF16) for i in range(2)] for k in range(3)]
    sm = [P.sb("sm%d" % i, [128, 8], F32) for i in range(2)]
    gcount = [0]

    def proj(i, g):
        k = gcount[0] % 3
        gcount[0] += 1
        for c in range(8):
            P.mm(pg[k][:], hT[i % 2][:, c, :], Wb[:, c, g * 512:(g + 1) * 512], c == 0, c == 7,
                 ["hT%d" % (i % 2), "Wb"], ["pg%d" % k])
        return pg[k], "pg%d" % k

    def rope(src, stok, dst_, dtok, H, b, eng="gpsimd"):
        sv = src.rearrange("p (h two d) -> p h two d", h=H, two=2)
        dv = dst_.rearrange("p (h two d) -> p h two d", h=H, two=2)
        cB = cs[b][:, 0, :].unsqueeze(1).to_broadcast([128, H, 32])
        sB = cs[b][:, 1, :].unsqueeze(1).to_broadcast([128, H, 32])
        t1 = wk[4][b][:, 0:H * 32].rearrange("p (h d) -> p h d", h=H)
        t2 = wk[5][b][:, 0:H * 32].rearrange("p (h d) -> p h d", h=H)
        a, t = "wk4_%d" % b, "wk5_%d" % b
        ctk = "cs%d" % b
        P.tt(t1, sv[:, :, 0, :], cB, ALU.mult, [stok, ctk], [a], eng=eng)
        P.tt(t2, sv[:, :, 1, :], sB, ALU.mult, [stok, ctk], [t], eng=eng)
        P.tt(dv[:, :, 0, :], t1, t2, ALU.subtract, [a, t], [dtok], eng=eng)
        P.tt(t1, sv[:, :, 1, :], cB, ALU.mult, [stok, ctk], [a], eng=eng)
        P.tt(t2, sv[:, :, 0, :], sB, ALU.mult, [stok, ctk], [t], eng=eng)
        P.tt(dv[:, :, 1, :], t1, t2, ALU.add, [a, t], [dtok], eng=eng)

    def qknorm(src, stok, H, gB, gtok, b, outw, otok):
        v = src.rearrange("p (h d) -> p h d", h=H)
        tmp = wk[3][b][:, 0:H * 64]
        P.tt(tmp, src, src, ALU.mult, [stok], ["wk3_%d" % b])
        P.add("vector", lambda e: e.tensor_reduce(out=sm[b][:, 0:H], in_=tmp.rearrange("p (h d) -> p h d", h=H),
                                                  axis=AX.X, op=ALU.add), ["wk3_%d" % b], ["sm%d" % b])
        P.act(sm[b][:, 0:H], sm[b][:, 0:H], AF.Sqrt, ["sm%d" % b], ["sm%d" % b], scale=1.0 / 64, bias=EPS)
        P.add("vector", lambda e: e.reciprocal(out=sm[b][:, 0:H], in_=sm[b][:, 0:H]), ["sm%d" % b], ["sm%d" % b])
        ov = outw.rearrange("p (h d) -> p h d", h=H)
        P.tt(ov, v, sm[b][:, 0:H].unsqueeze(2).to_broadcast([128, H, 64]), ALU.mult, [stok, "sm%d" % b], [otok])
        P.tt(ov, ov, gB[:, :].unsqueeze(1).to_broadcast([128, H, 64]), ALU.mult, [otok, gtok], [otok])

    for i in range(NT):
        b = i % 2
        j = 1 if i < CTXT else 0
        rs = slice(i * 128, (i + 1) * 128)
        P.dma(xt[b][:], x[rs, :], (), ["xt%d" % b])
        P.dma(cs[b][:, 0, :], cosd[rs, :], (), ["cs%d" % b])
        P.dma(cs[b][:, 1, :], sind[rs, :], (), ["cs%d" % b])
        emit_norm_T(P, nc, xt[b][:], "xt%d" % b, i, scal[j], bias[j], identf, hT[b], "hT%d" % b, "A", ptr, sq, ss, xh)
        pgt, pk = proj(i, 0)
        P.act(wk[0][b][:], pgt[:], AF.Silu, [pk], ["wk0_%d" % b])
        P.ts(ob[0][b][:], wk[0][b][:], 128.0 ** -0.5, ALU.mult, ["wk0_%d" % b], ["ob0_%d" % b], eng="gpsimd")
        P.dma(QS[rs, :], ob[0][b][:], ["ob0_%d" % b], ["QS"])
        for (g, KKo, LFo) in ((1, KKf, LFf), (2, KKb, LFb)):
            pgt, pk = proj(i, g)
            P.act(wk[0][b][:], pgt[:], AF.Sigmoid, [pk], ["wk0_%d" % b])
            P.tt(wk[1][b][:], wk[0][b][:], omlbB[:], ALU.mult, ["wk0_%d" % b, "omlbB"], ["wk1_%d" % b])
            P.tt(ob[1][b][:], omlbB[:], wk[1][b][:], ALU.subtract, ["wk1_%d" % b, "omlbB"], ["ob1_%d" % b], eng="gpsimd")
            P.stt(wk[2][b][:], wk[1][b][:], TINY, lbB[:], ALU.max, ALU.add, ["wk1_%d" % b, "lbB"], ["wk2_%d" % b])
            P.act(wk[2][b][:], wk[2][b][:], AF.Ln, ["wk2_%d" % b], ["wk2_%d" % b])
            P.dma(KKo[rs, :], ob[1][b][:], ["ob1_%d" % b], ["KK"])
            P.dma(LFo[rs, :], wk[2][b][:], ["wk2_%d" % b], ["LF"])
        pgt, pk = proj(i, 3)
        P.cp(ob[2][b][:], pgt[:], [pk], ["ob2_%d" % b])
        P.dma(VH[rs, :], ob[2][b][:], ["ob2_%d" % b], ["VH"])
        pgt, pk = proj(i, 4)
        P.act(wk[0][b][:], pgt[:], AF.Silu, [pk], ["wk0_%d" % b])
        P.dma(OG[rs, :], wk[0][b][:], ["wk0_%d" % b], ["OG"])
        pgt, pk = proj(i, 5)
        P.act(wk[0][b][:], pgt[:], AF.Copy, [pk], ["wk0_%d" % b])
        qknorm(wk[0][b][:], "wk0_%d" % b, 8, gq, "gq", b, wk[1][b][:], "wk1_%d" % b)
        rope(wk[1][b][:], "wk1_%d" % b, ob[0][b][:], "ob0_%d" % b, 8, b)
        P.dma(QA[rs, :], ob[0][b][:], ["ob0_%d" % b], ["QA"])
        pgt, pk = proj(i, 6)
        P.act(wk[0][b][:], pgt[:], AF.Copy, [pk], ["wk0_%d" % b])
        rope(wk[0][b][:], "wk0_%d" % b, ob[1][b][:], "ob1_%d" % b, 8, b)
        P.dma(QW[rs, :], ob[1][b][:], ["ob1_%d" % b], ["QW"])
        pgt, pk = proj(i, 7)
        P.act(wk[0][b][:], pgt[:], AF.Copy, [pk], ["wk0_%d" % b])
        qknorm(wk[0][b][:, 0:128], "wk0_%d" % b, 2, gk, "gk", b, wk[0][b][:, 0:128], "wk0_%d" % b)
        rope(wk[0][b][:, 0:256], "wk0_%d" % b, ob[2][b][:, 0:256], "ob2_%d" % b, 4, b)
        P.cp(ob[2][b][:, 256:512], wk[0][b][:, 256:512], ["wk0_%d" % b], ["ob2_%d" % b], eng="gpsimd")
        P.dma(KV4[rs, :], ob[2][b][:], ["ob2_%d" % b], ["KV4"])
    P.final_waits("sync")
    P.emit()
    return nc


def hgrn_consts():
    i = np.arange(128)
    ch = i // 64
    blk = i // 32
    out = []
    u = i[:, None]
    t = i[None, :]
    same_ch = (ch[:, None] == ch[None, :])
    r = (blk * 32 + 16)[None, :]
    Mdq = (((u > r) & (u <= t)).astype(np.float32) - ((u > t) & (u <= r)).astype(np.float32))
    Mcq = (same_ch & (u <= t)).astype(np.float32)
    cs = (ch * 64)[None, :]
    Moq_full = ((u >= cs + 32) & (u <= t) & same_ch).astype(np.float32)
    Mok_full = ((u > t) & (u <= cs + 31) & same_ch).astype(np.float32)
    second = np.concatenate([np.arange(32, 64), np.arange(96, 128)])
    first = np.concatenate([np.arange(0, 32), np.arange(64, 96)])
    Mend = (same_ch & (u > t)).astype(np.float32)
    Mdiag = ((blk[:, None] == blk[None, :]) & (u <= t)).astype(np.float32)
    Moff = (same_ch & ((i % 64) < 32)[:, None] & ((i % 64) >= 32)[None, :]).astype(np.float32)
    MCf = np.concatenate([Mdq, Mcq, -Mdq, Moq_full[:, second], Mok_full[:, first]], 1)
    out.append(dict(MC=MCf, Mend=Mend, Mdiag=Mdiag, Moff=Moff))
    fl = lambda M: M[::-1, ::-1].copy()
    Mdq_b, Mcq_b = fl(Mdq), fl(Mcq)
    Moq_b, Mok_b = fl(Moq_full), fl(Mok_full)
    MCb = np.concatenate([Mdq_b, Mcq_b, -Mdq_b, Moq_b[:, first], Mok_b[:, second]], 1)
    out.append(dict(MC=MCb, Mend=fl(Mend), Mdiag=fl(Mdiag), Moff=fl(Moff)))
    return out


def build_PB(NL, parts=(1, 1, 1)):
    NTB = CTXT + NL
    NTOK = NTB * 128
    nc = bass.Bass("TRN2", target_bir_lowering=False)
    dt_in = lambda n, s, d=F32: nc.dram_tensor(n, list(s), d, kind="ExternalInput").ap()
    dt_out = lambda n, s, d: nc.dram_tensor(n, list(s), d, kind="ExternalOutput").ap()
    qs = dt_in("qs", [NTOK, 128], BF16)
    kk = [dt_in("kk%d" % d, [NTOK, 128], BF16) for d in range(2)]
    lf = [dt_in("lf%d" % d, [NTOK, 128]) for d in range(2)]
    vh = dt_in("vh", [NTOK, 128], BF16)
    og = dt_in("og", [NTOK, 128])
    hg = dt_in("hg", [128])
    MCd = [dt_in("MC%d" % d, [128, 512]) for d in range(2)]
    Mendd = [dt_in("Mend%d" % d, [128, 128]) for d in range(2)]
    Mdiagd = [dt_in("Mdiag%d" % d, [128, 128]) for d in range(2)]
    Moffd = [dt_in("Moff%d" % d, [128, 128]) for d in range(2)]
    qa = dt_in("qa", [NTOK, 128], BF16)
    ka = dt_in("ka", [NTOK, 128], BF16)
    va = dt_in("va", [NTOK, 64], BF16)
    qw = dt_in("qw", [NTOK, 128], BF16)
    kw = dt_in("kw", [NTOK, 128], BF16)
    vw = dt_in("vw", [NTOK, 64], BF16)
    sink = dt_in("sink", [2])
    wm = dt_in("wm", [2, 128, 128])
    Ao = dt_out("A", [NTOK, 128], BF16)
    Bo = dt_out("B", [NTOK, 128], BF16)
    Co = dt_out("C", [NTOK, 128], BF16)

    P = Prog(nc)
    identf = P.ident("identf", F32)
    identb = P.sb("identb", [128, 128], BF16)
    P.cp(identb[:], identf[:], ["identf"], ["identb"])

    MC = [P.sb("MCs%d" % d, [128, 512], F32) for d in range(2)]
    Mend = [P.sb("Mends%d" % d, [128, 128], F32) for d in range(2)]
    Mdiag = [P.sb("Mdiags%d" % d, [128, 128], F32) for d in range(2)]
    Moff = [P.sb("Moffs%d" % d, [128, 128], F32) for d in range(2)]
    for d in range(2):
        P.dma(MC[d][:], MCd[d][:, :], (), ["consts"])
        P.dma(Mend[d][:], Mendd[d][:, :], (), ["consts"])
        P.dma(Mdiag[d][:], Mdiagd[d][:, :], (), ["consts"])
        P.dma(Moff[d][:], Moffd[d][:, :], (), ["consts"])
    hgB = P.sb("hgB", [128, 128], F32)
    P.dma(hgB[:], hg.partition_broadcast(128), (), ["consts"])
    Oacc = P.sb("Oacc", [128, NTB, 128], F32)
    Sm = P.sb("Sm", [128, 128], F32)
    Sb = [P.sb("Sb%d" % i, [128, 128], BF16) for i in range(2)]
    lft = [P.sb("lft%d" % i, [128, 128], F32) for i in range(2)]
    kkt = [P.sb("kkt%d" % i, [128, 128], BF16) for i in range(2)]
    qst = [P.sb("qst%d" % i, [128, 128], BF16) for i in range(2)]
    vt = [P.sb("vt%d" % i, [128, 128], BF16) for i in range(2)]
    ogt = [P.sb("ogt%d" % i, [128, 128], F32) for i in range(2)]
    qkT = [P.sb("qkT%d" % i, [128, 2, 128], BF16) for i in range(2)]
    E = [P.sb("E%d" % i, [128, 512], F32) for i in range(2)]
    E2 = [P.sb("E2%d" % i, [128, 128], F32) for i in range(2)]
    Kt = [P.sb("Kt%d" % i, [128, 128], BF16) for i in range(2)]
    QC = [P.sb("QC%d" % i, [128, 6, 128], BF16) for i in range(2)]
    Pm = [P.sb("Pm%d" % i, [128, 2, 128], BF16) for i in range(2)]
    tot = [P.sb("tot%d" % i, [128, 128], F32) for i in range(2)]
    Usb = [P.sb("Usb%d" % i, [128, 2, 128], F32) for i in range(2)]
    hs = [P.sb("hs%d" % i, [128, 2], F32) for i in range(2)]
    ao = [P.sb("ao%d" % i, [128, 128], BF16) for i in range(2)]
    for i in range(2):
        P.add("gpsimd", lambda e, i=i: e.memset(QC[i][:], 0.0), (), ["QC%d" % i])
    pbb = P.ps("pbb", [128, 8, 128], BF16)
    pbk = [P.ps("pbk%d" % i, [128, 512]) for i in range(6)]
    p_tr = pbb[:, 0:2, :]
    p_ex = pbk[0]
    p_e2 = pbk[1][:, 0:128]
    p_sc = pbk[2][:, 0:256].rearrange("p (c j) -> p c j", c=2)
    p_u = [pbk[3][:, 0:128], pbk[5][:, 0:128]]
    putok = ["p_u", "p_u1"]
    p_o = pbk[4][:, 0:128]
    P.add("vector", lambda e: e.memset(Sm[:], 0.0), (), ["Sm"])

    cnt = [0]

    def hgrn_A(ti, d, b):
        B = str(b)
        rs = slice(ti * 128, (ti + 1) * 128)
        P.dma(lft[b][:], lf[d][rs, :], (), ["lft" + B])
        P.dma(kkt[b][:], kk[d][rs, :], (), ["kkt" + B])
        P.dma(qst[b][:], qs[rs, :], (), ["qst" + B])
        P.dma(vt[b][:], vh[rs, :], (), ["vt" + B])
        P.tr(p_tr[:, 0, :], qst[b][:], identb[:], ["qst" + B, "identb"], ["p_tr"])
        P.tr(p_tr[:, 1, :], kkt[b][:], identb[:], ["kkt" + B, "identb"], ["p_tr"])
        P.cp(qkT[b][:], p_tr, ["p_tr"], ["qkT" + B])
        P.mm(p_ex[:], lft[b][:], MC[d][:], True, True, ["lft" + B, "consts"], ["p_ex"])
        P.act(E[b][:], p_ex[:], AF.Exp, ["p_ex"], ["E" + B])
        qT = qkT[b][:, 0, :]
        kT = qkT[b][:, 1, :]
        r = ["qkT" + B, "E" + B]
        w = ["QC" + B]
        P.tt(QC[b][:, 0, :], qT, E[b][:, 0:128], ALU.mult, r, w)
        P.tt(QC[b][:, 1, 0:64], qT[:, 0:64], E[b][:, 128:192], ALU.mult, r, w)
        P.tt(QC[b][:, 2, 64:128], qT[:, 64:128], E[b][:, 192:256], ALU.mult, r, w)
        P.tt(QC[b][:, 3, :], kT, E[b][:, 256:384], ALU.mult, r, w, eng="gpsimd")
        qa_sl, ka_sl = (slice(32, 64), slice(0, 32)) if d == 0 else (slice(0, 32), slice(32, 64))
        v3 = lambda ap: ap.rearrange("p (c j) -> p c j", c=2)
        P.tt(v3(QC[b][:, 4, :])[:, :, qa_sl], v3(qT)[:, :, qa_sl], E[b][:, 384:448].rearrange("p (c j) -> p c j", c=2),
             ALU.mult, r, w, eng="gpsimd")
        P.tt(v3(QC[b][:, 5, :])[:, :, ka_sl], v3(kT)[:, :, ka_sl], E[b][:, 448:512].rearrange("p (c j) -> p c j", c=2),
             ALU.mult, r, w, eng="gpsimd")
        P.mm(p_e2, Mend[d][:], lft[b][:], True, True, ["lft" + B, "consts"], ["p_e2"])
        P.act(E2[b][:], p_e2, AF.Exp, ["p_e2"], ["E2" + B])
        P.tt(Kt[b][:], kkt[b][:], E2[b][:], ALU.mult, ["kkt" + B, "E2" + B], ["Kt" + B])
        P.mm(p_sc[:, 0, :], QC[b][:, 3, :], QC[b][:, 0, :], True, True, ["QC" + B], ["p_sc"])
        P.mm(p_sc[:, 1, :], QC[b][:, 5, :], QC[b][:, 4, :], True, True, ["QC" + B], ["p_sc"])
        P.tt(Pm[b][:, 0, :], p_sc[:, 0, :], Mdiag[d][:], ALU.mult, ["p_sc", "consts"], ["Pm" + B])
        P.tt(Pm[b][:, 1, :], p_sc[:, 1, :], Moff[d][:], ALU.mult, ["p_sc", "consts"], ["Pm" + B])
        for c in range(2):
            P.mm(p_u[c], Kt[b][64 * c:64 * c + 64, :], vt[b][64 * c:64 * c + 64, :], True, True,
                 ["Kt" + B, "vt" + B], [putok[c]])
        P.cp(Usb[b][:, 0, :], p_u[0], [putok[0]], ["Usb" + B])
        P.cp(Usb[b][:, 1, :], p_u[1], [putok[1]], ["Usb" + B])

    def hgrn_B(ti, d, b, reset):
        B = str(b)
        rs = slice(ti * 128, (ti + 1) * 128)
        if reset:
            P.add("vector", lambda e: e.memset(Sm[:], 0.0), (), ["Sm"])
        order = (0, 1) if d == 0 else (1, 0)
        for c in order:
            dcol = 128 + 64 * c + (63 if d == 0 else 0)
            P.cp(Sb[c][:], Sm[:], ["Sm"], ["Sb%d" % c], eng="scalar")
            P.stt(Sm[:], Sm[:], E[b][:, dcol:dcol + 1], Usb[b][:, c, :], ALU.mult, ALU.add, ["Sm", "E" + B, "Usb" + B], ["Sm"])
        P.mm(p_o, Pm[b][:, 0, :], vt[b][:], True, False, ["Pm" + B, "vt" + B], ["p_o"])
        P.mm(p_o, Pm[b][:, 1, :], vt[b][:], False, False, ["Pm" + B, "vt" + B], ["p_o"])
        P.mm(p_o, QC[b][:, 1, :], Sb[0][:], False, False, ["QC" + B, "Sb0"], ["p_o"])
        P.mm(p_o, QC[b][:, 2, :], Sb[1][:], False, True, ["QC" + B, "Sb1"], ["p_o"])
        if d == 0:
            P.cp(Oacc[:, ti, :], p_o, ["p_o"], ["Oacc%d" % ti])
        else:
            P.dma(ogt[b][:], og[rs, :], (), ["ogt" + B])
            P.tt(tot[b][:], p_o, Oacc[:, ti, :], ALU.add, ["p_o", "Oacc%d" % ti], ["tot" + B])
            P.act(E2[b][:], tot[b][:], AF.Square, ["tot" + B], ["E2" + B, "hs" + B], accum=hs[b][:, 0:1])
            P.act(hs[b][:, 0:1], hs[b][:, 0:1], AF.Sqrt, ["hs" + B], ["hs" + B], scale=1.0 / 128, bias=EPS)
            P.add("vector", lambda e: e.reciprocal(out=hs[b][:, 0:1], in_=hs[b][:, 0:1]), ["hs" + B], ["hs" + B])
            P.stt(tot[b][:], tot[b][:], hs[b][:, 0:1], hgB[:], ALU.mult, ALU.mult, ["tot" + B, "hs" + B, "consts"], ["tot" + B])
            P.tt(ao[b][:], tot[b][:], ogt[b][:], ALU.mult, ["tot" + B, "ogt" + B], ["ao" + B])
            P.dma(Ao[rs, :], ao[b][:], ["ao" + B], ["Ao"])

    fwd_order = list(range(NTB))
    bwd_order = [1, 0] + list(range(NTB - 1, CTXT - 1, -1))
    if parts[0]:
        for d, order in ((0, fwd_order), (1, bwd_order)):
            hgrn_A(order[0], d, cnt[0] % 2)
            for n, ti in enumerate(order):
                b = cnt[0] % 2
                cnt[0] += 1
                if n + 1 < len(order):
                    hgrn_A(order[n + 1], d, 1 - b)
                hgrn_B(ti, d, b, n == 0)

    QT2 = P.sb("QT2", [128, NTOK], BF16)
    KT2 = P.sb("KT2", [128, NTOK], BF16)
    Vx = P.sb("Vx", [128, NTB, 72], BF16)
    ld = [P.sb("ld%d" % i, [128, 8, 128], BF16) for i in range(2)]
    PT = [P.sb("PT%d" % i, [128, 512], BF16) for i in range(3)]
    OT = [P.sb("OT%d" % i, [65, 512], F32) for i in range(2)]
    bo = [P.sb("bo%d" % i, [128, 4, 128], BF16) for i in range(2)]
    rec = [P.sb("rec%d" % i, [128, 1], F32) for i in range(2)]
    wmt = P.sb("wmt", [128, 2, 128], F32)
    P.dma(wmt[:], wm.rearrange("m k q -> k m q"), (), ["consts2"])
    esink = P.sb("esink", [128, 2], F32)
    P.dma(esink[:], sink.partition_broadcast(128), (), ["esink"])
    P.act(esink[:], esink[:], AF.Exp, ["esink"], ["esink"])
    p_s = [pbk[0], pbk[1], pbk[5]]
    p_ot = [pbk[2], pbk[3]]
    p_f = pbk[4]
    pstok = ["p_ex", "p_e2", "p_u1"]
    pottok = ["p_sc", "p_u"]
    st = dict(ld=0, pt=0, s=0, ot=0, bo=0, rec=0)

    def load_T(src, dstT, dtok):
        for t0 in range(0, NTB, 8):
            n = min(8, NTB - t0)
            b = st["ld"] % 2
            st["ld"] += 1
            P.dma(ld[b][:, 0:n, :], src[t0 * 128:(t0 + n) * 128, :].rearrange("(t p) c -> p t c", p=128), (), ["ld%d" % b])
            for k in range(n):
                P.tr(pbb[:, k, :], ld[b][:, k, :], identb[:], ["ld%d" % b, "identb"], ["p_tr"])
            P.cp(dstT[:, t0 * 128:(t0 + n) * 128], pbb[:, 0:n, :].rearrange("p t c -> p (t c)"), ["p_tr"], [dtok])

    def attn_pass(qsrc, ksrc, vsrc, outd, window):
        load_T(qsrc, QT2, "QT2")
        load_T(ksrc, KT2, "KT2")
        P.add("gpsimd", lambda e: e.memset(Vx[:, :, 64:65], 1.0), (), ["Vx"])
        for v0 in range(0, NTB, 32):
            vn = min(32, NTB - v0)
            P.dma(Vx[:, v0:v0 + vn, 0:64], vsrc[v0 * 128:(v0 + vn) * 128, :].rearrange("(t p) c -> p t c", p=128), (), ["Vx"])
        if window:
            groups = [(t, 1) for t in range(NTB)]
        else:
            groups = [(0, CTXT)] + [(t, min(4, NTB - t)) for t in range(CTXT, NTB, 4)]
        for (t0, nt) in groups:
            nq = nt * 128
            q0 = t0 * 128
            if t0 < CTXT:
                kbs = [(kb, None) for kb in range(CTXT)]
            elif window:
                kbs = [(kb, None) for kb in range(CTXT)]
                if t0 - 1 >= CTXT:
                    kbs.append((t0 - 1, 0))
                kbs.append((t0, None))
                if t0 + 1 < NTB:
                    kbs.append((t0 + 1, 1))
            else:
                kbs = [(kb, None) for kb in range(NTB)]
            gb = st["bo"] % 2
            st["bo"] += 1
            for e_ in range(2):
                hp = slice(64 * e_, 64 * e_ + 64)
                ob_ = st["ot"] % 2
                st["ot"] += 1
                def emit_S(n):
                    kb, msk = kbs[n]
                    sb_ = st["s"] % 3
                    st["s"] += 1
                    P.mm(p_s[sb_][:, 0:nq], KT2[hp, kb * 128:(kb + 1) * 128], QT2[hp, q0:q0 + nq], True, True,
                         ["KT2", "QT2"], [pstok[sb_]])
                    return sb_

                def emit_rest(n, sb_):
                    kb, msk = kbs[n]
                    pb_ = st["pt"] % 3
                    st["pt"] += 1
                    P.act(PT[pb_][:, 0:nq], p_s[sb_][:, 0:nq], AF.Exp, [pstok[sb_]], ["PT%d" % pb_], scale=0.125)
                    if msk is not None:
                        P.tt(PT[pb_][:, 0:nq], PT[pb_][:, 0:nq], wmt[:, msk, :], ALU.mult, ["PT%d" % pb_, "consts2"],
                             ["PT%d" % pb_], eng="gpsimd")
                    P.mm(p_ot[ob_][0:65, 0:nq], Vx[:, kb, 0:65], PT[pb_][:, 0:nq], n == 0, n == len(kbs) - 1,
                         ["Vx", "PT%d" % pb_], [pottok[ob_]])

                LA = 2
                pend = [emit_S(n) for n in range(min(LA, len(kbs)))]
                for n in range(len(kbs)):
                    if n + LA < len(kbs):
                        pend.append(emit_S(n + LA))
                    emit_rest(n, pend.pop(0))
                P.cp(OT[ob_][:, 0:nq], p_ot[ob_][0:65, 0:nq], [pottok[ob_]], ["OT%d" % ob_])
                for k in range(nt):
                    rb = st["rec"] % 2
                    st["rec"] += 1
                    P.tr(p_f[:, 0:65], OT[ob_][:, k * 128:(k + 1) * 128], identf[0:65, 0:65], ["OT%d" % ob_, "identf"], ["p_o"])
                    if window:
                        P.ts(rec[rb][:], p_f[:, 64:65], esink[:, e_:e_ + 1], ALU.add, ["p_o", "esink"], ["rec%d" % rb])
                        P.add("vector", lambda e, rb=rb: e.reciprocal(out=rec[rb][:], in_=rec[rb][:]), ["rec%d" % rb], ["rec%d" % rb])
                    else:
                        P.add("vector", lambda e, rb=rb: e.reciprocal(out=rec[rb][:], in_=p_f[:, 64:65]), ["p_o"], ["rec%d" % rb])
                    P.ts(bo[gb][:, k, hp], p_f[:, 0:64], rec[rb][:, 0:1], ALU.mult, ["p_o", "rec%d" % rb], ["bo%d" % gb])
            P.dma(outd[q0:q0 + nq, :].rearrange("(t p) c -> p t c", p=128), bo[gb][:, 0:nt, :], ["bo%d" % gb], ["outd"])

    if parts[1]:
        attn_pass(qa, ka, va, Bo, False)
    if parts[2]:
        attn_pass(qw, kw, vw, Co, True)
    P.final_waits("sync")
    P.emit()
    return nc


def barrier(P):
    for e in ENGS:
        need = {}
        for o in ENGS:
            if o != e and P.ccnt[o]:
                k, v = P._ev_sem(("c", o, P.ccnt[o]))
                need[k] = v
            n = P.dcnt[o]
            for i in range(max(0, n - DMA_SLOTS), n):
                k, v = P._ev_sem(("d", o, i))
                need[k] = max(need.get(k, 0), v)
        need = {k: v for k, v in need.items() if P.seen[e].get(k, 0) < v}
        for k, v in need.items():
            P.seen[e][k] = v
        P.ops[e].append((None, sorted(need.items(), key=str), None, False))


def build_PC(NT, NEXP=32):
    NTOK = NT * 128
    nc = bass.Bass("TRN2", target_bir_lowering=False)
    dt_in = lambda n, s, d=F32: nc.dram_tensor(n, list(s), d, kind="ExternalInput").ap()
    dt_out = lambda n, s, d: nc.dram_tensor(n, list(s), d, kind="ExternalOutput").ap()
    x = dt_in("x", [NTOK, D])
    brd = [dt_in(n, [NTOK, 512], BF16) for n in ("A", "B", "C")]
    cc = dt_in("cc", [D, 2])
    wmod = dt_in("wmod", [D, 6144])
    bmod = dt_in("bmod", [6144])
    n1g = dt_in("n1g", [D])
    n2g = dt_in("n2g", [D])
    fng = dt_in("fng", [D])
    wgt = dt_in("wgt", [D, 3072])
    wbr = [dt_in(n, [512, D]) for n in ("wba", "wbb", "wbc")]
    wout = dt_in("wout", [D, D])
    wr = dt_in("wr", [D, 36])
    wg = dt_in("wg", [NEXP, D, 512])
    wu = dt_in("wu", [NEXP, D, 512])
    wd = dt_in("wd", [NEXP, 512, D])
    Xn = dt_out("Xn", [NTOK, D], F32)
    Yn = dt_out("Yn", [NTOK, D], F32)
    X1 = nc.dram_tensor("X1s", [NTOK, D], F32, kind="Internal").ap()
    H2T = nc.dram_tensor("H2Ts", [128, 8, NTOK], BF16, kind="Internal").ap()
    WR = nc.dram_tensor("WRs", [NTOK, 32], F32, kind="Internal").ap()

    P = Prog(nc)
    identf = P.ident("identf", F32)
    identb = P.sb("identb", [128, 128], BF16)
    P.cp(identb[:], identf[:], ["identf"], ["identb"])
    one1 = P.sb("one1", [1, 128], F32)
    P.add("vector", lambda e: e.memset(one1[:], 1.0), (), ["one1"])
    arena = P.sb("arena", [128, 45056], BF16)
    Wgt = arena[:, 0:24576].rearrange("p (c n) -> p c n", c=8)
    Wbr = arena[:, 24576:36864].rearrange("p (b c n) -> p b c n", b=3, c=4)
    Wout = arena[:, 36864:45056].rearrange("p (c n) -> p c n", c=8)
    wrb = P.sb("wrb", [128, 8, 36], BF16)
    wst = [P.sb("wstC%d" % i, [128, 4, 512], F32) for i in range(2)]
    wsc = [0]

    def load_cast(dst, src_ap, dtok, eng=None):
        P.dma(dst, src_ap, (), [dtok], q="gpsimd")

    for g in range(6):
        for h in range(2):
            load_cast(Wgt[:, 4 * h:4 * h + 4, g * 512:(g + 1) * 512],
                      wgt[512 * h:512 * h + 512, g * 512:(g + 1) * 512].rearrange("(c p) n -> p c n", p=128), "Wgt")
    for bi in range(3):
        for h in range(2):
            load_cast(Wbr[:, bi, :, h * 512:(h + 1) * 512], wbr[bi][:, h * 512:(h + 1) * 512].rearrange("(c p) n -> p c n", p=128), "Wbr")
    for g in range(2):
        for h in range(2):
            load_cast(Wout[:, 4 * h:4 * h + 4, g * 512:(g + 1) * 512],
                      wout[512 * h:512 * h + 512, g * 512:(g + 1) * 512].rearrange("(c p) n -> p c n", p=128), "Wout")
    wrs = P.sb("wrs", [128, 8, 36], F32)
    P.dma(wrs[:], wr.rearrange("(c p) n -> p c n", p=128), (), ["wrs"])
    P.cp(wrb[:], wrs[:], ["wrs"], ["wrb"])

    pbk = [P.ps("pbk%d" % i, [128, 512]) for i in range(7)]
    pbb = P.ps("pbb", [128, 8, 128], BF16)
    scT = P.sb("scT", [128, 8, 2], F32)
    P.dma(scT[:], cc.rearrange("(c p) j -> p c j", p=128), (), ["scT"])
    P.act(scT[:], scT[:], AF.Silu, ["scT"], ["scT"])
    rowg = P.sb("rowg", [1, 512], F32)
    browg = P.sb("browg", [1, 512], F32)
    growg = P.sb("growg", [1, 512], F32)
    scal = [[P.sb("scal%d_%d" % (k, j), [128, 8], F32) for j in range(2)] for k in range(2)]
    bias = [[P.sb("bias%d_%d" % (k, j), [128, 8], F32) for j in range(2)] for k in range(2)]
    gaB = [[P.sb("gaB%d_%d" % (k, j), [128, D], F32) for j in range(2)] for k in range(2)]
    one11 = one1[:, 0:1]
    ngs = (n1g, n2g)
    for g in range(12):
        v, hf = g // 2, g % 2
        k, kind = v // 3, v % 3
        P.dma(browg[:], bmod[g * 512:(g + 1) * 512].rearrange("(o n) -> o n", o=1), (), ["browg"])
        if kind == 1:
            P.dma(growg[:], ngs[k][hf * 512:(hf + 1) * 512].rearrange("(o n) -> o n", o=1), (), ["growg"])
        for j in range(2):
            for h in range(2):
                b_ = wsc[0] % 2
                wsc[0] += 1
                P.dma(wst[b_][:], wmod[512 * h:512 * h + 512, g * 512:(g + 1) * 512].rearrange("(c p) n -> p c n", p=128),
                      (), ["wstC%d" % b_])
                for c in range(4):
                    P.mm(pbk[0][0:1, :], scT[:, 4 * h + c, j:j + 1], wst[b_][:, c, :], h == 0 and c == 0, h == 1 and c == 3,
                         ["wstC%d" % b_, "scT"], ["pb0"])
            P.tt(rowg[:], pbk[0][0:1, :], browg[:], ALU.add, ["pb0", "browg"], ["rowg"])
            if kind == 1:
                P.stt(rowg[:], rowg[:], 1.0, growg[:], ALU.add, ALU.mult, ["rowg", "growg"], ["rowg"])
            if kind < 2:
                for c in range(4):
                    P.mm(pbk[1][:, c:c + 1], rowg[:, c * 128:(c + 1) * 128], one11, True, True, ["rowg", "one1"], ["pb1"])
                dstc = (bias if kind == 0 else scal)[k][j]
                P.cp(dstc[:, hf * 4:hf * 4 + 4], pbk[1][:, 0:4], ["pb1"], ["modcols"])
            else:
                P.mm(pbk[1][:, :], one1[:, :], rowg[:], True, True, ["rowg", "one1"], ["pb1"])
                P.cp(gaB[k][j][:, hf * 512:(hf + 1) * 512], pbk[1][:, :], ["pb1"], ["gaB"])
    fngB = P.sb("fngB", [128, D], F32)
    P.dma(fngB[:], fng.partition_broadcast(128), (), ["fngB"])

    xt = P.sb("xt", [128, D], F32)
    ss = [P.sb("ss%d" % i, [128, 1], F32) for i in range(2)]
    xh = [P.sb("xh0", [128, D], F32)] * 2
    hT = [P.sb("hT%d" % i, [128, 8, 128], BF16) for i in range(2)]
    ptr = [pbk[2].rearrange("p (c j) -> p c j", c=4), pbk[3].rearrange("p (c j) -> p c j", c=4)]
    G = P.sb("G", [128, D], F32)
    brt = [P.sb("brt%d" % i, [128, 512], BF16) for i in range(2)]
    brT = [P.sb("brT%d" % i, [128, 4, 128], BF16) for i in range(2)]
    m = P.sb("m", [128, D], F32)
    tmp = P.sb("tmp", [128, D], F32)
    sq = [tmp, tmp]
    mT = P.sb("mT", [128, 8, 128], BF16)
    x1 = P.sb("x1", [128, D], F32)
    Lr = P.sb("Lr", [128, 36], F32)
    Lm = P.sb("Lm", [128, 32], F32)
    k1 = P.sb("k1", [128, 32], F32)
    k2 = P.sb("k2", [128, 32], F32)
    Wt = P.sb("Wt", [128, 32], F32)
    r8 = P.sb("r8", [128, 8], F32)
    g4 = P.sb("g4", [128, 4], F32)
    pen = P.sb("pen", [128, 4], F32)
    mmc = [0]

    def bank():
        k = 4 + (mmc[0] % 3)
        mmc[0] += 1
        return pbk[k], "pb%d" % k

    def norm_T(i, xin, xtok, k, j, hTt, hTtok):
        P.act(sq[0][:], xin, AF.Square, [xtok], ["tmp", "Css"], accum=ss[0][:])
        P.act(ss[0][:], ss[0][:], AF.Sqrt, ["Css"], ["Css"], scale=1.0 / D, bias=EPS)
        P.add("vector", lambda e: e.reciprocal(out=ss[0][:], in_=ss[0][:]), ["Css"], ["Css"])
        P.ts(xh[0][:], xin, ss[0][:, 0:1], ALU.mult, [xtok, "Css"], ["Cxh"])
        for half in range(2):
            ptk = "pb%d" % (2 + half)
            for c4 in range(4):
                c = half * 4 + c4
                P.tr(ptr[half][:, c4, :], xh[0][:, c * 128:(c + 1) * 128], identf[:], ["Cxh", "identf"], [ptk])
            for c4 in range(4):
                c = half * 4 + c4
                P.act(hTt[:, c, :], ptr[half][:, c4, :], AF.Identity, [ptk, "modcols"], [hTtok],
                      scale=scal[k][j][:, c:c + 1], bias=bias[k][j][:, c:c + 1])

    for i in range(NT):
        j = 1 if i < CTXT else 0
        rs = slice(i * 128, (i + 1) * 128)
        P.dma(xt[:], x[rs, :], (), ["xt"])
        norm_T(i, xt[:], "xt", 0, j, hT[0], "hT0")
        for bi in range(3):
            bb = bi % 2
            P.dma(brt[bb][:], brd[bi][rs, :], (), ["brt%d" % bb])
            for c in range(4):
                P.tr(pbb[:, c, :], brt[bb][:, c * 128:(c + 1) * 128], identb[:], ["brt%d" % bb, "identb"], ["pbb"])
            P.cp(brT[bb][:], pbb[:, 0:4, :], ["pbb"], ["brT%d" % bb])
            for hf in range(2):
                pk, tk = bank()
                for c in range(8):
                    P.mm(pk[:], hT[0][:, c, :], Wgt[:, c, bi * 1024 + hf * 512:bi * 1024 + (hf + 1) * 512], c == 0, c == 7,
                         ["hT0", "Wgt"], [tk])
                P.act(G[:, hf * 512:(hf + 1) * 512], pk[:], AF.Sigmoid, [tk], ["G"])
            for hf in range(2):
                pk, tk = bank()
                for c in range(4):
                    P.mm(pk[:], brT[bb][:, c, :], Wbr[:, bi, c, hf * 512:(hf + 1) * 512], c == 0, c == 3,
                         ["brT%d" % bb, "Wbr"], [tk])
                hs_ = slice(hf * 512, (hf + 1) * 512)
                if bi == 0:
                    P.tt(m[:, hs_], pk[:], G[:, hs_], ALU.mult, [tk, "G"], ["m"])
                else:
                    P.tt(tmp[:, hs_], pk[:], G[:, hs_], ALU.mult, [tk, "G"], ["tmp"])
                    P.tt(m[:, hs_], m[:, hs_], tmp[:, hs_], ALU.add, ["m", "tmp"], ["m"], eng="gpsimd")
        for half in range(2):
            ptk = "pb%d" % (2 + half)
            for c4 in range(4):
                c = half * 4 + c4
                P.tr(ptr[half][:, c4, :], m[:, c * 128:(c + 1) * 128], identf[:], ["m", "identf"], [ptk])
            P.cp(mT[:, half * 4:half * 4 + 4, :], ptr[half][:, :, :], [ptk], ["mT"], eng="scalar")
        for hf in range(2):
            pk, tk = bank()
            hs_ = slice(hf * 512, (hf + 1) * 512)
            for c in range(8):
                P.mm(pk[:], mT[:, c, :], Wout[:, c, hs_], c == 0, c == 7, ["mT", "Wout"], [tk])
            P.tt(x1[:, hs_], pk[:], gaB[0][j][:, hs_], ALU.mult, [tk, "gaB"], ["x1"])
            P.tt(x1[:, hs_], x1[:, hs_], xt[:, hs_], ALU.add, ["x1", "xt"], ["x1"], eng="gpsimd")
        P.dma(X1[rs, :], x1[:], ["x1"], ["X1s"])
        norm_T(i, x1[:], "x1", 1, j, hT[1], "hT1")
        P.dma(H2T[:, :, rs], hT[1][:], ["hT1"], ["H2Ts"])
        pk, tk = bank()
        for c in range(8):
            P.mm(pk[:, 0:36], hT[1][:, c, :], wrb[:, c, :], c == 0, c == 7, ["hT1", "wrb"], [tk])
        P.cp(Lr[:], pk[:, 0:36], [tk], ["Lr"])
        R_ = ["Lr", "r8", "g4", "pen", "Lm", "k1", "k2", "Wt"]
        P.add("vector", lambda e: e.tensor_reduce(out=r8[:, 0:1], in_=Lr[:, 0:4], axis=AX.X, op=ALU.max), R_, R_)
        P.ts(g4[:], Lr[:, 0:4], r8[:, 0:1], ALU.is_ge, R_, R_)
        P.ts(r8[:, 1:2], r8[:, 0:1], -1.0, ALU.mult, R_, R_)
        P.act(pen[:], Lr[:, 0:4], AF.Exp, R_, R_, bias=r8[:, 1:2], accum=r8[:, 2:3])
        P.add("vector", lambda e: e.reciprocal(out=r8[:, 2:3], in_=r8[:, 2:3]), R_, R_)
        P.ts(pen[:], g4[:], -1.0, ALU.add, R_, R_, s2=1e30, op1=ALU.mult)
        P.tt(Lm[:].rearrange("p (g j) -> p g j", g=4), Lr[:, 4:36].rearrange("p (g j) -> p g j", g=4),
             pen[:, :].unsqueeze(2).to_broadcast([128, 4, 8]), ALU.add, R_, R_)
        P.add("vector", lambda e: e.tensor_reduce(out=r8[:, 3:4], in_=Lm[:], axis=AX.X, op=ALU.max), R_, R_)
        P.ts(k1[:], Lm[:], r8[:, 3:4], ALU.is_ge, R_, R_)
        P.stt(Lm[:], k1[:], -1e30, Lm[:], ALU.mult, ALU.add, R_, R_)
        P.add("vector", lambda e: e.tensor_reduce(out=r8[:, 4:5], in_=Lm[:], axis=AX.X, op=ALU.max), R_, R_)
        P.ts(k2[:], Lm[:], r8[:, 4:5], ALU.is_ge, R_, R_)
        P.tt(r8[:, 5:6], r8[:, 4:5], r8[:, 3:4], ALU.subtract, R_, R_)
        P.act(r8[:, 5:6], r8[:, 5:6], AF.Exp, R_, R_)
        P.ts(r8[:, 6:7], r8[:, 5:6], 1.0, ALU.add, R_, R_)
        P.add("vector", lambda e: e.reciprocal(out=r8[:, 6:7], in_=r8[:, 6:7]), R_, R_)
        P.tt(r8[:, 7:8], r8[:, 5:6], r8[:, 6:7], ALU.mult, R_, R_)
        P.tt(r8[:, 6:7], r8[:, 6:7], r8[:, 2:3], ALU.mult, R_, R_)
        P.tt(r8[:, 7:8], r8[:, 7:8], r8[:, 2:3], ALU.mult, R_, R_)
        P.ts(Wt[:], k1[:], r8[:, 6:7], ALU.mult, R_, R_)
        P.stt(Wt[:], k2[:], r8[:, 7:8], Wt[:], ALU.mult, ALU.add, R_, R_)
        P.dma(WR[rs, :], Wt[:], R_, ["WRs"])

    barrier(P)
    SBT = 9
    WE = [arena[:, p * 12288:(p + 1) * 12288].rearrange("p (m c n) -> p m c n", m=3, c=8) for p in range(2)]
    h2sb = arena[:, 24576:33792].rearrange("p (c n) -> p c n", c=8)
    AT = [arena[:, 33792 + q * 2048:33792 + (q + 1) * 2048].rearrange("p (c n) -> p c n", c=4) for q in range(2)]
    ysb = P.sb("y", [128, 8, D], F32)
    ytl = [ysb[:, t, :] for t in range(8)] + [xt[:, :]]
    Wsb = P.sb("Wsb", [128, SBT, 32], F32)
    sg = [P.sb("sg%d" % i, [128, 512], F32) for i in range(2)]
    x1r = [m, tmp]
    cntm = dict(at=0, sg=0)
    for s0 in range(0, NT, SBT):
        ns = min(SBT, NT - s0)
        ntk = ns * 128
        P.dma(h2sb[:, :, 0:ntk], H2T[:, :, s0 * 128:s0 * 128 + ntk], ["H2Ts"], ["h2sb"])
        P.dma(Wsb[:, 0:ns, :], WR[s0 * 128:s0 * 128 + ntk, :].rearrange("(t p) e -> p t e", p=128), ["WRs"], ["Wsb"])
        for e_ in range(NEXP):
            p = e_ % 2
            wtok = "WE%d" % p
            for mi, src in enumerate((wg, wu)):
                for h in range(2):
                    load_cast(WE[p][:, mi, 4 * h:4 * h + 4, :], src[e_, 512 * h:512 * h + 512, :].rearrange("(c p) n -> p c n", p=128), wtok)
            for h in range(2):
                load_cast(WE[p][:, 2, :, :].rearrange("p (fc hf) n -> p fc hf n", hf=2)[:, :, h, :],
                          wd[e_, :, h * 512:(h + 1) * 512].rearrange("(c p) n -> p c n", p=128), wtok)
            for g0 in range(0, ns, 4):
                ng = min(4, ns - g0)
                n = ng * 128
                a = cntm["at"] % 2
                cntm["at"] += 1
                for fc in range(4):
                    pg_, tg = bank()
                    for c in range(8):
                        P.mm(pg_[:, 0:n], WE[p][:, 0, c, fc * 128:(fc + 1) * 128], h2sb[:, c, g0 * 128:g0 * 128 + n], c == 0, c == 7,
                             [wtok, "h2sb"], [tg])
                    pu_, tu = bank()
                    for c in range(8):
                        P.mm(pu_[:, 0:n], WE[p][:, 1, c, fc * 128:(fc + 1) * 128], h2sb[:, c, g0 * 128:g0 * 128 + n], c == 0, c == 7,
                             [wtok, "h2sb"], [tu])
                    sb_ = cntm["sg"] % 2
                    cntm["sg"] += 1
                    P.act(sg[sb_][:, 0:n], pg_[:, 0:n], AF.Silu, [tg], ["sg%d" % sb_])
                    P.tt(AT[a][:, fc, 0:n], pu_[:, 0:n], sg[sb_][:, 0:n], ALU.mult, [tu, "sg%d" % sb_], ["AT%d" % a])
                for t in range(ng):
                    ti = g0 + t
                    for hf in range(2):
                        py_, ty = bank()
                        for fc in range(4):
                            P.mm(py_[:], AT[a][:, fc, t * 128:(t + 1) * 128], WE[p][:, 2, fc * 2 + hf, :], fc == 0, fc == 3,
                                 ["AT%d" % a, wtok], [ty])
                        yv = ytl[ti][:, hf * 512:(hf + 1) * 512]
                        if e_ == 0:
                            P.ts(yv, py_[:], Wsb[:, ti, e_:e_ + 1], ALU.mult, [ty, "Wsb"], ["y%d" % ti])
                        else:
                            P.stt(yv, py_[:], Wsb[:, ti, e_:e_ + 1], yv, ALU.mult, ALU.add, [ty, "Wsb", "y%d" % ti], ["y%d" % ti])
        for t in range(ns):
            i = s0 + t
            j = 1 if i < CTXT else 0
            rs = slice(i * 128, (i + 1) * 128)
            xb = x1r[t % 2]
            xtk = "x1r%d" % (t % 2)
            P.dma(xb[:], X1[rs, :], ["X1s"], [xtk])
            P.tt(ytl[t], ytl[t], gaB[1][j][:], ALU.mult, ["y%d" % t, "gaB"], ["y%d" % t], eng="gpsimd")
            P.tt(xb[:], xb[:], ytl[t], ALU.add, [xtk, "y%d" % t], [xtk])
            P.dma(Xn[rs, :], xb[:], [xtk], ["Xn"])
            P.act(G[:], xb[:], AF.Square, [xtk], ["G", "Css"], accum=ss[0][:])
            P.act(ss[0][:], ss[0][:], AF.Sqrt, ["Css"], ["Css"], scale=1.0 / D, bias=EPS)
            P.add("vector", lambda e: e.reciprocal(out=ss[0][:], in_=ss[0][:]), ["Css"], ["Css"])
            P.stt(G[:], xb[:], ss[0][:, 0:1], fngB[:], ALU.mult, ALU.mult, [xtk, "Css", "fngB"], ["G"])
            P.dma(Yn[rs, :], G[:], ["G"], ["Yn"])
        barrier(P)
    P.final_waits("sync")
    P.emit()
    return nc


_CACHE = {}


def _rope_tables(S):
    pos = np.arange(S)
    row = (pos // 64).astype(np.float32)
    col = (pos % 64).astype(np.float32)
    inv = (10000.0 ** (-np.arange(16, dtype=np.float32) / np.float32(16))).astype(np.float32)
    ang = np.concatenate([row[:, None] * inv, col[:, None] * inv], axis=-1).astype(np.float32)
    return np.cos(ang).astype(np.float32), np.sin(ang).astype(np.float32)


def _prog(key, fn):
    if key not in _CACHE:
        _CACHE[key] = fn()
    return _CACHE[key]


def kernel(x, c, ctx, c_ctx, w_mod, b_mod, norm1_g, norm2_g, w_in, hgrn_lb_logits, hgrn_out_norm_g,
           attn_q_norm_g, attn_k_norm_g, swa_sink, w_branch_a, w_branch_b, w_branch_c, w_out,
           w_group, w_router, w_exp_gate, w_exp_up, w_exp_down, final_norm_g):
    f32 = lambda a: np.ascontiguousarray(np.asarray(a), dtype=np.float32)
    x, c, ctx, c_ctx = f32(x), f32(c), f32(ctx), f32(c_ctx)
    Bn, S, _ = x.shape
    QN = 4
    Lc = S // QN
    NT = CTXT + Lc // 128
    NL = S // 128
    depth = w_in.shape[0]
    cosL, sinL = _rope_tables(S)
    cos_c = np.ones((256, 32), np.float32)
    sin_c = np.zeros((256, 32), np.float32)
    hc = hgrn_consts()
    i = np.arange(128)
    wm = np.stack([(i[:, None] >= i[None, :]), (i[:, None] <= i[None, :])]).astype(np.float32)
    xl, xc = x, ctx
    cores = [(b, j) for b in range(Bn) for j in range(QN)]
    yout = None
    for layer in range(depth):
        xin = [np.concatenate([xc[b], xl[b, j * Lc:(j + 1) * Lc]], 0) for (b, j) in cores]
        ccs = [np.ascontiguousarray(np.stack([c[b], c_ctx], 1)) for (b, j) in cores]
        pa = _prog(("PA", NT, layer), lambda: build_PA(NT, layer))
        wmodA = f32(w_mod[layer][:, :2048])
        winA = f32(w_in[layer][:, :4096])
        ims = []
        for k, (b, j) in enumerate(cores):
            ims.append(dict(x=xin[k], cc=ccs[k], wmod=wmodA, bmod=f32(b_mod[layer][:2048]), n1g=f32(norm1_g[layer]),
                            win=winA, lbl=f32(hgrn_lb_logits), aqg=f32(attn_q_norm_g[layer]), akg=f32(attn_k_norm_g[layer]),
                            cos=np.concatenate([cos_c, cosL[j * Lc:(j + 1) * Lc]], 0),
                            sin=np.concatenate([sin_c, sinL[j * Lc:(j + 1) * Lc]], 0)))
        ra = _run(pa, ims)
        full = {}
        for nm in ("QS", "KKf", "KKb", "LFf", "LFb", "VH", "OG", "QA", "QW", "KV4"):
            full[nm] = [np.concatenate([np.asarray(ra[b * QN][nm])[:256]] +
                                       [np.asarray(ra[b * QN + j][nm])[256:] for j in range(QN)], 0) for b in range(Bn)]
        del ra
        pb = _prog(("PB", NL), lambda: build_PB(NL))
        ims = []
        for b in range(Bn):
            for hp in range(4):
                hs = slice(hp * 128, (hp + 1) * 128)
                kv = hp // 2
                ksl = lambda o: slice(o + kv * 64, o + kv * 64 + 64)
                kv4 = full["KV4"][b]
                d = dict(qs=full["QS"][b][:, hs], kk0=full["KKf"][b][:, hs], kk1=full["KKb"][b][:, hs],
                         lf0=full["LFf"][b][:, hs], lf1=full["LFb"][b][:, hs], vh=full["VH"][b][:, hs],
                         og=full["OG"][b][:, hs], hg=f32(hgrn_out_norm_g[layer]),
                         qa=full["QA"][b][:, hs], ka=np.concatenate([kv4[:, ksl(0)], kv4[:, ksl(0)]], 1), va=kv4[:, ksl(256)],
                         qw=full["QW"][b][:, hs], kw=np.concatenate([kv4[:, ksl(128)], kv4[:, ksl(128)]], 1), vw=kv4[:, ksl(384)],
                         sink=f32(swa_sink[layer][2 * hp:2 * hp + 2]), wm=wm)
                for dd in range(2):
                    for nm in ("MC", "Mend", "Mdiag", "Moff"):
                        d["%s%d" % (nm, dd)] = hc[dd][nm]
                ims.append({k_: np.ascontiguousarray(v) for k_, v in d.items()})
        del full
        rb = _run(pb, ims)
        br = {}
        for nm in ("A", "B", "C"):
            br[nm] = [np.concatenate([np.asarray(rb[b * 4 + hp][nm]) for hp in range(4)], 1) for b in range(Bn)]
        del rb
        pc = _prog(("PC", NT), lambda: build_PC(NT))
        wmodC = f32(w_mod[layer])
        shared = dict(wmod=wmodC, bmod=f32(b_mod[layer]), n1g=f32(norm1_g[layer]), n2g=f32(norm2_g[layer]),
                      fng=f32(final_norm_g), wgt=f32(w_in[layer][:, 4096:7168]), wba=f32(w_branch_a[layer]),
                      wbb=f32(w_branch_b[layer]), wbc=f32(w_branch_c[layer]), wout=f32(w_out[layer]),
                      wr=f32(np.concatenate([np.asarray(w_group[layer]), np.asarray(w_router[layer])], 1)),
                      wg=f32(w_exp_gate[layer]), wu=f32(w_exp_up[layer]), wd=f32(w_exp_down[layer]))
        ims = []
        for k, (b, j) in enumerate(cores):
            d = dict(shared)
            d["x"] = xin[k]
            d["cc"] = ccs[k]
            for nm in ("A", "B", "C"):
                d[nm] = np.ascontiguousarray(np.concatenate([br[nm][b][:256], br[nm][b][256 + j * Lc:256 + (j + 1) * Lc]], 0))
            ims.append(d)
        rc = _run(pc, ims)
        xl = np.stack([np.concatenate([np.asarray(rc[b * QN + j]["Xn"])[256:] for j in range(QN)], 0) for b in range(Bn)])
        xc = np.stack([np.asarray(rc[b * QN]["Xn"])[:256] for b in range(Bn)])
        if layer == depth - 1:
            yout = np.stack([np.concatenate([np.asarray(rc[b * QN + j]["Yn"])[256:] for j in range(QN)], 0) for b in range(Bn)])
        del rc
    return yout.astype(np.float32)
```

```python
import numpy as np
import ml_dtypes
import concourse.bass as bass
import concourse.mybir as mybir
from concourse.bass_utils import run_bass_kernel_spmd

F32 = mybir.dt.float32
BF16 = mybir.dt.bfloat16
AF = mybir.ActivationFunctionType
ALU = mybir.AluOpType
AX = mybir.AxisListType
NPBF = ml_dtypes.bfloat16

ENGS = ["sync", "scalar", "vector", "gpsimd", "tensor"]
EPOCH = 12000
DMA_SLOTS = 6
DMA_EPOCH = 700

D = 1024
CTXT = 2
EPS = 1e-6
TINY = 1e-30


class Prog:
    def __init__(self, nc):
        self.nc = nc
        self.ops = {e: [] for e in ENGS}
        self.ccnt = {e: 0 for e in ENGS}
        self.dcnt = {e: 0 for e in ENGS}
        self.tok_w = {}
        self.tok_r = {}
        self.seen = {e: {} for e in ENGS}
        self.sems = {}
        self.nsem = 0
        self.uid = 0

    def _sem(self, key):
        if key not in self.sems:
            self.sems[key] = self.nc.alloc_semaphore(name="s%d" % self.nsem)
            self.nsem += 1
        return self.sems[key]

    def _ev_sem(self, ev):
        kind, eng, idx = ev
        if kind == "c":
            ep = (idx - 1) // EPOCH
            return ("c", eng, ep), idx - ep * EPOCH
        slot = idx % DMA_SLOTS
        use = idx // DMA_SLOTS
        ep = use // DMA_EPOCH
        return ("d", eng, slot, ep), 16 * (use - ep * DMA_EPOCH + 1)

    def add(self, eng, fn, reads=(), writes=(), dma=False):
        deps = set()
        for t in reads:
            w = self.tok_w.get(t)
            if w is not None:
                deps.add(w)
        for t in writes:
            w = self.tok_w.get(t)
            if w is not None:
                deps.add(w)
            for r in self.tok_r.get(t, ()):
                deps.add(r)
        if dma:
            i = self.dcnt[eng]
            self.dcnt[eng] += 1
            ev = ("d", eng, i)
            if i >= DMA_SLOTS:
                deps.add(("d", eng, i - DMA_SLOTS))
        else:
            self.ccnt[eng] += 1
            ev = ("c", eng, self.ccnt[eng])
        need = {}
        for d in deps:
            if d == ev:
                continue
            if d[0] == "c" and d[1] == eng == "tensor" and not dma:
                continue
            k, v = self._ev_sem(d)
            if self.seen[eng].get(k, 0) >= v:
                continue
            if need.get(k, 0) < v:
                need[k] = v
        for k, v in need.items():
            self.seen[eng][k] = v
        self.ops[eng].append((fn, sorted(need.items(), key=str), self._ev_sem(ev), dma))
        for t in reads:
            self.tok_r.setdefault(t, []).append(ev)
        for t in writes:
            self.tok_w[t] = ev
            self.tok_r[t] = []
        return ev

    def final_waits(self, eng="sync"):
        need = {}
        for e in ENGS:
            if self.ccnt[e] and e != eng:
                k, v = self._ev_sem(("c", e, self.ccnt[e]))
                need[k] = v
            n = self.dcnt[e]
            for i in range(max(0, n - DMA_SLOTS), n):
                k, v = self._ev_sem(("d", e, i))
                need[k] = max(need.get(k, 0), v)
        self.ops[eng].append((None, sorted(need.items(), key=str), None, False))

    def emit(self):
        for e in ENGS:
            for (fn, waits, inc, dma) in self.ops[e]:
                for k, v in waits:
                    self._sem(k)
                if inc is not None:
                    self._sem(inc[0])
        with self.nc.Block() as block:
            for e in ENGS:
                if not self.ops[e]:
                    continue

                def body(engh, e=e):
                    for (fn, waits, inc, dma) in self.ops[e]:
                        for k, v in waits:
                            engh.wait_ge(self.sems[k], v)
                        if fn is None:
                            continue
                        ins = fn(engh)
                        ins.then_inc(self.sems[inc[0]], 16 if dma else 1)

                getattr(block, e)(body)

    def sb(self, name, shape, dt):
        return self.nc.alloc_sbuf_tensor(name, list(shape), dt)

    def ps(self, name, shape, dt=F32):
        return self.nc.alloc_psum_tensor(name, list(shape), dt)

    def dma(self, out, in_, r, w, q=None):
        if q is None:
            q = "sync" if (self.dcnt["sync"] <= self.dcnt["gpsimd"]) else "gpsimd"
        self.add(q, lambda e: e.dma_start(out=out, in_=in_), r, w, dma=True)

    def act(self, out, in_, func, r, w, scale=1.0, bias=0.0, accum=None):
        if accum is None:
            self.add("scalar", lambda e: e.activation(out=out, in_=in_, func=func, scale=scale, bias=bias), r, w)
        else:
            self.add("scalar", lambda e: e.activation(out=out, in_=in_, func=func, scale=scale, bias=bias,
                                                      accum_out=accum), r, w)

    def tt(self, out, a, b, op, r, w, eng="vector"):
        self.add(eng, lambda e: e.tensor_tensor(out=out, in0=a, in1=b, op=op), r, w)

    def ts(self, out, a, s1, op0, r, w, s2=None, op1=None, eng="vector"):
        if op1 is None:
            self.add(eng, lambda e: e.tensor_scalar(out=out, in0=a, scalar1=s1, scalar2=None, op0=op0), r, w)
        else:
            self.add(eng, lambda e: e.tensor_scalar(out=out, in0=a, scalar1=s1, scalar2=s2, op0=op0, op1=op1), r, w)

    def stt(self, out, a, s, b, op0, op1, r, w):
        self.add("vector", lambda e: e.scalar_tensor_tensor(out=out, in0=a, scalar=s, in1=b, op0=op0, op1=op1), r, w)

    def cp(self, out, in_, r, w, eng="vector"):
        if eng == "scalar":
            self.add(eng, lambda e: e.activation(out=out, in_=in_, func=AF.Copy), r, w)
        else:
            self.add(eng, lambda e: e.tensor_copy(out=out, in_=in_), r, w)

    def mm(self, out, lhsT, rhs, start, stop, r, w):
        self.add("tensor", lambda e: e.matmul(out, lhsT=lhsT, rhs=rhs, start=start, stop=stop), r, w)

    def tr(self, out, in_, ident, r, w):
        self.add("tensor", lambda e: e.transpose(out=out, in_=in_, identity=ident), r, w)

    def ident(self, name, dt):
        t = self.sb(name, [128, 128], dt)
        self.add("gpsimd", lambda e: e.memset(t[:], 0.0), (), [name])
        self.add("gpsimd", lambda e: e.affine_select(out=t[:], in_=t[:], compare_op=ALU.not_equal, fill=1.0,
                                                     base=0, pattern=[[-1, 128]], channel_multiplier=1), [name], [name])
        return t


def _run(nc, in_maps):
    res = run_bass_kernel_spmd(nc, in_maps, core_ids=list(range(len(in_maps))))
    return res.results


def emit_mod(P, nc, cc, wmod, bmod, ncols, pfx, wst, wtoks):
    scT = P.sb(pfx + "scT", [128, 8, 2], F32)
    P.dma(scT[:], cc.rearrange("(c p) j -> p c j", p=128), (), ["scT"])
    P.act(scT[:], scT[:], AF.Silu, ["scT"], ["scT"])
    rows = P.sb(pfx + "rows", [1, 2, ncols], F32)
    brow = P.sb(pfx + "brow", [1, ncols], F32)
    P.dma(brow[:], bmod.rearrange("(o n) -> o n", o=1), (), ["brow"])
    pm = P.ps(pfx + "pm", [1, 2, 512])
    for g in range(ncols // 512):
        w = wst[g % 2]
        wt = wtoks[g % 2]
        P.dma(w[:], wmod[:, g * 512:(g + 1) * 512].rearrange("(c p) n -> p c n", p=128), (), [wt])
        for j in range(2):
            for c in range(8):
                P.mm(pm[:, j, :], scT[:, c, j:j + 1], w[:, c, :], c == 0, c == 7, [wt, "scT"], ["pm"])
        for j in range(2):
            P.tt(rows[:, j, g * 512:(g + 1) * 512], pm[:, j, :], brow[:, g * 512:(g + 1) * 512], ALU.add,
                 ["pm", "brow"], ["modrow"])
    return rows


def row_to_cols(P, nc, row_ap, n, cols_out, one11, pcol, r, w):
    k = n // 128
    for c in range(k):
        P.mm(pcol[:, c:c + 1], row_ap[:, c * 128:(c + 1) * 128], one11, True, True, r, ["pcol"])
    P.cp(cols_out, pcol[:, 0:k], ["pcol"], w)


def row_bcast(P, nc, row_ap, n, out_tile, ones1, pb, r, w):
    for g in range(0, n, 512):
        m = min(512, n - g)
        P.mm(pb[:, 0:m], ones1, row_ap[:, g:g + m], True, True, r, ["pb"])
        P.cp(out_tile[:, g:g + m], pb[:, 0:m], ["pb"], w)


def emit_norm_T(P, nc, xt, xtok, i, scal, bias, ident, hT, hTtok, pfx, ptr, sq, ss, xh):
    b = i % 2
    P.act(sq[b][:], xt, AF.Square, [xtok], [pfx + "sq%d" % b, pfx + "ss%d" % b], accum=ss[b][:])
    P.act(ss[b][:], ss[b][:], AF.Sqrt, [pfx + "ss%d" % b], [pfx + "ss%d" % b], scale=1.0 / D, bias=EPS)
    P.add("vector", lambda e: e.reciprocal(out=ss[b][:], in_=ss[b][:]), [pfx + "ss%d" % b], [pfx + "ss%d" % b])
    P.ts(xh[b][:], xt, ss[b][:, 0:1], ALU.mult, [xtok, pfx + "ss%d" % b], [pfx + "xh%d" % b])
    for half in range(2):
        pt = ptr[half]
        ptk = pfx + "ptr%d" % half
        for c4 in range(4):
            c = half * 4 + c4
            P.tr(pt[:, c4, :], xh[b][:, c * 128:(c + 1) * 128], ident[:], [pfx + "xh%d" % b, "identf"], [ptk])
        for c4 in range(4):
            c = half * 4 + c4
            P.act(hT[:, c, :], pt[:, c4, :], AF.Identity, [ptk, "modcols"], [hTtok],
                  scale=scal[:, c:c + 1], bias=bias[:, c:c + 1])


def build_PA(NT, layer):
    NTOK = NT * 128
    nc = bass.Bass("TRN2", target_bir_lowering=False)
    dt_in = lambda n, s, d=F32: nc.dram_tensor(n, list(s), d, kind="ExternalInput").ap()
    dt_out = lambda n, s, d: nc.dram_tensor(n, list(s), d, kind="ExternalOutput").ap()
    x = dt_in("x", [NTOK, D])
    cc = dt_in("cc", [D, 2])
    wmod = dt_in("wmod", [D, 2048])
    bmod = dt_in("bmod", [2048])
    n1g = dt_in("n1g", [D])
    win = dt_in("win", [D, 4096])
    lbl = dt_in("lbl", [2, 512])
    aqg = dt_in("aqg", [64])
    akg = dt_in("akg", [64])
    cosd = dt_in("cos", [NTOK, 32])
    sind = dt_in("sin", [NTOK, 32])
    QS = dt_out("QS", [NTOK, 512], BF16)
    KKf = dt_out("KKf", [NTOK, 512], BF16)
    KKb = dt_out("KKb", [NTOK, 512], BF16)
    LFf = dt_out("LFf", [NTOK, 512], F32)
    LFb = dt_out("LFb", [NTOK, 512], F32)
    VH = dt_out("VH", [NTOK, 512], BF16)
    OG = dt_out("OG", [NTOK, 512], F32)
    QA = dt_out("QA", [NTOK, 512], BF16)
    QW = dt_out("QW", [NTOK, 512], BF16)
    KV4 = dt_out("KV4", [NTOK, 512], BF16)

    P = Prog(nc)
    identf = P.ident("identf", F32)
    one1 = P.sb("one1", [1, 128], F32)
    P.add("vector", lambda e: e.memset(one1[:], 1.0), (), ["one1"])
    Wb = P.sb("Wb", [128, 8, 4096], BF16)
    wst = [P.sb("wstA%d" % i, [128, 8, 512], F32) for i in range(2)]
    src_groups = [(0, 512), (512, 512), (1024, 512), (1536, 512), (2048, 512), (2560, 512), (3328, 512),
                  (3072, 128), (3840, 128), (3200, 128), (3968, 128)]
    dst = 0
    for gi, (s0, n) in enumerate(src_groups):
        P.dma(Wb[:, :, dst:dst + n], win[:, s0:s0 + n].rearrange("(c p) n -> p c n", p=128), (), ["Wb"], q="gpsimd")
        dst += n
    rows = emit_mod(P, nc, cc, wmod, bmod, 2048, "A", wst, ["wstA0", "wstA1"])
    g1row = P.sb("g1row", [1, D], F32)
    P.dma(g1row[:], n1g.rearrange("(o n) -> o n", o=1), (), ["g1row"])
    scrow = P.sb("scrow", [1, 2, D], F32)
    for j in range(2):
        P.stt(scrow[:, j, :], rows[:, j, 1024:2048], 1.0, g1row[:], ALU.add, ALU.mult, ["modrow", "g1row"], ["scrow"])
    pcol = P.ps("pcol", [128, 16])
    one11 = one1[:, 0:1]
    scal = [P.sb("scal%d" % j, [128, 8], F32) for j in range(2)]
    bias = [P.sb("bias%d" % j, [128, 8], F32) for j in range(2)]
    for j in range(2):
        row_to_cols(P, nc, scrow[:, j, :], D, scal[j][:], one11, pcol, ["scrow", "one1"], ["modcols"])
        row_to_cols(P, nc, rows[:, j, 0:1024], D, bias[j][:], one11, pcol, ["modrow", "one1"], ["modcols"])
    lbB = P.sb("lbB", [128, 512], F32)
    omlbB = P.sb("omlbB", [128, 512], F32)
    e0 = P.sb("e0", [128, 512], F32)
    e1 = P.sb("e1", [128, 512], F32)
    P.dma(e0[:], lbl[0, :].partition_broadcast(128), (), ["e0"])
    P.dma(e1[:], lbl[1, :].partition_broadcast(128), (), ["e1"])
    P.act(e0[:], e0[:], AF.Exp, ["e0"], ["e0"])
    P.act(e1[:], e1[:], AF.Exp, ["e1"], ["e1"])
    P.tt(lbB[:], e0[:], e1[:], ALU.add, ["e0", "e1"], ["lbB"])
    P.add("vector", lambda e: e.reciprocal(out=lbB[:], in_=lbB[:]), ["lbB"], ["lbB"])
    P.tt(e0[:], e0[:], lbB[:], ALU.mult, ["e0", "lbB"], ["e0"])
    P.tt(e1[:], e1[:], lbB[:], ALU.mult, ["e1", "lbB"], ["e1"])
    if layer == 0:
        P.tt(lbB[:], e0[:], e0[:], ALU.subtract, ["e0"], ["lbB"])
    else:
        P.tt(lbB[:], e0[:], e1[:], ALU.add, ["e0", "e1"], ["lbB"])
        P.tt(lbB[:], lbB[:], e0[:], ALU.subtract, ["lbB", "e0"], ["lbB"])
    P.ts(omlbB[:], lbB[:], -1.0, ALU.mult, ["lbB"], ["omlbB"], s2=1.0, op1=ALU.add)
    gq = P.sb("gq", [128, 64], F32)
    gk = P.sb("gk", [128, 64], F32)
    P.dma(gq[:], aqg.partition_broadcast(128), (), ["gq"])
    P.dma(gk[:], akg.partition_broadcast(128), (), ["gk"])

    xt = [P.sb("xt%d" % i, [128, D], F32) for i in range(2)]
    sq = [P.sb("sq%d" % i, [128, D], F32) for i in range(2)]
    ss = [P.sb("ss%d" % i, [128, 1], F32) for i in range(2)]
    xh = [P.sb("xh%d" % i, [128, D], F32) for i in range(2)]
    hT = [P.sb("hT%d" % i, [128, 8, 128], BF16) for i in range(2)]
    cs = [P.sb("cs%d" % i, [128, 2, 32], F32) for i in range(2)]
    ptr = [P.ps("ptr%d" % i, [128, 4, 128]) for i in range(2)]
    pg = [P.ps("pg%d" % i, [128, 512]) for i in range(3)]
    NW = 6
    wk = [[P.sb("wk%d_%d" % (k, i), [128, 512], F32) for i in range(2)] for k in range(NW)]
    ob = [[P.sb("ob%d_%d" % (k, i), [128, 512], BF16) for i in range(2)] for k in range(3)]
    sm = [P.sb("sm%d" % i, [128, 8], F32) for i in range(2)]
    gcount = [0]

    def proj(i, g):
        k = gcount[0] % 3
        gcount[0] += 1
        for c in range(8):
            P.mm(pg[k][:], hT[i % 2][:, c, :], Wb[:, c, g * 512:(g + 1) * 512], c == 0, c == 7,
                 ["hT%d" % (i % 2), "Wb"], ["pg%d" % k])
        return pg[k], "pg%d" % k

    def rope(src, stok, dst_, dtok, H, b, eng="gpsimd"):
        sv = src.rearrange("p (h two d) -> p h two d", h=H, two=2)
        dv = dst_.rearrange("p (h two d) -> p h two d", h=H, two=2)
        cB = cs[b][:, 0, :].unsqueeze(1).to_broadcast([128, H, 32])
        sB = cs[b][:, 1, :].unsqueeze(1).to_broadcast([128, H, 32])
        t1 = wk[4][b][:, 0:H * 32].rearrange("p (h d) -> p h d", h=H)
        t2 = wk[5][b][:, 0:H * 32].rearrange("p (h d) -> p h d", h=H)
        a, t = "wk4_%d" % b, "wk5_%d" % b
        ctk = "cs%d" % b
        P.tt(t1, sv[:, :, 0, :], cB, ALU.mult, [stok, ctk], [a], eng=eng)
        P.tt(t2, sv[:, :, 1, :], sB, ALU.mult, [stok, ctk], [t], eng=eng)
        P.tt(dv[:, :, 0, :], t1, t2, ALU.subtract, [a, t], [dtok], eng=eng)
        P.tt(t1, sv[:, :, 1, :], cB, ALU.mult, [stok, ctk], [a], eng=eng)
        P.tt(t2, sv[:, :, 0, :], sB, ALU.mult, [stok, ctk], [t], eng=eng)
        P.tt(dv[:, :, 1, :], t1, t2, ALU.add, [a, t], [dtok], eng=eng)

    def qknorm(src, stok, H, gB, gtok, b, outw, otok):
        v = src.rearrange("p (h d) -> p h d", h=H)
        tmp = wk[3][b][:, 0:H * 64]
        P.tt(tmp, src, src, ALU.mult, [stok], ["wk3_%d" % b])
        P.add("vector", lambda e: e.tensor_reduce(out=sm[b][:, 0:H], in_=tmp.rearrange("p (h d) -> p h d", h=H),
                                                  axis=AX.X, op=ALU.add), ["wk3_%d" % b], ["sm%d" % b])
        P.act(sm[b][:, 0:H], sm[b][:, 0:H], AF.Sqrt, ["sm%d" % b], ["sm%d" % b], scale=1.0 / 64, bias=EPS)
        P.add("vector", lambda e: e.reciprocal(out=sm[b][:, 0:H], in_=sm[b][:, 0:H]), ["sm%d" % b], ["sm%d" % b])
        ov = outw.rearrange("p (h d) -> p h d", h=H)
        P.tt(ov, v, sm[b][:, 0:H].unsqueeze(2).to_broadcast([128, H, 64]), ALU.mult, [stok, "sm%d" % b], [otok])
        P.tt(ov, ov, gB[:, :].unsqueeze(1).to_broadcast([128, H, 64]), ALU.mult, [otok, gtok], [otok])

    for i in range(NT):
        b = i % 2
        j = 1 if i < CTXT else 0
        rs = slice(i * 128, (i + 1) * 128)
        P.dma(xt[b][:], x[rs, :], (), ["xt%d" % b])
        P.dma(cs[b][:, 0, :], cosd[rs, :], (), ["cs%d" % b])
        P.dma(cs[b][:, 1, :], sind[rs, :], (), ["cs%d" % b])
        emit_norm_T(P, nc, xt[b][:], "xt%d" % b, i, scal[j], bias[j], identf, hT[b], "hT%d" % b, "A", ptr, sq, ss, xh)
        pgt, pk = proj(i, 0)
        P.act(wk[0][b][:], pgt[:], AF.Silu, [pk], ["wk0_%d" % b])
        P.ts(ob[0][b][:], wk[0][b][:], 128.0 ** -0.5, ALU.mult, ["wk0_%d" % b], ["ob0_%d" % b], eng="gpsimd")
        P.dma(QS[rs, :], ob[0][b][:], ["ob0_%d" % b], ["QS"])
        for (g, KKo, LFo) in ((1, KKf, LFf), (2, KKb, LFb)):
            pgt, pk = proj(i, g)
            P.act(wk[0][b][:], pgt[:], AF.Sigmoid, [pk], ["wk0_%d" % b])
            P.tt(wk[1][b][:], wk[0][b][:], omlbB[:], ALU.mult, ["wk0_%d" % b, "omlbB"], ["wk1_%d" % b])
            P.tt(ob[1][b][:], omlbB[:], wk[1][b][:], ALU.subtract, ["wk1_%d" % b, "omlbB"], ["ob1_%d" % b], eng="gpsimd")
            P.stt(wk[2][b][:], wk[1][b][:], TINY, lbB[:], ALU.max, ALU.add, ["wk1_%d" % b, "lbB"], ["wk2_%d" % b])
            P.act(wk[2][b][:], wk[2][b][:], AF.Ln, ["wk2_%d" % b], ["wk2_%d" % b])
            P.dma(KKo[rs, :], ob[1][b][:], ["ob1_%d" % b], ["KK"])
            P.dma(LFo[rs, :], wk[2][b][:], ["wk2_%d" % b], ["LF"])
        pgt, pk = proj(i, 3)
        P.cp(ob[2][b][:], pgt[:], [pk], ["ob2_%d" % b])
        P.dma(VH[rs, :], ob[2][b][:], ["ob2_%d" % b], ["VH"])
        pgt, pk = proj(i, 4)
        P.act(wk[0][b][:], pgt[:], AF.Silu, [pk], ["wk0_%d" % b])
        P.dma(OG[rs, :], wk[0][b][:], ["wk0_%d" % b], ["OG"])
        pgt, pk = proj(i, 5)
        P.act(wk[0][b][:], pgt[:], AF.Copy, [pk], ["wk0_%d" % b])
        qknorm(wk[0][b][:], "wk0_%d" % b, 8, gq, "gq", b, wk[1][b][:], "wk1_%d" % b)
        rope(wk[1][b][:], "wk1_%d" % b, ob[0][b][:], "ob0_%d" % b, 8, b)
        P.dma(QA[rs, :], ob[0][b][:], ["ob0_%d" % b], ["QA"])
        pgt, pk = proj(i, 6)
        P.act(wk[0][b][:], pgt[:], AF.Copy, [pk], ["wk0_%d" % b])
        rope(wk[0][b][:], "wk0_%d" % b, ob[1][b][:], "ob1_%d" % b, 8, b)
        P.dma(QW[rs, :], ob[1][b][:], ["ob1_%d" % b], ["QW"])
        pgt, pk = proj(i, 7)
        P.act(wk[0][b][:], pgt[:], AF.Copy, [pk], ["wk0_%d" % b])
        qknorm(wk[0][b][:, 0:128], "wk0_%d" % b, 2, gk, "gk", b, wk[0][b][:, 0:128], "wk0_%d" % b)
        rope(wk[0][b][:, 0:256], "wk0_%d" % b, ob[2][b][:, 0:256], "ob2_%d" % b, 4, b)
        P.cp(ob[2][b][:, 256:512], wk[0][b][:, 256:512], ["wk0_%d" % b], ["ob2_%d" % b], eng="gpsimd")
        P.dma(KV4[rs, :], ob[2][b][:], ["ob2_%d" % b], ["KV4"])
    P.final_waits("sync")
    P.emit()
    return nc


def hgrn_consts():
    i = np.arange(128)
    ch = i // 64
    blk = i // 32
    out = []
    u = i[:, None]
    t = i[None, :]
    same_ch = (ch[:, None] == ch[None, :])
    r = (blk * 32 + 16)[None, :]
    Mdq = (((u > r) & (u <= t)).astype(np.float32) - ((u > t) & (u <= r)).astype(np.float32))
    Mcq = (same_ch & (u <= t)).astype(np.float32)
    cs = (ch * 64)[None, :]
    Moq_full = ((u >= cs + 32) & (u <= t) & same_ch).astype(np.float32)
    Mok_full = ((u > t) & (u <= cs + 31) & same_ch).astype(np.float32)
    second = np.concatenate([np.arange(32, 64), np.arange(96, 128)])
    first = np.concatenate([np.arange(0, 32), np.arange(64, 96)])
    Mend = (same_ch & (u > t)).astype(np.float32)
    Mdiag = ((blk[:, None] == blk[None, :]) & (u <= t)).astype(np.float32)
    Moff = (same_ch & ((i % 64) < 32)[:, None] & ((i % 64) >= 32)[None, :]).astype(np.float32)
    MCf = np.concatenate([Mdq, Mcq, -Mdq, Moq_full[:, second], Mok_full[:, first]], 1)
    out.append(dict(MC=MCf, Mend=Mend, Mdiag=Mdiag, Moff=Moff))
    fl = lambda M: M[::-1, ::-1].copy()
    Mdq_b, Mcq_b = fl(Mdq), fl(Mcq)
    Moq_b, Mok_b = fl(Moq_full), fl(Mok_full)
    MCb = np.concatenate([Mdq_b, Mcq_b, -Mdq_b, Moq_b[:, first], Mok_b[:, second]], 1)
    out.append(dict(MC=MCb, Mend=fl(Mend), Mdiag=fl(Mdiag), Moff=fl(Moff)))
    return out


def build_PB(NL, parts=(1, 1, 1)):
    NTB = CTXT + NL
    NTOK = NTB * 128
    nc = bass.Bass("TRN2", target_bir_lowering=False)
    dt_in = lambda n, s, d=F32: nc.dram_tensor(n, list(s), d, kind="ExternalInput").ap()
    dt_out = lambda n, s, d: nc.dram_tensor(n, list(s), d, kind="ExternalOutput").ap()
    qs = dt_in("qs", [NTOK, 128], BF16)
    kk = [dt_in("kk%d" % d, [NTOK, 128], BF16) for d in range(2)]
    lf = [dt_in("lf%d" % d, [NTOK, 128]) for d in range(2)]
    vh = dt_in("vh", [NTOK, 128], BF16)
    og = dt_in("og", [NTOK, 128])
    hg = dt_in("hg", [128])
    MCd = [dt_in("MC%d" % d, [128, 512]) for d in range(2)]
    Mendd = [dt_in("Mend%d" % d, [128, 128]) for d in range(2)]
    Mdiagd = [dt_in("Mdiag%d" % d, [128, 128]) for d in range(2)]
    Moffd = [dt_in("Moff%d" % d, [128, 128]) for d in range(2)]
    qa = dt_in("qa", [NTOK, 128], BF16)
    ka = dt_in("ka", [NTOK, 128], BF16)
    va = dt_in("va", [NTOK, 64], BF16)
    qw = dt_in("qw", [NTOK, 128], BF16)
    kw = dt_in("kw", [NTOK, 128], BF16)
    vw = dt_in("vw", [NTOK, 64], BF16)
    sink = dt_in("sink", [2])
    wm = dt_in("wm", [2, 128, 128])
    Ao = dt_out("A", [NTOK, 128], BF16)
    Bo = dt_out("B", [NTOK, 128], BF16)
    Co = dt_out("C", [NTOK, 128], BF16)

    P = Prog(nc)
    identf = P.ident("identf", F32)
    identb = P.sb("identb", [128, 128], BF16)
    P.cp(identb[:], identf[:], ["identf"], ["identb"])

    MC = [P.sb("MCs%d" % d, [128, 512], F32) for d in range(2)]
    Mend = [P.sb("Mends%d" % d, [128, 128], F32) for d in range(2)]
    Mdiag = [P.sb("Mdiags%d" % d, [128, 128], F32) for d in range(2)]
    Moff = [P.sb("Moffs%d" % d, [128, 128], F32) for d in range(2)]
    for d in range(2):
        P.dma(MC[d][:], MCd[d][:, :], (), ["consts"])
        P.dma(Mend[d][:], Mendd[d][:, :], (), ["consts"])
        P.dma(Mdiag[d][:], Mdiagd[d][:, :], (), ["consts"])
        P.dma(Moff[d][:], Moffd[d][:, :], (), ["consts"])
    hgB = P.sb("hgB", [128, 128], F32)
    P.dma(hgB[:], hg.partition_broadcast(128), (), ["consts"])
    Oacc = P.sb("Oacc", [128, NTB, 128], F32)
    Sm = P.sb("Sm", [128, 128], F32)
    Sb = [P.sb("Sb%d" % i, [128, 128], BF16) for i in range(2)]
    lft = [P.sb("lft%d" % i, [128, 128], F32) for i in range(3)]
    kkt = [P.sb("kkt%d" % i, [128, 128], BF16) for i in range(3)]
    qst = [P.sb("qst%d" % i, [128, 128], BF16) for i in range(3)]
    vt = [P.sb("vt%d" % i, [128, 128], BF16) for i in range(3)]
    ogt = [P.sb("ogt%d" % i, [128, 128], F32) for i in range(3)]
    qkT = [P.sb("qkT%d" % i, [128, 2, 128], BF16) for i in range(3)]
    E = [P.sb("E%d" % i, [128, 512], F32) for i in range(3)]
    E2 = [P.sb("E2%d" % i, [128, 128], F32) for i in range(3)]
    Kt = [P.sb("Kt%d" % i, [128, 128], BF16) for i in range(3)]
    QC = [P.sb("QC%d" % i, [128, 6, 128], BF16) for i in range(3)]
    Pm = [P.sb("Pm%d" % i, [128, 2, 128], BF16) for i in range(3)]
    tot = [P.sb("tot%d" % i, [128, 128], F32) for i in range(3)]
    Usb = [P.sb("Usb%d" % i, [128, 2, 128], F32) for i in range(3)]
    hs = [P.sb("hs%d" % i, [128, 2], F32) for i in range(3)]
    ao = [P.sb("ao%d" % i, [128, 128], BF16) for i in range(3)]
    for i in range(3):
        P.add("gpsimd", lambda e, i=i: e.memset(QC[i][:], 0.0), (), ["QC%d" % i])
    pbb = P.ps("pbb", [128, 8, 128], BF16)
    pall = P.ps("pall", [128, 7, 512])
    pbk = [pall[:, i, :] for i in range(7)]
    p_tr = pbb[:, 0:2, :]
    p_ex = pbk[0]
    p_e2 = pbk[1][:, 0:128]
    p_sc = pbk[2][:, 0:256].rearrange("p (c j) -> p c j", c=2)
    p_u = [pbk[3][:, 0:128], pbk[5][:, 0:128]]
    putok = ["p_u", "p_u1"]
    p_o = pbk[4][:, 0:128]
    P.add("vector", lambda e: e.memset(Sm[:], 0.0), (), ["Sm"])

    cnt = [0]

    def hgrn_A(ti, d, b):
        B = str(b)
        rs = slice(ti * 128, (ti + 1) * 128)
        P.dma(lft[b][:], lf[d][rs, :], (), ["lft" + B])
        P.dma(kkt[b][:], kk[d][rs, :], (), ["kkt" + B])
        P.dma(qst[b][:], qs[rs, :], (), ["qst" + B])
        P.dma(vt[b][:], vh[rs, :], (), ["vt" + B])
        P.tr(p_tr[:, 0, :], qst[b][:], identb[:], ["qst" + B, "identb"], ["p_tr"])
        P.tr(p_tr[:, 1, :], kkt[b][:], identb[:], ["kkt" + B, "identb"], ["p_tr"])
        P.cp(qkT[b][:], p_tr, ["p_tr"], ["qkT" + B])
        P.mm(p_ex, lft[b][:], MC[d][:], True, True, ["lft" + B, "consts"], ["p_ex"])
        P.act(E[b][:], p_ex, AF.Exp, ["p_ex"], ["E" + B])
        qT = qkT[b][:, 0, :]
        kT = qkT[b][:, 1, :]
        r = ["qkT" + B, "E" + B]
        w = ["QC" + B]
        P.tt(QC[b][:, 0, :], qT, E[b][:, 0:128], ALU.mult, r, w)
        P.tt(QC[b][:, 1, 0:64], qT[:, 0:64], E[b][:, 128:192], ALU.mult, r, w)
        P.tt(QC[b][:, 2, 64:128], qT[:, 64:128], E[b][:, 192:256], ALU.mult, r, w)
        P.tt(QC[b][:, 3, :], kT, E[b][:, 256:384], ALU.mult, r, w, eng="gpsimd")
        qa_sl, ka_sl = (slice(32, 64), slice(0, 32)) if d == 0 else (slice(0, 32), slice(32, 64))
        v3 = lambda ap: ap.rearrange("p (c j) -> p c j", c=2)
        P.tt(v3(QC[b][:, 4, :])[:, :, qa_sl], v3(qT)[:, :, qa_sl], E[b][:, 384:448].rearrange("p (c j) -> p c j", c=2),
             ALU.mult, r, w, eng="gpsimd")
        P.tt(v3(QC[b][:, 5, :])[:, :, ka_sl], v3(kT)[:, :, ka_sl], E[b][:, 448:512].rearrange("p (c j) -> p c j", c=2),
             ALU.mult, r, w, eng="gpsimd")
        P.mm(p_e2, Mend[d][:], lft[b][:], True, True, ["lft" + B, "consts"], ["p_e2"])
        P.act(E2[b][:], p_e2, AF.Exp, ["p_e2"], ["E2" + B])
        P.tt(Kt[b][:], kkt[b][:], E2[b][:], ALU.mult, ["kkt" + B, "E2" + B], ["Kt" + B])
        P.mm(p_sc[:, 0, :], QC[b][:, 3, :], QC[b][:, 0, :], True, True, ["QC" + B], ["p_sc"])
        P.mm(p_sc[:, 1, :], QC[b][:, 5, :], QC[b][:, 4, :], True, True, ["QC" + B], ["p_sc"])
        P.tt(Pm[b][:, 0, :], p_sc[:, 0, :], Mdiag[d][:], ALU.mult, ["p_sc", "consts"], ["Pm" + B])
        P.tt(Pm[b][:, 1, :], p_sc[:, 1, :], Moff[d][:], ALU.mult, ["p_sc", "consts"], ["Pm" + B])
        for c in range(2):
            P.mm(p_u[c], Kt[b][64 * c:64 * c + 64, :], vt[b][64 * c:64 * c + 64, :], True, True,
                 ["Kt" + B, "vt" + B], [putok[c]])
        P.cp(Usb[b][:, 0, :], p_u[0], [putok[0]], ["Usb" + B])
        P.cp(Usb[b][:, 1, :], p_u[1], [putok[1]], ["Usb" + B])

    def hgrn_B(ti, d, b, reset):
        B = str(b)
        rs = slice(ti * 128, (ti + 1) * 128)
        if reset:
            P.add("vector", lambda e: e.memset(Sm[:], 0.0), (), ["Sm"])
        order = (0, 1) if d == 0 else (1, 0)
        for c in order:
            dcol = 128 + 64 * c + (63 if d == 0 else 0)
            P.cp(Sb[c][:], Sm[:], ["Sm"], ["Sb%d" % c], eng="scalar")
            P.stt(Sm[:], Sm[:], E[b][:, dcol:dcol + 1], Usb[b][:, c, :], ALU.mult, ALU.add, ["Sm", "E" + B, "Usb" + B], ["Sm"])
        P.mm(p_o, Pm[b][:, 0, :], vt[b][:], True, False, ["Pm" + B, "vt" + B], ["p_o"])
        P.mm(p_o, Pm[b][:, 1, :], vt[b][:], False, False, ["Pm" + B, "vt" + B], ["p_o"])
        P.mm(p_o, QC[b][:, 1, :], Sb[0][:], False, False, ["QC" + B, "Sb0"], ["p_o"])
        P.mm(p_o, QC[b][:, 2, :], Sb[1][:], False, True, ["QC" + B, "Sb1"], ["p_o"])
        if d == 0:
            P.cp(Oacc[:, ti, :], p_o, ["p_o"], ["Oacc%d" % ti])
        else:
            P.dma(ogt[b][:], og[rs, :], (), ["ogt" + B])
            P.tt(tot[b][:], p_o, Oacc[:, ti, :], ALU.add, ["p_o", "Oacc%d" % ti], ["tot" + B])
            P.act(E2[b][:], tot[b][:], AF.Square, ["tot" + B], ["E2" + B, "hs" + B], accum=hs[b][:, 0:1])
            P.act(hs[b][:, 0:1], hs[b][:, 0:1], AF.Sqrt, ["hs" + B], ["hs" + B], scale=1.0 / 128, bias=EPS)
            P.add("vector", lambda e: e.reciprocal(out=hs[b][:, 0:1], in_=hs[b][:, 0:1]), ["hs" + B], ["hs" + B])
            P.stt(tot[b][:], tot[b][:], hs[b][:, 0:1], hgB[:], ALU.mult, ALU.mult, ["tot" + B, "hs" + B, "consts"], ["tot" + B])
            P.tt(ao[b][:], tot[b][:], ogt[b][:], ALU.mult, ["tot" + B, "ogt" + B], ["ao" + B])
            P.dma(Ao[rs, :], ao[b][:], ["ao" + B], ["Ao"])

    fwd_order = list(range(NTB))
    bwd_order = [1, 0] + list(range(NTB - 1, CTXT - 1, -1))
    if parts[0]:
        for d, order in ((0, fwd_order), (1, bwd_order)):
            base = cnt[0]
            for n in range(min(2, len(order))):
                hgrn_A(order[n], d, (base + n) % 3)
            for n, ti in enumerate(order):
                if n + 2 < len(order):
                    hgrn_A(order[n + 2], d, (base + n + 2) % 3)
                hgrn_B(ti, d, (base + n) % 3, n == 0)
            cnt[0] = base + len(order)

    QT2 = P.sb("QT2", [128, NTOK], BF16)
    KT2 = P.sb("KT2", [128, NTOK], BF16)
    Vx = P.sb("Vx", [128, NTB, 72], BF16)
    ld = [P.sb("ld%d" % i, [128, 8, 128], BF16) for i in range(2)]
    PT = [P.sb("PT%d" % i, [128, 2, 512], BF16) for i in range(3)]
    OT = [P.sb("OT%d" % i, [65, 512], F32) for i in range(2)]
    bo = [P.sb("bo%d" % i, [128, 4, 128], BF16) for i in range(2)]
    rec = [P.sb("rec%d" % i, [128, 1], F32) for i in range(2)]
    wmt = P.sb("wmt", [128, 2, 128], F32)
    P.dma(wmt[:], wm.rearrange("m k q -> k m q"), (), ["consts2"])
    esink = P.sb("esink", [128, 2], F32)
    P.dma(esink[:], sink.partition_broadcast(128), (), ["esink"])
    P.act(esink[:], esink[:], AF.Exp, ["esink"], ["esink"])
    p_s = [pall[:, 0:2, :], pall[:, 2:4, :]]
    p_ot = [pbk[4], pbk[5]]
    p_f = pbk[6]
    pstok = [["p_ex", "p_e2"], ["p_sc", "p_u"]]
    pottok = ["p_o", "p_u1"]
    st = dict(ld=0, pt=0, s=0, ot=0, bo=0, rec=0)

    def load_T(src, dstT, dtok):
        for t0 in range(0, NTB, 8):
            n = min(8, NTB - t0)
            b = st["ld"] % 2
            st["ld"] += 1
            P.dma(ld[b][:, 0:n, :], src[t0 * 128:(t0 + n) * 128, :].rearrange("(t p) c -> p t c", p=128), (), ["ld%d" % b])
            for k in range(n):
                P.tr(pbb[:, k, :], ld[b][:, k, :], identb[:], ["ld%d" % b, "identb"], ["p_tr"])
            P.cp(dstT[:, t0 * 128:(t0 + n) * 128], pbb[:, 0:n, :].rearrange("p t c -> p (t c)"), ["p_tr"], [dtok])

    def attn_pass(qsrc, ksrc, vsrc, outd, window):
        load_T(qsrc, QT2, "QT2")
        load_T(ksrc, KT2, "KT2")
        P.add("gpsimd", lambda e: e.memset(Vx[:, :, 64:65], 1.0), (), ["Vx"])
        for v0 in range(0, NTB, 32):
            vn = min(32, NTB - v0)
            P.dma(Vx[:, v0:v0 + vn, 0:64], vsrc[v0 * 128:(v0 + vn) * 128, :].rearrange("(t p) c -> p t c", p=128), (), ["Vx"])
        if window:
            groups = [(t, 1) for t in range(NTB)]
        else:
            groups = [(0, CTXT)] + [(t, min(4, NTB - t)) for t in range(CTXT, NTB, 4)]
        for (t0, nt) in groups:
            nq = nt * 128
            q0 = t0 * 128
            if t0 < CTXT:
                kbs = [(kb, None) for kb in range(CTXT)]
            elif window:
                kbs = [(kb, None) for kb in range(CTXT)]
                if t0 - 1 >= CTXT:
                    kbs.append((t0 - 1, 0))
                kbs.append((t0, None))
                if t0 + 1 < NTB:
                    kbs.append((t0 + 1, 1))
            else:
                kbs = [(kb, None) for kb in range(NTB)]
            gb = st["bo"] % 2
            st["bo"] += 1
            for e_ in range(2):
                hp = slice(64 * e_, 64 * e_ + 64)
                ob_ = st["ot"] % 2
                st["ot"] += 1
                steps = []
                i_ = 0
                while i_ < len(kbs):
                    if (not window) and i_ + 1 < len(kbs):
                        steps.append([kbs[i_], kbs[i_ + 1]])
                        i_ += 2
                    else:
                        steps.append([kbs[i_]])
                        i_ += 1
                nsteps = len(steps)

                def emit_S(n):
                    sb_ = st["s"] % 2
                    st["s"] += 1
                    for u, (kb, msk) in enumerate(steps[n]):
                        P.mm(p_s[sb_][:, u, 0:nq], KT2[hp, kb * 128:(kb + 1) * 128], QT2[hp, q0:q0 + nq], True, True,
                             ["KT2", "QT2"], pstok[sb_])
                    return sb_

                def emit_rest(n, sb_):
                    pb_ = st["pt"] % 3
                    st["pt"] += 1
                    nu = len(steps[n])
                    P.act(PT[pb_][:, 0:nu, 0:nq], p_s[sb_][:, 0:nu, 0:nq], AF.Exp, pstok[sb_], ["PT%d" % pb_], scale=0.125)
                    for u, (kb, msk) in enumerate(steps[n]):
                        if msk is not None:
                            P.tt(PT[pb_][:, u, 0:nq], PT[pb_][:, u, 0:nq], wmt[:, msk, :], ALU.mult, ["PT%d" % pb_, "consts2"],
                                 ["PT%d" % pb_], eng="gpsimd")
                        P.mm(p_ot[ob_][0:65, 0:nq], Vx[:, kb, 0:65], PT[pb_][:, u, 0:nq], n == 0 and u == 0,
                             n == nsteps - 1 and u == nu - 1, ["Vx", "PT%d" % pb_], [pottok[ob_]])

                LA = 1
                pend = [emit_S(n) for n in range(min(LA, nsteps))]
                for n in range(nsteps):
                    if n + LA < nsteps:
                        pend.append(emit_S(n + LA))
                    emit_rest(n, pend.pop(0))
                P.cp(OT[ob_][:, 0:nq], p_ot[ob_][0:65, 0:nq], [pottok[ob_]], ["OT%d" % ob_])
                for k in range(nt):
                    rb = st["rec"] % 2
                    st["rec"] += 1
                    P.tr(p_f[:, 0:65], OT[ob_][:, k * 128:(k + 1) * 128], identf[0:65, 0:65], ["OT%d" % ob_, "identf"], ["p_f6"])
                    if window:
                        P.ts(rec[rb][:], p_f[:, 64:65], esink[:, e_:e_ + 1], ALU.add, ["p_f6", "esink"], ["rec%d" % rb])
                        P.add("vector", lambda e, rb=rb: e.reciprocal(out=rec[rb][:], in_=rec[rb][:]), ["rec%d" % rb], ["rec%d" % rb])
                    else:
                        P.add("vector", lambda e, rb=rb: e.reciprocal(out=rec[rb][:], in_=p_f[:, 64:65]), ["p_f6"], ["rec%d" % rb])
                    P.ts(bo[gb][:, k, hp], p_f[:, 0:64], rec[rb][:, 0:1], ALU.mult, ["p_f6", "rec%d" % rb], ["bo%d" % gb])
            P.dma(outd[q0:q0 + nq, :].rearrange("(t p) c -> p t c", p=128), bo[gb][:, 0:nt, :], ["bo%d" % gb], ["outd"])

    if parts[1]:
        attn_pass(qa, ka, va, Bo, False)
    if parts[2]:
        attn_pass(qw, kw, vw, Co, True)
    P.final_waits("sync")
    P.emit()
    return nc


def barrier(P):
    for e in ENGS:
        need = {}
        for o in ENGS:
            if o != e and P.ccnt[o]:
                k, v = P._ev_sem(("c", o, P.ccnt[o]))
                need[k] = v
            n = P.dcnt[o]
            for i in range(max(0, n - DMA_SLOTS), n):
                k, v = P._ev_sem(("d", o, i))
                need[k] = max(need.get(k, 0), v)
        need = {k: v for k, v in need.items() if P.seen[e].get(k, 0) < v}
        for k, v in need.items():
            P.seen[e][k] = v
        P.ops[e].append((None, sorted(need.items(), key=str), None, False))


def build_PC(NT, NEXP=32):
    NTOK = NT * 128
    nc = bass.Bass("TRN2", target_bir_lowering=False)
    dt_in = lambda n, s, d=F32: nc.dram_tensor(n, list(s), d, kind="ExternalInput").ap()
    dt_out = lambda n, s, d: nc.dram_tensor(n, list(s), d, kind="ExternalOutput").ap()
    x = dt_in("x", [NTOK, D])
    brd = [dt_in(n, [NTOK, 512], BF16) for n in ("A", "B", "C")]
    cc = dt_in("cc", [D, 2])
    wmod = dt_in("wmod", [D, 6144])
    bmod = dt_in("bmod", [6144])
    n1g = dt_in("n1g", [D])
    n2g = dt_in("n2g", [D])
    fng = dt_in("fng", [D])
    wgt = dt_in("wgt", [D, 3072])
    wbr = [dt_in(n, [512, D]) for n in ("wba", "wbb", "wbc")]
    wout = dt_in("wout", [D, D])
    wr = dt_in("wr", [D, 36])
    wg = dt_in("wg", [NEXP, D, 512])
    wu = dt_in("wu", [NEXP, D, 512])
    wd = dt_in("wd", [NEXP, 512, D])
    Xn = dt_out("Xn", [NTOK, D], F32)
    Yn = dt_out("Yn", [NTOK, D], F32)
    X1 = nc.dram_tensor("X1s", [NTOK, D], F32, kind="Internal").ap()
    H2T = nc.dram_tensor("H2Ts", [128, 8, NTOK], BF16, kind="Internal").ap()
    WR = nc.dram_tensor("WRs", [NTOK, 32], F32, kind="Internal").ap()

    P = Prog(nc)
    identf = P.ident("identf", F32)
    identb = P.sb("identb", [128, 128], BF16)
    P.cp(identb[:], identf[:], ["identf"], ["identb"])
    one1 = P.sb("one1", [1, 128], F32)
    P.add("vector", lambda e: e.memset(one1[:], 1.0), (), ["one1"])
    arena = P.sb("arena", [128, 45056], BF16)
    Wgt = arena[:, 0:24576].rearrange("p (c n) -> p c n", c=8)
    Wbr = arena[:, 24576:36864].rearrange("p (b c n) -> p b c n", b=3, c=4)
    Wout = arena[:, 36864:45056].rearrange("p (c n) -> p c n", c=8)
    wrb = P.sb("wrb", [128, 8, 36], BF16)
    wst = [P.sb("wstC%d" % i, [128, 4, 512], F32) for i in range(2)]
    wsc = [0]

    def load_cast(dst, src_ap, dtok, eng=None):
        P.dma(dst, src_ap, (), [dtok], q="gpsimd")

    for g in range(6):
        for h in range(2):
            load_cast(Wgt[:, 4 * h:4 * h + 4, g * 512:(g + 1) * 512],
                      wgt[512 * h:512 * h + 512, g * 512:(g + 1) * 512].rearrange("(c p) n -> p c n", p=128), "Wgt")
    for bi in range(3):
        for h in range(2):
            load_cast(Wbr[:, bi, :, h * 512:(h + 1) * 512], wbr[bi][:, h * 512:(h + 1) * 512].rearrange("(c p) n -> p c n", p=128), "Wbr")
    for g in range(2):
        for h in range(2):
            load_cast(Wout[:, 4 * h:4 * h + 4, g * 512:(g + 1) * 512],
                      wout[512 * h:512 * h + 512, g * 512:(g + 1) * 512].rearrange("(c p) n -> p c n", p=128), "Wout")
    wrs = P.sb("wrs", [128, 8, 36], F32)
    P.dma(wrs[:], wr.rearrange("(c p) n -> p c n", p=128), (), ["wrs"])
    P.cp(wrb[:], wrs[:], ["wrs"], ["wrb"])

    pbk = [P.ps("pbk%d" % i, [128, 512]) for i in range(7)]
    pbb = P.ps("pbb", [128, 8, 128], BF16)
    scT = P.sb("scT", [128, 8, 2], F32)
    P.dma(scT[:], cc.rearrange("(c p) j -> p c j", p=128), (), ["scT"])
    P.act(scT[:], scT[:], AF.Silu, ["scT"], ["scT"])
    rowg = P.sb("rowg", [1, 512], F32)
    browg = P.sb("browg", [1, 512], F32)
    growg = P.sb("growg", [1, 512], F32)
    scal = [[P.sb("scal%d_%d" % (k, j), [128, 8], F32) for j in range(2)] for k in range(2)]
    bias = [[P.sb("bias%d_%d" % (k, j), [128, 8], F32) for j in range(2)] for k in range(2)]
    gaB = [[P.sb("gaB%d_%d" % (k, j), [128, D], F32) for j in range(2)] for k in range(2)]
    one11 = one1[:, 0:1]
    ngs = (n1g, n2g)
    for g in range(12):
        v, hf = g // 2, g % 2
        k, kind = v // 3, v % 3
        P.dma(browg[:], bmod[g * 512:(g + 1) * 512].rearrange("(o n) -> o n", o=1), (), ["browg"])
        if kind == 1:
            P.dma(growg[:], ngs[k][hf * 512:(hf + 1) * 512].rearrange("(o n) -> o n", o=1), (), ["growg"])
        for j in range(2):
            for h in range(2):
                b_ = wsc[0] % 2
                wsc[0] += 1
                P.dma(wst[b_][:], wmod[512 * h:512 * h + 512, g * 512:(g + 1) * 512].rearrange("(c p) n -> p c n", p=128),
                      (), ["wstC%d" % b_])
                for c in range(4):
                    P.mm(pbk[0][0:1, :], scT[:, 4 * h + c, j:j + 1], wst[b_][:, c, :], h == 0 and c == 0, h == 1 and c == 3,
                         ["wstC%d" % b_, "scT"], ["pb0"])
            P.tt(rowg[:], pbk[0][0:1, :], browg[:], ALU.add, ["pb0", "browg"], ["rowg"])
            if kind == 1:
                P.stt(rowg[:], rowg[:], 1.0, growg[:], ALU.add, ALU.mult, ["rowg", "growg"], ["rowg"])
            if kind < 2:
                for c in range(4):
                    P.mm(pbk[1][:, c:c + 1], rowg[:, c * 128:(c + 1) * 128], one11, True, True, ["rowg", "one1"], ["pb1"])
                dstc = (bias if kind == 0 else scal)[k][j]
                P.cp(dstc[:, hf * 4:hf * 4 + 4], pbk[1][:, 0:4], ["pb1"], ["modcols"])
            else:
                P.mm(pbk[1][:, :], one1[:, :], rowg[:], True, True, ["rowg", "one1"], ["pb1"])
                P.cp(gaB[k][j][:, hf * 512:(hf + 1) * 512], pbk[1][:, :], ["pb1"], ["gaB"])
    fngB = P.sb("fngB", [128, D], F32)
    P.dma(fngB[:], fng.partition_broadcast(128), (), ["fngB"])

    xt = P.sb("xt", [128, D], F32)
    ss = [P.sb("ss%d" % i, [128, 1], F32) for i in range(2)]
    xh = [P.sb("xh0", [128, D], F32)] * 2
    hT = [P.sb("hT%d" % i, [128, 8, 128], BF16) for i in range(2)]
    ptr = [pbk[2].rearrange("p (c j) -> p c j", c=4), pbk[3].rearrange("p (c j) -> p c j", c=4)]
    G = P.sb("G", [128, D], F32)
    brt = [P.sb("brt%d" % i, [128, 512], BF16) for i in range(2)]
    brT = [P.sb("brT%d" % i, [128, 4, 128], BF16) for i in range(2)]
    m = P.sb("m", [128, D], F32)
    tmp = P.sb("tmp", [128, D], F32)
    sq = [tmp, tmp]
    mT = P.sb("mT", [128, 8, 128], BF16)
    x1 = P.sb("x1", [128, D], F32)
    Lr = P.sb("Lr", [128, 36], F32)
    Lm = P.sb("Lm", [128, 32], F32)
    k1 = P.sb("k1", [128, 32], F32)
    k2 = P.sb("k2", [128, 32], F32)
    Wt = P.sb("Wt", [128, 32], F32)
    r8 = P.sb("r8", [128, 8], F32)
    g4 = P.sb("g4", [128, 4], F32)
    pen = P.sb("pen", [128, 4], F32)
    mmc = [0]

    def bank():
        k = 4 + (mmc[0] % 3)
        mmc[0] += 1
        return pbk[k], "pb%d" % k

    def norm_T(i, xin, xtok, k, j, hTt, hTtok):
        P.act(sq[0][:], xin, AF.Square, [xtok], ["tmp", "Css"], accum=ss[0][:])
        P.act(ss[0][:], ss[0][:], AF.Sqrt, ["Css"], ["Css"], scale=1.0 / D, bias=EPS)
        P.add("vector", lambda e: e.reciprocal(out=ss[0][:], in_=ss[0][:]), ["Css"], ["Css"])
        P.ts(xh[0][:], xin, ss[0][:, 0:1], ALU.mult, [xtok, "Css"], ["Cxh"])
        for half in range(2):
            ptk = "pb%d" % (2 + half)
            for c4 in range(4):
                c = half * 4 + c4
                P.tr(ptr[half][:, c4, :], xh[0][:, c * 128:(c + 1) * 128], identf[:], ["Cxh", "identf"], [ptk])
            for c4 in range(4):
                c = half * 4 + c4
                P.act(hTt[:, c, :], ptr[half][:, c4, :], AF.Identity, [ptk, "modcols"], [hTtok],
                      scale=scal[k][j][:, c:c + 1], bias=bias[k][j][:, c:c + 1])

    for i in range(NT):
        j = 1 if i < CTXT else 0
        rs = slice(i * 128, (i + 1) * 128)
        P.dma(xt[:], x[rs, :], (), ["xt"])
        norm_T(i, xt[:], "xt", 0, j, hT[0], "hT0")
        for bi in range(3):
            bb = bi % 2
            P.dma(brt[bb][:], brd[bi][rs, :], (), ["brt%d" % bb])
            for c in range(4):
                P.tr(pbb[:, c, :], brt[bb][:, c * 128:(c + 1) * 128], identb[:], ["brt%d" % bb, "identb"], ["pbb"])
            P.cp(brT[bb][:], pbb[:, 0:4, :], ["pbb"], ["brT%d" % bb])
            for hf in range(2):
                pk, tk = bank()
                for c in range(8):
                    P.mm(pk[:], hT[0][:, c, :], Wgt[:, c, bi * 1024 + hf * 512:bi * 1024 + (hf + 1) * 512], c == 0, c == 7,
                         ["hT0", "Wgt"], [tk])
                P.act(G[:, hf * 512:(hf + 1) * 512], pk[:], AF.Sigmoid, [tk], ["G"])
            for hf in range(2):
                pk, tk = bank()
                for c in range(4):
                    P.mm(pk[:], brT[bb][:, c, :], Wbr[:, bi, c, hf * 512:(hf + 1) * 512], c == 0, c == 3,
                         ["brT%d" % bb, "Wbr"], [tk])
                hs_ = slice(hf * 512, (hf + 1) * 512)
                if bi == 0:
                    P.tt(m[:, hs_], pk[:], G[:, hs_], ALU.mult, [tk, "G"], ["m"])
                else:
                    P.tt(tmp[:, hs_], pk[:], G[:, hs_], ALU.mult, [tk, "G"], ["tmp"])
                    P.tt(m[:, hs_], m[:, hs_], tmp[:, hs_], ALU.add, ["m", "tmp"], ["m"], eng="gpsimd")
        for half in range(2):
            ptk = "pb%d" % (2 + half)
            for c4 in range(4):
                c = half * 4 + c4
                P.tr(ptr[half][:, c4, :], m[:, c * 128:(c + 1) * 128], identf[:], ["m", "identf"], [ptk])
            P.cp(mT[:, half * 4:half * 4 + 4, :], ptr[half][:, :, :], [ptk], ["mT"], eng="scalar")
        for hf in range(2):
            pk, tk = bank()
            hs_ = slice(hf * 512, (hf + 1) * 512)
            for c in range(8):
                P.mm(pk[:], mT[:, c, :], Wout[:, c, hs_], c == 0, c == 7, ["mT", "Wout"], [tk])
            P.tt(x1[:, hs_], pk[:], gaB[0][j][:, hs_], ALU.mult, [tk, "gaB"], ["x1"])
            P.tt(x1[:, hs_], x1[:, hs_], xt[:, hs_], ALU.add, ["x1", "xt"], ["x1"], eng="gpsimd")
        P.dma(X1[rs, :], x1[:], ["x1"], ["X1s"])
        norm_T(i, x1[:], "x1", 1, j, hT[1], "hT1")
        P.dma(H2T[:, :, rs], hT[1][:], ["hT1"], ["H2Ts"])
        pk, tk = bank()
        for c in range(8):
            P.mm(pk[:, 0:36], hT[1][:, c, :], wrb[:, c, :], c == 0, c == 7, ["hT1", "wrb"], [tk])
        P.cp(Lr[:], pk[:, 0:36], [tk], ["Lr"])
        R_ = ["Lr", "r8", "g4", "pen", "Lm", "k1", "k2", "Wt"]
        P.add("vector", lambda e: e.tensor_reduce(out=r8[:, 0:1], in_=Lr[:, 0:4], axis=AX.X, op=ALU.max), R_, R_)
        P.ts(g4[:], Lr[:, 0:4], r8[:, 0:1], ALU.is_ge, R_, R_)
        P.ts(r8[:, 1:2], r8[:, 0:1], -1.0, ALU.mult, R_, R_)
        P.act(pen[:], Lr[:, 0:4], AF.Exp, R_, R_, bias=r8[:, 1:2], accum=r8[:, 2:3])
        P.add("vector", lambda e: e.reciprocal(out=r8[:, 2:3], in_=r8[:, 2:3]), R_, R_)
        P.ts(pen[:], g4[:], -1.0, ALU.add, R_, R_, s2=1e30, op1=ALU.mult)
        P.tt(Lm[:].rearrange("p (g j) -> p g j", g=4), Lr[:, 4:36].rearrange("p (g j) -> p g j", g=4),
             pen[:, :].unsqueeze(2).to_broadcast([128, 4, 8]), ALU.add, R_, R_)
        P.add("vector", lambda e: e.tensor_reduce(out=r8[:, 3:4], in_=Lm[:], axis=AX.X, op=ALU.max), R_, R_)
        P.ts(k1[:], Lm[:], r8[:, 3:4], ALU.is_ge, R_, R_)
        P.stt(Lm[:], k1[:], -1e30, Lm[:], ALU.mult, ALU.add, R_, R_)
        P.add("vector", lambda e: e.tensor_reduce(out=r8[:, 4:5], in_=Lm[:], axis=AX.X, op=ALU.max), R_, R_)
        P.ts(k2[:], Lm[:], r8[:, 4:5], ALU.is_ge, R_, R_)
        P.tt(r8[:, 5:6], r8[:, 4:5], r8[:, 3:4], ALU.subtract, R_, R_)
        P.act(r8[:, 5:6], r8[:, 5:6], AF.Exp, R_, R_)
        P.ts(r8[:, 6:7], r8[:, 5:6], 1.0, ALU.add, R_, R_)
        P.add("vector", lambda e: e.reciprocal(out=r8[:, 6:7], in_=r8[:, 6:7]), R_, R_)
        P.tt(r8[:, 7:8], r8[:, 5:6], r8[:, 6:7], ALU.mult, R_, R_)
        P.tt(r8[:, 6:7], r8[:, 6:7], r8[:, 2:3], ALU.mult, R_, R_)
        P.tt(r8[:, 7:8], r8[:, 7:8], r8[:, 2:3], ALU.mult, R_, R_)
        P.ts(Wt[:], k1[:], r8[:, 6:7], ALU.mult, R_, R_)
        P.stt(Wt[:], k2[:], r8[:, 7:8], Wt[:], ALU.mult, ALU.add, R_, R_)
        P.dma(WR[rs, :], Wt[:], R_, ["WRs"])

    barrier(P)
    SBT = 9
    WE = [arena[:, p * 12288:(p + 1) * 12288].rearrange("p (m c n) -> p m c n", m=3, c=8) for p in range(2)]
    h2sb = arena[:, 24576:33792].rearrange("p (c n) -> p c n", c=8)
    AT = [arena[:, 33792 + q * 2048:33792 + (q + 1) * 2048].rearrange("p (c n) -> p c n", c=4) for q in range(2)]
    ysb = P.sb("y", [128, 8, D], F32)
    ytl = [ysb[:, t, :] for t in range(8)] + [xt[:, :]]
    Wsb = P.sb("Wsb", [128, SBT, 32], F32)
    sg = [P.sb("sg%d" % i, [128, 512], F32) for i in range(2)]
    x1r = [m, tmp]
    cntm = dict(at=0, sg=0)
    for s0 in range(0, NT, SBT):
        ns = min(SBT, NT - s0)
        ntk = ns * 128
        P.dma(h2sb[:, :, 0:ntk], H2T[:, :, s0 * 128:s0 * 128 + ntk], ["H2Ts"], ["h2sb"])
        P.dma(Wsb[:, 0:ns, :], WR[s0 * 128:s0 * 128 + ntk, :].rearrange("(t p) e -> p t e", p=128), ["WRs"], ["Wsb"])
        for e_ in range(NEXP):
            p = e_ % 2
            wtok = "WE%d" % p
            for mi, src in enumerate((wg, wu)):
                for h in range(2):
                    load_cast(WE[p][:, mi, 4 * h:4 * h + 4, :], src[e_, 512 * h:512 * h + 512, :].rearrange("(c p) n -> p c n", p=128), wtok)
            for h in range(2):
                load_cast(WE[p][:, 2, :, :].rearrange("p (fc hf) n -> p fc hf n", hf=2)[:, :, h, :],
                          wd[e_, :, h * 512:(h + 1) * 512].rearrange("(c p) n -> p c n", p=128), wtok)
            for g0 in range(0, ns, 4):
                ng = min(4, ns - g0)
                n = ng * 128
                a = cntm["at"] % 2
                cntm["at"] += 1
                for fc in range(4):
                    pg_, tg = bank()
                    for c in range(8):
                        P.mm(pg_[:, 0:n], WE[p][:, 0, c, fc * 128:(fc + 1) * 128], h2sb[:, c, g0 * 128:g0 * 128 + n], c == 0, c == 7,
                             [wtok, "h2sb"], [tg])
                    pu_, tu = bank()
                    for c in range(8):
                        P.mm(pu_[:, 0:n], WE[p][:, 1, c, fc * 128:(fc + 1) * 128], h2sb[:, c, g0 * 128:g0 * 128 + n], c == 0, c == 7,
                             [wtok, "h2sb"], [tu])
                    sb_ = cntm["sg"] % 2
                    cntm["sg"] += 1
                    P.act(sg[sb_][:, 0:n], pg_[:, 0:n], AF.Silu, [tg], ["sg%d" % sb_])
                    P.tt(AT[a][:, fc, 0:n], pu_[:, 0:n], sg[sb_][:, 0:n], ALU.mult, [tu, "sg%d" % sb_], ["AT%d" % a])
                for t in range(ng):
                    ti = g0 + t
                    for hf in range(2):
                        py_, ty = bank()
                        for fc in range(4):
                            P.mm(py_[:], AT[a][:, fc, t * 128:(t + 1) * 128], WE[p][:, 2, fc * 2 + hf, :], fc == 0, fc == 3,
                                 ["AT%d" % a, wtok], [ty])
                        yv = ytl[ti][:, hf * 512:(hf + 1) * 512]
                        if e_ == 0:
                            P.ts(yv, py_[:], Wsb[:, ti, e_:e_ + 1], ALU.mult, [ty, "Wsb"], ["y%d" % ti])
                        else:
                            P.stt(yv, py_[:], Wsb[:, ti, e_:e_ + 1], yv, ALU.mult, ALU.add, [ty, "Wsb", "y%d" % ti], ["y%d" % ti])
        for t in range(ns):
            i = s0 + t
            j = 1 if i < CTXT else 0
            rs = slice(i * 128, (i + 1) * 128)
            xb = x1r[t % 2]
            xtk = "x1r%d" % (t % 2)
            P.dma(xb[:], X1[rs, :], ["X1s"], [xtk])
            P.tt(ytl[t], ytl[t], gaB[1][j][:], ALU.mult, ["y%d" % t, "gaB"], ["y%d" % t], eng="gpsimd")
            P.tt(xb[:], xb[:], ytl[t], ALU.add, [xtk, "y%d" % t], [xtk])
            P.dma(Xn[rs, :], xb[:], [xtk], ["Xn"])
            P.act(G[:], xb[:], AF.Square, [xtk], ["G", "Css"], accum=ss[0][:])
            P.act(ss[0][:], ss[0][:], AF.Sqrt, ["Css"], ["Css"], scale=1.0 / D, bias=EPS)
            P.add("vector", lambda e: e.reciprocal(out=ss[0][:], in_=ss[0][:]), ["Css"], ["Css"])
            P.stt(G[:], xb[:], ss[0][:, 0:1], fngB[:], ALU.mult, ALU.mult, [xtk, "Css", "fngB"], ["G"])
            P.dma(Yn[rs, :], G[:], ["G"], ["Yn"])
        barrier(P)
    P.final_waits("sync")
    P.emit()
    return nc


_CACHE = {}


def _rope_tables(S):
    pos = np.arange(S)
    row = (pos // 64).astype(np.float32)
    col = (pos % 64).astype(np.float32)
    inv = (10000.0 ** (-np.arange(16, dtype=np.float32) / np.float32(16))).astype(np.float32)
    ang = np.concatenate([row[:, None] * inv, col[:, None] * inv], axis=-1).astype(np.float32)
    return np.cos(ang).astype(np.float32), np.sin(ang).astype(np.float32)


def _prog(key, fn):
    if key not in _CACHE:
        _CACHE[key] = fn()
    return _CACHE[key]


def kernel(x, c, ctx, c_ctx, w_mod, b_mod, norm1_g, norm2_g, w_in, hgrn_lb_logits, hgrn_out_norm_g,
           attn_q_norm_g, attn_k_norm_g, swa_sink, w_branch_a, w_branch_b, w_branch_c, w_out,
           w_group, w_router, w_exp_gate, w_exp_up, w_exp_down, final_norm_g):
    f32 = lambda a: np.ascontiguousarray(np.asarray(a), dtype=np.float32)
    x, c, ctx, c_ctx = f32(x), f32(c), f32(ctx), f32(c_ctx)
    Bn, S, _ = x.shape
    QN = 4
    Lc = S // QN
    NT = CTXT + Lc // 128
    NL = S // 128
    depth = w_in.shape[0]
    cosL, sinL = _rope_tables(S)
    cos_c = np.ones((256, 32), np.float32)
    sin_c = np.zeros((256, 32), np.float32)
    hc = hgrn_consts()
    i = np.arange(128)
    wm = np.stack([(i[:, None] >= i[None, :]), (i[:, None] <= i[None, :])]).astype(np.float32)
    xl, xc = x, ctx
    cores = [(b, j) for b in range(Bn) for j in range(QN)]
    yout = None
    for layer in range(depth):
        xin = [np.concatenate([xc[b], xl[b, j * Lc:(j + 1) * Lc]], 0) for (b, j) in cores]
        ccs = [np.ascontiguousarray(np.stack([c[b], c_ctx], 1)) for (b, j) in cores]
        pa = _prog(("PA", NT, layer), lambda: build_PA(NT, layer))
        wmodA = f32(w_mod[layer][:, :2048])
        winA = f32(w_in[layer][:, :4096])
        ims = []
        for k, (b, j) in enumerate(cores):
            ims.append(dict(x=xin[k], cc=ccs[k], wmod=wmodA, bmod=f32(b_mod[layer][:2048]), n1g=f32(norm1_g[layer]),
                            win=winA, lbl=f32(hgrn_lb_logits), aqg=f32(attn_q_norm_g[layer]), akg=f32(attn_k_norm_g[layer]),
                            cos=np.concatenate([cos_c, cosL[j * Lc:(j + 1) * Lc]], 0),
                            sin=np.concatenate([sin_c, sinL[j * Lc:(j + 1) * Lc]], 0)))
        ra = _run(pa, ims)
        full = {}
        for nm in ("QS", "KKf", "KKb", "LFf", "LFb", "VH", "OG", "QA", "QW", "KV4"):
            full[nm] = [np.concatenate([np.asarray(ra[b * QN][nm])[:256]] +
                                       [np.asarray(ra[b * QN + j][nm])[256:] for j in range(QN)], 0) for b in range(Bn)]
        del ra
        pb = _prog(("PB", NL), lambda: build_PB(NL))
        ims = []
        for b in range(Bn):
            for hp in range(4):
                hs = slice(hp * 128, (hp + 1) * 128)
                kv = hp // 2
                ksl = lambda o: slice(o + kv * 64, o + kv * 64 + 64)
                kv4 = full["KV4"][b]
                d = dict(qs=full["QS"][b][:, hs], kk0=full["KKf"][b][:, hs], kk1=full["KKb"][b][:, hs],
                         lf0=full["LFf"][b][:, hs], lf1=full["LFb"][b][:, hs], vh=full["VH"][b][:, hs],
                         og=full["OG"][b][:, hs], hg=f32(hgrn_out_norm_g[layer]),
                         qa=full["QA"][b][:, hs], ka=np.concatenate([kv4[:, ksl(0)], kv4[:, ksl(0)]], 1), va=kv4[:, ksl(256)],
                         qw=full["QW"][b][:, hs], kw=np.concatenate([kv4[:, ksl(128)], kv4[:, ksl(128)]], 1), vw=kv4[:, ksl(384)],
                         sink=f32(swa_sink[layer][2 * hp:2 * hp + 2]), wm=wm)
                for dd in range(2):
                    for nm in ("MC", "Mend", "Mdiag", "Moff"):
                        d["%s%d" % (nm, dd)] = hc[dd][nm]
                ims.append({k_: np.ascontiguousarray(v) for k_, v in d.items()})
        del full
        rb = _run(pb, ims)
        br = {}
        for nm in ("A", "B", "C"):
            br[nm] = [np.concatenate([np.asarray(rb[b * 4 + hp][nm]) for hp in range(4)], 1) for b in range(Bn)]
        del rb
        pc = _prog(("PC", NT), lambda: build_PC(NT))
        wmodC = f32(w_mod[layer])
        shared = dict(wmod=wmodC, bmod=f32(b_mod[layer]), n1g=f32(norm1_g[layer]), n2g=f32(norm2_g[layer]),
                      fng=f32(final_norm_g), wgt=f32(w_in[layer][:, 4096:7168]), wba=f32(w_branch_a[layer]),
                      wbb=f32(w_branch_b[layer]), wbc=f32(w_branch_c[layer]), wout=f32(w_out[layer]),
                      wr=f32(np.concatenate([np.asarray(w_group[layer]), np.asarray(w_router[layer])], 1)),
                      wg=f32(w_exp_gate[layer]), wu=f32(w_exp_up[layer]), wd=f32(w_exp_down[layer]))
        ims = []
        for k, (b, j) in enumerate(cores):
            d = dict(shared)
            d["x"] = xin[k]
            d["cc"] = ccs[k]
            for nm in ("A", "B", "C"):
                d[nm] = np.ascontiguousarray(np.concatenate([br[nm][b][:256], br[nm][b][256 + j * Lc:256 + (j + 1) * Lc]], 0))
            ims.append(d)
        rc = _run(pc, ims)
        xl = np.stack([np.concatenate([np.asarray(rc[b * QN + j]["Xn"])[256:] for j in range(QN)], 0) for b in range(Bn)])
        xc = np.stack([np.asarray(rc[b * QN]["Xn"])[:256] for b in range(Bn)])
        if layer == depth - 1:
            yout = np.stack([np.concatenate([np.asarray(rc[b * QN + j]["Yn"])[256:] for j in range(QN)], 0) for b in range(Bn)])
        del rc
    return yout.astype(np.float32)
```

```python
import numpy as np
import ml_dtypes
import concourse.bass as bass
import concourse.mybir as mybir
from concourse.bass_utils import run_bass_kernel_spmd

F32 = mybir.dt.float32
BF16 = mybir.dt.bfloat16
AF = mybir.ActivationFunctionType
ALU = mybir.AluOpType
AX = mybir.AxisListType
NPBF = ml_dtypes.bfloat16

ENGS = ["sync", "scalar", "vector", "gpsimd", "tensor"]
EPOCH = 12000
DMA_SLOTS = 6
DMA_EPOCH = 700

D = 1024
CTXT = 2
EPS = 1e-6
TINY = 1e-30


class Prog:
    def __init__(self, nc):
        self.nc = nc
        self.ops = {e: [] for e in ENGS}
        self.ccnt = {e: 0 for e in ENGS}
        self.dcnt = {e: 0 for e in ENGS}
        self.tok_w = {}
        self.tok_r = {}
        self.seen = {e: {} for e in ENGS}
        self.sems = {}
        self.nsem = 0
        self.uid = 0

    def _sem(self, key):
        if key not in self.sems:
            self.sems[key] = self.nc.alloc_semaphore(name="s%d" % self.nsem)
            self.nsem += 1
        return self.sems[key]

    def _ev_sem(self, ev):
        kind, eng, idx = ev
        if kind == "c":
            ep = (idx - 1) // EPOCH
            return ("c", eng, ep), idx - ep * EPOCH
        slot = idx % DMA_SLOTS
        use = idx // DMA_SLOTS
        ep = use // DMA_EPOCH
        return ("d", eng, slot, ep), 16 * (use - ep * DMA_EPOCH + 1)

    def add(self, eng, fn, reads=(), writes=(), dma=False):
        deps = set()
        for t in reads:
            w = self.tok_w.get(t)
            if w is not None:
                deps.add(w)
        for t in writes:
            w = self.tok_w.get(t)
            if w is not None:
                deps.add(w)
            for r in self.tok_r.get(t, ()):
                deps.add(r)
        if dma:
            i = self.dcnt[eng]
            self.dcnt[eng] += 1
            ev = ("d", eng, i)
            if i >= DMA_SLOTS:
                deps.add(("d", eng, i - DMA_SLOTS))
        else:
            self.ccnt[eng] += 1
            ev = ("c", eng, self.ccnt[eng])
        need = {}
        for d in deps:
            if d == ev:
                continue
            if d[0] == "c" and d[1] == eng == "tensor" and not dma:
                continue
            k, v = self._ev_sem(d)
            if self.seen[eng].get(k, 0) >= v:
                continue
            if need.get(k, 0) < v:
                need[k] = v
        for k, v in need.items():
            self.seen[eng][k] = v
        self.ops[eng].append((fn, sorted(need.items(), key=str), self._ev_sem(ev), dma))
        for t in reads:
            self.tok_r.setdefault(t, []).append(ev)
        for t in writes:
            self.tok_w[t] = ev
            self.tok_r[t] = []
        return ev

    def final_waits(self, eng="sync"):
        need = {}
        for e in ENGS:
            if self.ccnt[e] and e != eng:
                k, v = self._ev_sem(("c", e, self.ccnt[e]))
                need[k] = v
            n = self.dcnt[e]
            for i in range(max(0, n - DMA_SLOTS), n):
                k, v = self._ev_sem(("d", e, i))
                need[k] = max(need.get(k, 0), v)
        self.ops[eng].append((None, sorted(need.items(), key=str), None, False))

    def emit(self):
        for e in ENGS:
            for (fn, waits, inc, dma) in self.ops[e]:
                for k, v in waits:
                    self._sem(k)
                if inc is not None:
                    self._sem(inc[0])
        with self.nc.Block() as block:
            for e in ENGS:
                if not self.ops[e]:
                    continue

                def body(engh, e=e):
                    for (fn, waits, inc, dma) in self.ops[e]:
                        for k, v in waits:
                            engh.wait_ge(self.sems[k], v)
                        if fn is None:
                            continue
                        ins = fn(engh)
                        ins.then_inc(self.sems[inc[0]], 16 if dma else 1)

                getattr(block, e)(body)

    def sb(self, name, shape, dt):
        return self.nc.alloc_sbuf_tensor(name, list(shape), dt)

    def ps(self, name, shape, dt=F32):
        return self.nc.alloc_psum_tensor(name, list(shape), dt)

    def dma(self, out, in_, r, w, q=None):
        if q is None:
            q = "sync" if (self.dcnt["sync"] <= self.dcnt["gpsimd"]) else "gpsimd"
        self.add(q, lambda e: e.dma_start(out=out, in_=in_), r, w, dma=True)

    def act(self, out, in_, func, r, w, scale=1.0, bias=0.0, accum=None):
        if accum is None:
            self.add("scalar", lambda e: e.activation(out=out, in_=in_, func=func, scale=scale, bias=bias), r, w)
        else:
            self.add("scalar", lambda e: e.activation(out=out, in_=in_, func=func, scale=scale, bias=bias,
                                                      accum_out=accum), r, w)

    def tt(self, out, a, b, op, r, w, eng="vector"):
        self.add(eng, lambda e: e.tensor_tensor(out=out, in0=a, in1=b, op=op), r, w)

    def ts(self, out, a, s1, op0, r, w, s2=None, op1=None, eng="vector"):
        if op1 is None:
            self.add(eng, lambda e: e.tensor_scalar(out=out, in0=a, scalar1=s1, scalar2=None, op0=op0), r, w)
        else:
            self.add(eng, lambda e: e.tensor_scalar(out=out, in0=a, scalar1=s1, scalar2=s2, op0=op0, op1=op1), r, w)

    def stt(self, out, a, s, b, op0, op1, r, w):
        self.add("vector", lambda e: e.scalar_tensor_tensor(out=out, in0=a, scalar=s, in1=b, op0=op0, op1=op1), r, w)

    def cp(self, out, in_, r, w, eng="vector"):
        if eng == "scalar":
            self.add(eng, lambda e: e.activation(out=out, in_=in_, func=AF.Copy), r, w)
        else:
            self.add(eng, lambda e: e.tensor_copy(out=out, in_=in_), r, w)

    def mm(self, out, lhsT, rhs, start, stop, r, w):
        self.add("tensor", lambda e: e.matmul(out, lhsT=lhsT, rhs=rhs, start=start, stop=stop), r, w)

    def tr(self, out, in_, ident, r, w):
        self.add("tensor", lambda e: e.transpose(out=out, in_=in_, identity=ident), r, w)

    def ident(self, name, dt):
        t = self.sb(name, [128, 128], dt)
        self.add("gpsimd", lambda e: e.memset(t[:], 0.0), (), [name])
        self.add("gpsimd", lambda e: e.affine_select(out=t[:], in_=t[:], compare_op=ALU.not_equal, fill=1.0,
                                                     base=0, pattern=[[-1, 128]], channel_multiplier=1), [name], [name])
        return t


def _run(nc, in_maps):
    res = run_bass_kernel_spmd(nc, in_maps, core_ids=list(range(len(in_maps))))
    return res.results


def emit_mod(P, nc, cc, wmod, bmod, ncols, pfx, wst, wtoks):
    scT = P.sb(pfx + "scT", [128, 8, 2], F32)
    P.dma(scT[:], cc.rearrange("(c p) j -> p c j", p=128), (), ["scT"])
    P.act(scT[:], scT[:], AF.Silu, ["scT"], ["scT"])
    rows = P.sb(pfx + "rows", [1, 2, ncols], F32)
    brow = P.sb(pfx + "brow", [1, ncols], F32)
    P.dma(brow[:], bmod.rearrange("(o n) -> o n", o=1), (), ["brow"])
    pm = P.ps(pfx + "pm", [1, 2, 512])
    for g in range(ncols // 512):
        w = wst[g % 2]
        wt = wtoks[g % 2]
        P.dma(w[:], wmod[:, g * 512:(g + 1) * 512].rearrange("(c p) n -> p c n", p=128), (), [wt])
        for j in range(2):
            for c in range(8):
                P.mm(pm[:, j, :], scT[:, c, j:j + 1], w[:, c, :], c == 0, c == 7, [wt, "scT"], ["pm"])
        for j in range(2):
            P.tt(rows[:, j, g * 512:(g + 1) * 512], pm[:, j, :], brow[:, g * 512:(g + 1) * 512], ALU.add,
                 ["pm", "brow"], ["modrow"])
    return rows


def row_to_cols(P, nc, row_ap, n, cols_out, one11, pcol, r, w):
    k = n // 128
    for c in range(k):
        P.mm(pcol[:, c:c + 1], row_ap[:, c * 128:(c + 1) * 128], one11, True, True, r, ["pcol"])
    P.cp(cols_out, pcol[:, 0:k], ["pcol"], w)


def row_bcast(P, nc, row_ap, n, out_tile, ones1, pb, r, w):
    for g in range(0, n, 512):
        m = min(512, n - g)
        P.mm(pb[:, 0:m], ones1, row_ap[:, g:g + m], True, True, r, ["pb"])
        P.cp(out_tile[:, g:g + m], pb[:, 0:m], ["pb"], w)


def emit_norm_T(P, nc, xt, xtok, i, scal, bias, ident, hT, hTtok, pfx, ptr, sq, ss, xh):
    b = i % 2
    P.act(sq[b][:], xt, AF.Square, [xtok], [pfx + "sq%d" % b, pfx + "ss%d" % b], accum=ss[b][:])
    P.act(ss[b][:], ss[b][:], AF.Sqrt, [pfx + "ss%d" % b], [pfx + "ss%d" % b], scale=1.0 / D, bias=EPS)
    P.add("vector", lambda e: e.reciprocal(out=ss[b][:], in_=ss[b][:]), [pfx + "ss%d" % b], [pfx + "ss%d" % b])
    P.ts(xh[b][:], xt, ss[b][:, 0:1], ALU.mult, [xtok, pfx + "ss%d" % b], [pfx + "xh%d" % b])
    for half in range(2):
        pt = ptr[half]
        ptk = pfx + "ptr%d" % half
        for c4 in range(4):
            c = half * 4 + c4
            P.tr(pt[:, c4, :], xh[b][:, c * 128:(c + 1) * 128], ident[:], [pfx + "xh%d" % b, "identf"], [ptk])
        for c4 in range(4):
            c = half * 4 + c4
            P.act(hT[:, c, :], pt[:, c4, :], AF.Identity, [ptk, "modcols"], [hTtok],
                  scale=scal[:, c:c + 1], bias=bias[:, c:c + 1])


def build_PA(NT, layer):
    NTOK = NT * 128
    nc = bass.Bass("TRN2", target_bir_lowering=False)
    dt_in = lambda n, s, d=F32: nc.dram_tensor(n, list(s), d, kind="ExternalInput").ap()
    dt_out = lambda n, s, d: nc.dram_tensor(n, list(s), d, kind="ExternalOutput").ap()
    x = dt_in("x", [NTOK, D])
    cc = dt_in("cc", [D, 2])
    wmod = dt_in("wmod", [D, 2048])
    bmod = dt_in("bmod", [2048])
    n1g = dt_in("n1g", [D])
    win = dt_in("win", [D, 4096])
    lbl = dt_in("lbl", [2, 512])
    aqg = dt_in("aqg", [64])
    akg = dt_in("akg", [64])
    cosd = dt_in("cos", [NTOK, 32])
    sind = dt_in("sin", [NTOK, 32])
    QS = dt_out("QS", [NTOK, 512], BF16)
    KKf = dt_out("KKf", [NTOK, 512], BF16)
    KKb = dt_out("KKb", [NTOK, 512], BF16)
    LFf = dt_out("LFf", [NTOK, 512], F32)
    LFb = dt_out("LFb", [NTOK, 512], F32)
    VH = dt_out("VH", [NTOK, 512], BF16)
    OG = dt_out("OG", [NTOK, 512], F32)
    QA = dt_out("QA", [NTOK, 512], BF16)
    QW = dt_out("QW", [NTOK, 512], BF16)
    KV4 = dt_out("KV4", [NTOK, 512], BF16)

    P = Prog(nc)
    identf = P.ident("identf", F32)
    one1 = P.sb("one1", [1, 128], F32)
    P.add("vector", lambda e: e.memset(one1[:], 1.0), (), ["one1"])
    Wb = P.sb("Wb", [128, 8, 4096], BF16)
    wst = [P.sb("wstA%d" % i, [128, 8, 512], F32) for i in range(2)]
    src_groups = [(0, 512), (512, 512), (1024, 512), (1536, 512), (2048, 512), (2560, 512), (3328, 512),
                  (3072, 128), (3840, 128), (3200, 128), (3968, 128)]
    dst = 0
    for gi, (s0, n) in enumerate(src_groups):
        P.dma(Wb[:, :, dst:dst + n], win[:, s0:s0 + n].rearrange("(c p) n -> p c n", p=128), (), ["Wb"], q="gpsimd")
        dst += n
    rows = emit_mod(P, nc, cc, wmod, bmod, 2048, "A", wst, ["wstA0", "wstA1"])
    g1row = P.sb("g1row", [1, D], F32)
    P.dma(g1row[:], n1g.rearrange("(o n) -> o n", o=1), (), ["g1row"])
    scrow = P.sb("scrow", [1, 2, D], F32)
    for j in range(2):
        P.stt(scrow[:, j, :], rows[:, j, 1024:2048], 1.0, g1row[:], ALU.add, ALU.mult, ["modrow", "g1row"], ["scrow"])
    pcol = P.ps("pcol", [128, 16])
    one11 = one1[:, 0:1]
    scal = [P.sb("scal%d" % j, [128, 8], F32) for j in range(2)]
    bias = [P.sb("bias%d" % j, [128, 8], F32) for j in range(2)]
    for j in range(2):
        row_to_cols(P, nc, scrow[:, j, :], D, scal[j][:], one11, pcol, ["scrow", "one1"], ["modcols"])
        row_to_cols(P, nc, rows[:, j, 0:1024], D, bias[j][:], one11, pcol, ["modrow", "one1"], ["modcols"])
    lbB = P.sb("lbB", [128, 512], F32)
    omlbB = P.sb("omlbB", [128, 512], F32)
    e0 = P.sb("e0", [128, 512], F32)
    e1 = P.sb("e1", [128, 512], F32)
    P.dma(e0[:], lbl[0, :].partition_broadcast(128), (), ["e0"])
    P.dma(e1[:], lbl[1, :].partition_broadcast(128), (), ["e1"])
    P.act(e0[:], e0[:], AF.Exp, ["e0"], ["e0"])
    P.act(e1[:], e1[:], AF.Exp, ["e1"], ["e1"])
    P.tt(lbB[:], e0[:], e1[:], ALU.add, ["e0", "e1"], ["lbB"])
    P.add("vector", lambda e: e.reciprocal(out=lbB[:], in_=lbB[:]), ["lbB"], ["lbB"])
    P.tt(e0[:], e0[:], lbB[:], ALU.mult, ["e0", "lbB"], ["e0"])
    P.tt(e1[:], e1[:], lbB[:], ALU.mult, ["e1", "lbB"], ["e1"])
    if layer == 0:
        P.tt(lbB[:], e0[:], e0[:], ALU.subtract, ["e0"], ["lbB"])
    else:
        P.tt(lbB[:], e0[:], e1[:], ALU.add, ["e0", "e1"], ["lbB"])
        P.tt(lbB[:], lbB[:], e0[:], ALU.subtract, ["lbB", "e0"], ["lbB"])
    P.ts(omlbB[:], lbB[:], -1.0, ALU.mult, ["lbB"], ["omlbB"], s2=1.0, op1=ALU.add)
    gq = P.sb("gq", [128, 64], F32)
    gk = P.sb("gk", [128, 64], F32)
    P.dma(gq[:], aqg.partition_broadcast(128), (), ["gq"])
    P.dma(gk[:], akg.partition_broadcast(128), (), ["gk"])

    xt = [P.sb("xt%d" % i, [128, D], F32) for i in range(2)]
    sq = [P.sb("sq%d" % i, [128, D], F32) for i in range(2)]
    ss = [P.sb("ss%d" % i, [128, 1], F32) for i in range(2)]
    xh = [P.sb("xh%d" % i, [128, D], F32) for i in range(2)]
    hT = [P.sb("hT%d" % i, [128, 8, 128], BF16) for i in range(2)]
    cs = [P.sb("cs%d" % i, [128, 2, 32], F32) for i in range(2)]
    ptr = [P.ps("ptr%d" % i, [128, 4, 128]) for i in range(2)]
    pg = [P.ps("pg%d" % i, [128, 512]) for i in range(3)]
    NW = 6
    wk = [[P.sb("wk%d_%d" % (k, i), [128, 512], F32) for i in range(2)] for k in range(NW)]
    ob = [[P.sb("ob%d_%d" % (k, i), [128, 512], BF16) for i in range(2)] for k in range(3)]
    sm = [P.sb("sm%d" % i, [128, 8], F32) for i in range(2)]
    gcount = [0]

    def proj(i, g):
        k = gcount[0] % 3
        gcount[0] += 1
        for c in range(8):
            P.mm(pg[k][:], hT[i % 2][:, c, :], Wb[:, c, g * 512:(g + 1) * 512], c == 0, c == 7,
                 ["hT%d" % (i % 2), "Wb"], ["pg%d" % k])
        return pg[k], "pg%d" % k

    def rope(src, stok, dst_, dtok, H, b, eng="gpsimd"):
        sv = src.rearrange("p (h two d) -> p h two d", h=H, two=2)
        dv = dst_.rearrange("p (h two d) -> p h two d", h=H, two=2)
        cB = cs[b][:, 0, :].unsqueeze(1).to_broadcast([128, H, 32])
        sB = cs[b][:, 1, :].unsqueeze(1).to_broadcast([128, H, 32])
        t1 = wk[4][b][:, 0:H * 32].rearrange("p (h d) -> p h d", h=H)
        t2 = wk[5][b][:, 0:H * 32].rearrange("p (h d) -> p h d", h=H)
        a, t = "wk4_%d" % b, "wk5_%d" % b
        ctk = "cs%d" % b
        P.tt(t1, sv[:, :, 0, :], cB, ALU.mult, [stok, ctk], [a], eng=eng)
        P.tt(t2, sv[:, :, 1, :], sB, ALU.mult, [stok, ctk], [t], eng=eng)
        P.tt(dv[:, :, 0, :], t1, t2, ALU.subtract, [a, t], [dtok], eng=eng)
        P.tt(t1, sv[:, :, 1, :], cB, ALU.mult, [stok, ctk], [a], eng=eng)
        P.tt(t2, sv[:, :, 0, :], sB, ALU.mult, [stok, ctk], [t], eng=eng)
        P.tt(dv[:, :, 1, :], t1, t2, ALU.add, [a, t], [dtok], eng=eng)

    def qknorm(src, stok, H, gB, gtok, b, outw, otok):
        v = src.rearrange("p (h d) -> p h d", h=H)
        tmp = wk[3][b][:, 0:H * 64]
        P.tt(tmp, src, src, ALU.mult, [stok], ["wk3_%d" % b])
        P.add("vector", lambda e: e.tensor_reduce(out=sm[b][:, 0:H], in_=tmp.rearrange("p (h d) -> p h d", h=H),
                                                  axis=AX.X, op=ALU.add), ["wk3_%d" % b], ["sm%d" % b])
        P.act(sm[b][:, 0:H], sm[b][:, 0:H], AF.Sqrt, ["sm%d" % b], ["sm%d" % b], scale=1.0 / 64, bias=EPS)
        P.add("vector", lambda e: e.reciprocal(out=sm[b][:, 0:H], in_=sm[b][:, 0:H]), ["sm%d" % b], ["sm%d" % b])
        ov = outw.rearrange("p (h d) -> p h d", h=H)
        P.tt(ov, v, sm[b][:, 0:H].unsqueeze(2).to_broadcast([128, H, 64]), ALU.mult, [stok, "sm%d" % b], [otok])
        P.tt(ov, ov, gB[:, :].unsqueeze(1).to_broadcast([128, H, 64]), ALU.mult, [otok, gtok], [otok])

    for i in range(NT):
        b = i % 2
        j = 1 if i < CTXT else 0
        rs = slice(i * 128, (i + 1) * 128)
        P.dma(xt[b][:], x[rs, :], (), ["xt%d" % b])
        P.dma(cs[b][:, 0, :], cosd[rs, :], (), ["cs%d" % b])
        P.dma(cs[b][:, 1, :], sind[rs, :], (), ["cs%d" % b])
        emit_norm_T(P, nc, xt[b][:], "xt%d" % b, i, scal[j], bias[j], identf, hT[b], "hT%d" % b, "A", ptr, sq, ss, xh)
        pgt, pk = proj(i, 0)
        P.act(wk[0][b][:], pgt[:], AF.Silu, [pk], ["wk0_%d" % b])
        P.ts(ob[0][b][:], wk[0][b][:], 128.0 ** -0.5, ALU.mult, ["wk0_%d" % b], ["ob0_%d" % b], eng="gpsimd")
        P.dma(QS[rs, :], ob[0][b][:], ["ob0_%d" % b], ["QS"])
        for (g, KKo, LFo) in ((1, KKf, LFf), (2, KKb, LFb)):
            pgt, pk = proj(i, g)
            P.act(wk[0][b][:], pgt[:], AF.Sigmoid, [pk], ["wk0_%d" % b])
            P.tt(wk[1][b][:], wk[0][b][:], omlbB[:], ALU.mult, ["wk0_%d" % b, "omlbB"], ["wk1_%d" % b])
            P.tt(ob[1][b][:], omlbB[:], wk[1][b][:], ALU.subtract, ["wk1_%d" % b, "omlbB"], ["ob1_%d" % b], eng="gpsimd")
            P.stt(wk[2][b][:], wk[1][b][:], TINY, lbB[:], ALU.max, ALU.add, ["wk1_%d" % b, "lbB"], ["wk2_%d" % b])
            P.act(wk[2][b][:], wk[2][b][:], AF.Ln, ["wk2_%d" % b], ["wk2_%d" % b])
            P.dma(KKo[rs, :], ob[1][b][:], ["ob1_%d" % b], ["KK"])
            P.dma(LFo[rs, :], wk[2][b][:], ["wk2_%d" % b], ["LF"])
        pgt, pk = proj(i, 3)
        P.cp(ob[2][b][:], pgt[:], [pk], ["ob2_%d" % b])
        P.dma(VH[rs, :], ob[2][b][:], ["ob2_%d" % b], ["VH"])
        pgt, pk = proj(i, 4)
        P.act(wk[0][b][:], pgt[:], AF.Silu, [pk], ["wk0_%d" % b])
        P.dma(OG[rs, :], wk[0][b][:], ["wk0_%d" % b], ["OG"])
        pgt, pk = proj(i, 5)
        P.act(wk[0][b][:], pgt[:], AF.Copy, [pk], ["wk0_%d" % b])
        qknorm(wk[0][b][:], "wk0_%d" % b, 8, gq, "gq", b, wk[1][b][:], "wk1_%d" % b)
        rope(wk[1][b][:], "wk1_%d" % b, ob[0][b][:], "ob0_%d" % b, 8, b)
        P.dma(QA[rs, :], ob[0][b][:], ["ob0_%d" % b], ["QA"])
        pgt, pk = proj(i, 6)
        P.act(wk[0][b][:], pgt[:], AF.Copy, [pk], ["wk0_%d" % b])
        rope(wk[0][b][:], "wk0_%d" % b, ob[1][b][:], "ob1_%d" % b, 8, b)
        P.dma(QW[rs, :], ob[1][b][:], ["ob1_%d" % b], ["QW"])
        pgt, pk = proj(i, 7)
        P.act(wk[0][b][:], pgt[:], AF.Copy, [pk], ["wk0_%d" % b])
        qknorm(wk[0][b][:, 0:128], "wk0_%d" % b, 2, gk, "gk", b, wk[0][b][:, 0:128], "wk0_%d" % b)
        rope(wk[0][b][:, 0:256], "wk0_%d" % b, ob[2][b][:, 0:256], "ob2_%d" % b, 4, b)
        P.cp(ob[2][b][:, 256:512], wk[0][b][:, 256:512], ["wk0_%d" % b], ["ob2_%d" % b], eng="gpsimd")
        P.dma(KV4[rs, :], ob[2][b][:], ["ob2_%d" % b], ["KV4"])
    P.final_waits("sync")
    P.emit()
    return nc


def hgrn_consts():
    i = np.arange(128)
    ch = i // 64
    blk = i // 32
    out = []
    u = i[:, None]
    t = i[None, :]
    same_ch = (ch[:, None] == ch[None, :])
    r = (blk * 32 + 16)[None, :]
    Mdq = (((u > r) & (u <= t)).astype(np.float32) - ((u > t) & (u <= r)).astype(np.float32))
    Mcq = (same_ch & (u <= t)).astype(np.float32)
    cs = (ch * 64)[None, :]
    Moq_full = ((u >= cs + 32) & (u <= t) & same_ch).astype(np.float32)
    Mok_full = ((u > t) & (u <= cs + 31) & same_ch).astype(np.float32)
    second = np.concatenate([np.arange(32, 64), np.arange(96, 128)])
    first = np.concatenate([np.arange(0, 32), np.arange(64, 96)])
    Mend = (same_ch & (u > t)).astype(np.float32)
    Mdiag = ((blk[:, None] == blk[None, :]) & (u <= t)).astype(np.float32)
    Moff = (same_ch & ((i % 64) < 32)[:, None] & ((i % 64) >= 32)[None, :]).astype(np.float32)
    MCf = np.concatenate([Mdq, Mcq, -Mdq, Moq_full[:, second], Mok_full[:, first]], 1)
    out.append(dict(MC=MCf, Mend=Mend, Mdiag=Mdiag, Moff=Moff))
    fl = lambda M: M[::-1, ::-1].copy()
    Mdq_b, Mcq_b = fl(Mdq), fl(Mcq)
    Moq_b, Mok_b = fl(Moq_full), fl(Mok_full)
    MCb = np.concatenate([Mdq_b, Mcq_b, -Mdq_b, Moq_b[:, first], Mok_b[:, second]], 1)
    out.append(dict(MC=MCb, Mend=fl(Mend), Mdiag=fl(Mdiag), Moff=fl(Moff)))
    return out


def build_PB(NL, parts=(1, 1, 1)):
    NTB = CTXT + NL
    NTOK = NTB * 128
    nc = bass.Bass("TRN2", target_bir_lowering=False)
    dt_in = lambda n, s, d=F32: nc.dram_tensor(n, list(s), d, kind="ExternalInput").ap()
    dt_out = lambda n, s, d: nc.dram_tensor(n, list(s), d, kind="ExternalOutput").ap()
    qs = dt_in("qs", [NTOK, 128], BF16)
    kk = [dt_in("kk%d" % d, [NTOK, 128], BF16) for d in range(2)]
    lf = [dt_in("lf%d" % d, [NTOK, 128]) for d in range(2)]
    vh = dt_in("vh", [NTOK, 128], BF16)
    og = dt_in("og", [NTOK, 128])
    hg = dt_in("hg", [128])
    MCd = [dt_in("MC%d" % d, [128, 512]) for d in range(2)]
    Mendd = [dt_in("Mend%d" % d, [128, 128]) for d in range(2)]
    Mdiagd = [dt_in("Mdiag%d" % d, [128, 128]) for d in range(2)]
    Moffd = [dt_in("Moff%d" % d, [128, 128]) for d in range(2)]
    qa = dt_in("qa", [NTOK, 128], BF16)
    ka = dt_in("ka", [NTOK, 128], BF16)
    va = dt_in("va", [NTOK, 64], BF16)
    qw = dt_in("qw", [NTOK, 128], BF16)
    kw = dt_in("kw", [NTOK, 128], BF16)
    vw = dt_in("vw", [NTOK, 64], BF16)
    sink = dt_in("sink", [2])
    wm = dt_in("wm", [2, 128, 128])
    Ao = dt_out("A", [NTOK, 128], BF16)
    Bo = dt_out("B", [NTOK, 128], BF16)
    Co = dt_out("C", [NTOK, 128], BF16)

    P = Prog(nc)
    identf = P.ident("identf", F32)
    identb = P.sb("identb", [128, 128], BF16)
    P.cp(identb[:], identf[:], ["identf"], ["identb"])

    MC = [P.sb("MCs%d" % d, [128, 512], F32) for d in range(2)]
    Mend = [P.sb("Mends%d" % d, [128, 128], F32) for d in range(2)]
    Mdiag = [P.sb("Mdiags%d" % d, [128, 128], F32) for d in range(2)]
    Moff = [P.sb("Moffs%d" % d, [128, 128], F32) for d in range(2)]
    for d in range(2):
        P.dma(MC[d][:], MCd[d][:, :], (), ["consts"])
        P.dma(Mend[d][:], Mendd[d][:, :], (), ["consts"])
        P.dma(Mdiag[d][:], Mdiagd[d][:, :], (), ["consts"])
        P.dma(Moff[d][:], Moffd[d][:, :], (), ["consts"])
    hgB = P.sb("hgB", [128, 128], F32)
    P.dma(hgB[:], hg.partition_broadcast(128), (), ["consts"])
    Oacc = P.sb("Oacc", [128, NTB, 128], F32)
    Sm = P.sb("Sm", [128, 128], F32)
    Sb = [P.sb("Sb%d" % i, [128, 128], BF16) for i in range(2)]
    lft = [P.sb("lft%d" % i, [128, 128], F32) for i in range(3)]
    kkt = [P.sb("kkt%d" % i, [128, 128], BF16) for i in range(3)]
    qst = [P.sb("qst%d" % i, [128, 128], BF16) for i in range(3)]
    vt = [P.sb("vt%d" % i, [128, 128], BF16) for i in range(3)]
    ogt = [P.sb("ogt%d" % i, [128, 128], F32) for i in range(3)]
    qkT = [P.sb("qkT%d" % i, [128, 2, 128], BF16) for i in range(3)]
    E = [P.sb("E%d" % i, [128, 512], F32) for i in range(3)]
    E2 = [P.sb("E2%d" % i, [128, 128], F32) for i in range(3)]
    Kt = [P.sb("Kt%d" % i, [128, 128], BF16) for i in range(3)]
    QC = [P.sb("QC%d" % i, [128, 6, 128], BF16) for i in range(3)]
    Pm = [P.sb("Pm%d" % i, [128, 2, 128], BF16) for i in range(3)]
    tot = [P.sb("tot%d" % i, [128, 128], F32) for i in range(3)]
    Usb = [P.sb("Usb%d" % i, [128, 2, 128], F32) for i in range(3)]
    hs = [P.sb("hs%d" % i, [128, 2], F32) for i in range(3)]
    ao = [P.sb("ao%d" % i, [128, 128], BF16) for i in range(3)]
    for i in range(3):
        P.add("gpsimd", lambda e, i=i: e.memset(QC[i][:], 0.0), (), ["QC%d" % i])
    pbb = P.ps("pbb", [128, 8, 128], BF16)
    pall = P.ps("pall", [128, 7, 512])
    pbk = [pall[:, i, :] for i in range(7)]
    p_tr = pbb[:, 0:2, :]
    p_ex = pbk[0]
    p_e2 = pbk[1][:, 0:128]
    p_sc = pbk[2][:, 0:256].rearrange("p (c j) -> p c j", c=2)
    p_u = [pbk[3][:, 0:128], pbk[5][:, 0:128]]
    putok = ["p_u", "p_u1"]
    p_o = pbk[4][:, 0:128]
    P.add("vector", lambda e: e.memset(Sm[:], 0.0), (), ["Sm"])

    cnt = [0]

    def hgrn_A(ti, d, b):
        B = str(b)
        rs = slice(ti * 128, (ti + 1) * 128)
        P.dma(lft[b][:], lf[d][rs, :], (), ["lft" + B])
        P.dma(kkt[b][:], kk[d][rs, :], (), ["kkt" + B])
        P.dma(qst[b][:], qs[rs, :], (), ["qst" + B])
        P.dma(vt[b][:], vh[rs, :], (), ["vt" + B])
        P.tr(p_tr[:, 0, :], qst[b][:], identb[:], ["qst" + B, "identb"], ["p_tr"])
        P.tr(p_tr[:, 1, :], kkt[b][:], identb[:], ["kkt" + B, "identb"], ["p_tr"])
        P.cp(qkT[b][:], p_tr, ["p_tr"], ["qkT" + B])
        P.mm(p_ex, lft[b][:], MC[d][:], True, True, ["lft" + B, "consts"], ["p_ex"])
        P.act(E[b][:], p_ex, AF.Exp, ["p_ex"], ["E" + B])
        qT = qkT[b][:, 0, :]
        kT = qkT[b][:, 1, :]
        r = ["qkT" + B, "E" + B]
        w = ["QC" + B]
        P.tt(QC[b][:, 0, :], qT, E[b][:, 0:128], ALU.mult, r, w)
        P.tt(QC[b][:, 1, 0:64], qT[:, 0:64], E[b][:, 128:192], ALU.mult, r, w)
        P.tt(QC[b][:, 2, 64:128], qT[:, 64:128], E[b][:, 192:256], ALU.mult, r, w)
        P.tt(QC[b][:, 3, :], kT, E[b][:, 256:384], ALU.mult, r, w, eng="gpsimd")
        qa_sl, ka_sl = (slice(32, 64), slice(0, 32)) if d == 0 else (slice(0, 32), slice(32, 64))
        v3 = lambda ap: ap.rearrange("p (c j) -> p c j", c=2)
        P.tt(v3(QC[b][:, 4, :])[:, :, qa_sl], v3(qT)[:, :, qa_sl], E[b][:, 384:448].rearrange("p (c j) -> p c j", c=2),
             ALU.mult, r, w, eng="gpsimd")
        P.tt(v3(QC[b][:, 5, :])[:, :, ka_sl], v3(kT)[:, :, ka_sl], E[b][:, 448:512].rearrange("p (c j) -> p c j", c=2),
             ALU.mult, r, w, eng="gpsimd")
        P.mm(p_e2, Mend[d][:], lft[b][:], True, True, ["lft" + B, "consts"], ["p_e2"])
        P.act(E2[b][:], p_e2, AF.Exp, ["p_e2"], ["E2" + B])
        P.tt(Kt[b][:], kkt[b][:], E2[b][:], ALU.mult, ["kkt" + B, "E2" + B], ["Kt" + B])
        P.mm(p_sc[:, 0, :], QC[b][:, 3, :], QC[b][:, 0, :], True, True, ["QC" + B], ["p_sc"])
        P.mm(p_sc[:, 1, :], QC[b][:, 5, :], QC[b][:, 4, :], True, True, ["QC" + B], ["p_sc"])
        P.tt(Pm[b][:, 0, :], p_sc[:, 0, :], Mdiag[d][:], ALU.mult, ["p_sc", "consts"], ["Pm" + B])
        P.tt(Pm[b][:, 1, :], p_sc[:, 1, :], Moff[d][:], ALU.mult, ["p_sc", "consts"], ["Pm" + B])
        for c in range(2):
            P.mm(p_u[c], Kt[b][64 * c:64 * c + 64, :], vt[b][64 * c:64 * c + 64, :], True, True,
                 ["Kt" + B, "vt" + B], [putok[c]])
        P.cp(Usb[b][:, 0, :], p_u[0], [putok[0]], ["Usb" + B])
        P.cp(Usb[b][:, 1, :], p_u[1], [putok[1]], ["Usb" + B])

    def hgrn_B(ti, d, b, reset):
        B = str(b)
        rs = slice(ti * 128, (ti + 1) * 128)
        if reset:
            P.add("vector", lambda e: e.memset(Sm[:], 0.0), (), ["Sm"])
        order = (0, 1) if d == 0 else (1, 0)
        for c in order:
            dcol = 128 + 64 * c + (63 if d == 0 else 0)
            P.cp(Sb[c][:], Sm[:], ["Sm"], ["Sb%d" % c], eng="scalar")
            P.stt(Sm[:], Sm[:], E[b][:, dcol:dcol + 1], Usb[b][:, c, :], ALU.mult, ALU.add, ["Sm", "E" + B, "Usb" + B], ["Sm"])
        P.mm(p_o, Pm[b][:, 0, :], vt[b][:], True, False, ["Pm" + B, "vt" + B], ["p_o"])
        P.mm(p_o, Pm[b][:, 1, :], vt[b][:], False, False, ["Pm" + B, "vt" + B], ["p_o"])
        P.mm(p_o, QC[b][:, 1, :], Sb[0][:], False, False, ["QC" + B, "Sb0"], ["p_o"])
        P.mm(p_o, QC[b][:, 2, :], Sb[1][:], False, True, ["QC" + B, "Sb1"], ["p_o"])
        if d == 0:
            P.cp(Oacc[:, ti, :], p_o, ["p_o"], ["Oacc%d" % ti])
        else:
            P.dma(ogt[b][:], og[rs, :], (), ["ogt" + B])
            P.tt(tot[b][:], p_o, Oacc[:, ti, :], ALU.add, ["p_o", "Oacc%d" % ti], ["tot" + B])
            P.act(E2[b][:], tot[b][:], AF.Square, ["tot" + B], ["E2" + B, "hs" + B], accum=hs[b][:, 0:1])
            P.act(hs[b][:, 0:1], hs[b][:, 0:1], AF.Sqrt, ["hs" + B], ["hs" + B], scale=1.0 / 128, bias=EPS)
            P.add("vector", lambda e: e.reciprocal(out=hs[b][:, 0:1], in_=hs[b][:, 0:1]), ["hs" + B], ["hs" + B])
            P.stt(tot[b][:], tot[b][:], hs[b][:, 0:1], hgB[:], ALU.mult, ALU.mult, ["tot" + B, "hs" + B, "consts"], ["tot" + B])
            P.tt(ao[b][:], tot[b][:], ogt[b][:], ALU.mult, ["tot" + B, "ogt" + B], ["ao" + B])
            P.dma(Ao[rs, :], ao[b][:], ["ao" + B], ["Ao"])

    fwd_order = list(range(NTB))
    bwd_order = [1, 0] + list(range(NTB - 1, CTXT - 1, -1))
    if parts[0]:
        for d, order in ((0, fwd_order), (1, bwd_order)):
            base = cnt[0]
            for n in range(min(2, len(order))):
                hgrn_A(order[n], d, (base + n) % 3)
            for n, ti in enumerate(order):
                if n + 2 < len(order):
                    hgrn_A(order[n + 2], d, (base + n + 2) % 3)
                hgrn_B(ti, d, (base + n) % 3, n == 0)
            cnt[0] = base + len(order)

    QT2 = P.sb("QT2", [128, NTOK], BF16)
    KT2 = P.sb("KT2", [128, NTOK], BF16)
    Vx = P.sb("Vx", [128, NTB, 72], BF16)
    ld = [P.sb("ld%d" % i, [128, 8, 128], BF16) for i in range(2)]
    PT = [P.sb("PT%d" % i, [128, 2, 512], BF16) for i in range(3)]
    OT = [P.sb("OT%d" % i, [65, 512], F32) for i in range(2)]
    bo = [P.sb("bo%d" % i, [128, 4, 128], BF16) for i in range(2)]
    rec = [P.sb("rec%d" % i, [128, 1], F32) for i in range(2)]
    wmt = P.sb("wmt", [128, 2, 128], F32)
    P.dma(wmt[:], wm.rearrange("m k q -> k m q"), (), ["consts2"])
    esink = P.sb("esink", [128, 2], F32)
    P.dma(esink[:], sink.partition_broadcast(128), (), ["esink"])
    P.act(esink[:], esink[:], AF.Exp, ["esink"], ["esink"])
    p_s = [pall[:, 0:2, :], pall[:, 2:4, :]]
    p_ot = [pbk[4], pbk[5]]
    p_f = pbk[6]
    pstok = [["p_ex", "p_e2"], ["p_sc", "p_u"]]
    pottok = ["p_o", "p_u1"]
    st = dict(ld=0, pt=0, s=0, ot=0, bo=0, rec=0)

    def load_T(src, dstT, dtok):
        for t0 in range(0, NTB, 8):
            n = min(8, NTB - t0)
            b = st["ld"] % 2
            st["ld"] += 1
            P.dma(ld[b][:, 0:n, :], src[t0 * 128:(t0 + n) * 128, :].rearrange("(t p) c -> p t c", p=128), (), ["ld%d" % b])
            for k in range(n):
                P.tr(pbb[:, k, :], ld[b][:, k, :], identb[:], ["ld%d" % b, "identb"], ["p_tr"])
            P.cp(dstT[:, t0 * 128:(t0 + n) * 128], pbb[:, 0:n, :].rearrange("p t c -> p (t c)"), ["p_tr"], [dtok])

    def attn_pass(qsrc, ksrc, vsrc, outd, window):
        load_T(qsrc, QT2, "QT2")
        load_T(ksrc, KT2, "KT2")
        P.add("gpsimd", lambda e: e.memset(Vx[:, :, 64:65], 1.0), (), ["Vx"])
        for v0 in range(0, NTB, 32):
            vn = min(32, NTB - v0)
            P.dma(Vx[:, v0:v0 + vn, 0:64], vsrc[v0 * 128:(v0 + vn) * 128, :].rearrange("(t p) c -> p t c", p=128), (), ["Vx"])
        if window:
            groups = [(t, 1) for t in range(NTB)]
        else:
            groups = [(0, CTXT)] + [(t, min(4, NTB - t)) for t in range(CTXT, NTB, 4)]
        for (t0, nt) in groups:
            nq = nt * 128
            q0 = t0 * 128
            if t0 < CTXT:
                kbs = [(kb, None) for kb in range(CTXT)]
            elif window:
                kbs = [(kb, None) for kb in range(CTXT)]
                if t0 - 1 >= CTXT:
                    kbs.append((t0 - 1, 0))
                kbs.append((t0, None))
                if t0 + 1 < NTB:
                    kbs.append((t0 + 1, 1))
            else:
                kbs = [(kb, None) for kb in range(NTB)]
            gb = st["bo"] % 2
            st["bo"] += 1
            for e_ in range(2):
                hp = slice(64 * e_, 64 * e_ + 64)
                ob_ = st["ot"] % 2
                st["ot"] += 1
                steps = []
                i_ = 0
                while i_ < len(kbs):
                    if (not window) and i_ + 1 < len(kbs):
                        steps.append([kbs[i_], kbs[i_ + 1]])
                        i_ += 2
                    else:
                        steps.append([kbs[i_]])
                        i_ += 1
                nsteps = len(steps)

                def emit_S(n):
                    sb_ = st["s"] % 2
                    st["s"] += 1
                    for u, (kb, msk) in enumerate(steps[n]):
                        P.mm(p_s[sb_][:, u, 0:nq], KT2[hp, kb * 128:(kb + 1) * 128], QT2[hp, q0:q0 + nq], True, True,
                             ["KT2", "QT2"], pstok[sb_])
                    return sb_

                def emit_rest(n, sb_):
                    pb_ = st["pt"] % 3
                    st["pt"] += 1
                    nu = len(steps[n])
                    P.act(PT[pb_][:, 0:nu, 0:nq], p_s[sb_][:, 0:nu, 0:nq], AF.Exp, pstok[sb_], ["PT%d" % pb_], scale=0.125)
                    for u, (kb, msk) in enumerate(steps[n]):
                        if msk is not None:
                            P.tt(PT[pb_][:, u, 0:nq], PT[pb_][:, u, 0:nq], wmt[:, msk, :], ALU.mult, ["PT%d" % pb_, "consts2"],
                                 ["PT%d" % pb_], eng="gpsimd")
                        P.mm(p_ot[ob_][0:65, 0:nq], Vx[:, kb, 0:65], PT[pb_][:, u, 0:nq], n == 0 and u == 0,
                             n == nsteps - 1 and u == nu - 1, ["Vx", "PT%d" % pb_], [pottok[ob_]])

                LA = 1
                pend = [emit_S(n) for n in range(min(LA, nsteps))]
                for n in range(nsteps):
                    if n + LA < nsteps:
                        pend.append(emit_S(n + LA))
                    emit_rest(n, pend.pop(0))
                P.cp(OT[ob_][:, 0:nq], p_ot[ob_][0:65, 0:nq], [pottok[ob_]], ["OT%d" % ob_])
                for k in range(nt):
                    rb = st["rec"] % 2
                    st["rec"] += 1
                    P.tr(p_f[:, 0:65], OT[ob_][:, k * 128:(k + 1) * 128], identf[0:65, 0:65], ["OT%d" % ob_, "identf"], ["p_f6"])
                    if window:
                        P.ts(rec[rb][:], p_f[:, 64:65], esink[:, e_:e_ + 1], ALU.add, ["p_f6", "esink"], ["rec%d" % rb])
                        P.add("vector", lambda e, rb=rb: e.reciprocal(out=rec[rb][:], in_=rec[rb][:]), ["rec%d" % rb], ["rec%d" % rb])
                    else:
                        P.add("vector", lambda e, rb=rb: e.reciprocal(out=rec[rb][:], in_=p_f[:, 64:65]), ["p_f6"], ["rec%d" % rb])
                    P.ts(bo[gb][:, k, hp], p_f[:, 0:64], rec[rb][:, 0:1], ALU.mult, ["p_f6", "rec%d" % rb], ["bo%d" % gb])
            P.dma(outd[q0:q0 + nq, :].rearrange("(t p) c -> p t c", p=128), bo[gb][:, 0:nt, :], ["bo%d" % gb], ["outd"])

    if parts[1]:
        attn_pass(qa, ka, va, Bo, False)
    if parts[2]:
        attn_pass(qw, kw, vw, Co, True)
    P.final_waits("sync")
    P.emit()
    return nc


def barrier(P):
    for e in ENGS:
        need = {}
        for o in ENGS:
            if o != e and P.ccnt[o]:
                k, v = P._ev_sem(("c", o, P.ccnt[o]))
                need[k] = v
            n = P.dcnt[o]
            for i in range(max(0, n - DMA_SLOTS), n):
                k, v = P._ev_sem(("d", o, i))
                need[k] = max(need.get(k, 0), v)
        need = {k: v for k, v in need.items() if P.seen[e].get(k, 0) < v}
        for k, v in need.items():
            P.seen[e][k] = v
        P.ops[e].append((None, sorted(need.items(), key=str), None, False))


def build_PC(NT, NEXP=32):
    NTOK = NT * 128
    nc = bass.Bass("TRN2", target_bir_lowering=False)
    dt_in = lambda n, s, d=F32: nc.dram_tensor(n, list(s), d, kind="ExternalInput").ap()
    dt_out = lambda n, s, d: nc.dram_tensor(n, list(s), d, kind="ExternalOutput").ap()
    x = dt_in("x", [NTOK, D])
    brd = [dt_in(n, [NTOK, 512], BF16) for n in ("A", "B", "C")]
    cc = dt_in("cc", [D, 2])
    wmod = dt_in("wmod", [D, 6144])
    bmod = dt_in("bmod", [6144])
    n1g = dt_in("n1g", [D])
    n2g = dt_in("n2g", [D])
    fng = dt_in("fng", [D])
    wgt = dt_in("wgt", [D, 3072])
    wbr = [dt_in(n, [512, D]) for n in ("wba", "wbb", "wbc")]
    wout = dt_in("wout", [D, D])
    wr = dt_in("wr", [D, 36])
    wg = dt_in("wg", [NEXP, D, 512])
    wu = dt_in("wu", [NEXP, D, 512])
    wd = dt_in("wd", [NEXP, 512, D])
    Xn = dt_out("Xn", [NTOK, D], F32)
    Yn = dt_out("Yn", [NTOK, D], F32)
    X1 = nc.dram_tensor("X1s", [NTOK, D], F32, kind="Internal").ap()
    H2T = nc.dram_tensor("H2Ts", [128, 8, NTOK], BF16, kind="Internal").ap()
    WR = nc.dram_tensor("WRs", [NTOK, 32], F32, kind="Internal").ap()

    P = Prog(nc)
    identf = P.ident("identf", F32)
    identb = P.sb("identb", [128, 128], BF16)
    P.cp(identb[:], identf[:], ["identf"], ["identb"])
    one1 = P.sb("one1", [1, 128], F32)
    P.add("vector", lambda e: e.memset(one1[:], 1.0), (), ["one1"])
    arena = P.sb("arena", [128, 45056], BF16)
    Wgt = arena[:, 0:24576].rearrange("p (c n) -> p c n", c=8)
    Wbr = arena[:, 24576:36864].rearrange("p (b c n) -> p b c n", b=3, c=4)
    Wout = arena[:, 36864:45056].rearrange("p (c n) -> p c n", c=8)
    wrb = P.sb("wrb", [128, 8, 36], BF16)
    wst = [P.sb("wstC%d" % i, [128, 4, 512], F32) for i in range(2)]
    wsc = [0]

    def load_cast(dst, src_ap, dtok, eng=None):
        P.dma(dst, src_ap, (), [dtok], q="gpsimd")

    for g in range(6):
        for h in range(2):
            load_cast(Wgt[:, 4 * h:4 * h + 4, g * 512:(g + 1) * 512],
                      wgt[512 * h:512 * h + 512, g * 512:(g + 1) * 512].rearrange("(c p) n -> p c n", p=128), "Wgt")
    for bi in range(3):
        for h in range(2):
            load_cast(Wbr[:, bi, :, h * 512:(h + 1) * 512], wbr[bi][:, h * 512:(h + 1) * 512].rearrange("(c p) n -> p c n", p=128), "Wbr")
    for g in range(2):
        for h in range(2):
            load_cast(Wout[:, 4 * h:4 * h + 4, g * 512:(g + 1) * 512],
                      wout[512 * h:512 * h + 512, g * 512:(g + 1) * 512].rearrange("(c p) n -> p c n", p=128), "Wout")
    wrs = P.sb("wrs", [128, 8, 36], F32)
    P.dma(wrs[:], wr.rearrange("(c p) n -> p c n", p=128), (), ["wrs"])
    P.cp(wrb[:], wrs[:], ["wrs"], ["wrb"])

    pbk = [P.ps("pbk%d" % i, [128, 512]) for i in range(7)]
    pbb = P.ps("pbb", [128, 8, 128], BF16)
    scT = P.sb("scT", [128, 8, 2], F32)
    P.dma(scT[:], cc.rearrange("(c p) j -> p c j", p=128), (), ["scT"])
    P.act(scT[:], scT[:], AF.Silu, ["scT"], ["scT"])
    rowg = P.sb("rowg", [1, 512], F32)
    browg = P.sb("browg", [1, 512], F32)
    growg = P.sb("growg", [1, 512], F32)
    scal = [[P.sb("scal%d_%d" % (k, j), [128, 8], F32) for j in range(2)] for k in range(2)]
    bias = [[P.sb("bias%d_%d" % (k, j), [128, 8], F32) for j in range(2)] for k in range(2)]
    gaB = [[P.sb("gaB%d_%d" % (k, j), [128, D], F32) for j in range(2)] for k in range(2)]
    one11 = one1[:, 0:1]
    ngs = (n1g, n2g)
    for g in range(12):
        v, hf = g // 2, g % 2
        k, kind = v // 3, v % 3
        P.dma(browg[:], bmod[g * 512:(g + 1) * 512].rearrange("(o n) -> o n", o=1), (), ["browg"])
        if kind == 1:
            P.dma(growg[:], ngs[k][hf * 512:(hf + 1) * 512].rearrange("(o n) -> o n", o=1), (), ["growg"])
        for j in range(2):
            for h in range(2):
                b_ = wsc[0] % 2
                wsc[0] += 1
                P.dma(wst[b_][:], wmod[512 * h:512 * h + 512, g * 512:(g + 1) * 512].rearrange("(c p) n -> p c n", p=128),
                      (), ["wstC%d" % b_])
                for c in range(4):
                    P.mm(pbk[0][0:1, :], scT[:, 4 * h + c, j:j + 1], wst[b_][:, c, :], h == 0 and c == 0, h == 1 and c == 3,
                         ["wstC%d" % b_, "scT"], ["pb0"])
            P.tt(rowg[:], pbk[0][0:1, :], browg[:], ALU.add, ["pb0", "browg"], ["rowg"])
            if kind == 1:
                P.stt(rowg[:], rowg[:], 1.0, growg[:], ALU.add, ALU.mult, ["rowg", "growg"], ["rowg"])
            if kind < 2:
                for c in range(4):
                    P.mm(pbk[1][:, c:c + 1], rowg[:, c * 128:(c + 1) * 128], one11, True, True, ["rowg", "one1"], ["pb1"])
                dstc = (bias if kind == 0 else scal)[k][j]
                P.cp(dstc[:, hf * 4:hf * 4 + 4], pbk[1][:, 0:4], ["pb1"], ["modcols"])
            else:
                P.mm(pbk[1][:, :], one1[:, :], rowg[:], True, True, ["rowg", "one1"], ["pb1"])
                P.cp(gaB[k][j][:, hf * 512:(hf + 1) * 512], pbk[1][:, :], ["pb1"], ["gaB"])
    fngB = P.sb("fngB", [128, D], F32)
    P.dma(fngB[:], fng.partition_broadcast(128), (), ["fngB"])

    xt = P.sb("xt", [128, D], F32)
    ss = [P.sb("ss%d" % i, [128, 1], F32) for i in range(2)]
    xh = [P.sb("xh0", [128, D], F32)] * 2
    hT = [P.sb("hT%d" % i, [128, 8, 128], BF16) for i in range(2)]
    ptr = [pbk[2].rearrange("p (c j) -> p c j", c=4), pbk[3].rearrange("p (c j) -> p c j", c=4)]
    G = P.sb("G", [128, D], F32)
    brt = [P.sb("brt%d" % i, [128, 512], BF16) for i in range(2)]
    brT = [P.sb("brT%d" % i, [128, 4, 128], BF16) for i in range(2)]
    m = P.sb("m", [128, D], F32)
    tmp = P.sb("tmp", [128, D], F32)
    sq = [tmp, tmp]
    mT = P.sb("mT", [128, 8, 128], BF16)
    x1 = P.sb("x1", [128, D], F32)
    Lr = P.sb("Lr", [128, 36], F32)
    Lm = P.sb("Lm", [128, 32], F32)
    k1 = P.sb("k1", [128, 32], F32)
    k2 = P.sb("k2", [128, 32], F32)
    Wt = P.sb("Wt", [128, 32], F32)
    r8 = P.sb("r8", [128, 8], F32)
    g4 = P.sb("g4", [128, 4], F32)
    pen = P.sb("pen", [128, 4], F32)
    mmc = [0]

    def bank():
        k = 4 + (mmc[0] % 3)
        mmc[0] += 1
        return pbk[k], "pb%d" % k

    def norm_T(i, xin, xtok, k, j, hTt, hTtok):
        P.act(sq[0][:], xin, AF.Square, [xtok], ["tmp", "Css"], accum=ss[0][:])
        P.act(ss[0][:], ss[0][:], AF.Sqrt, ["Css"], ["Css"], scale=1.0 / D, bias=EPS)
        P.add("vector", lambda e: e.reciprocal(out=ss[0][:], in_=ss[0][:]), ["Css"], ["Css"])
        P.ts(xh[0][:], xin, ss[0][:, 0:1], ALU.mult, [xtok, "Css"], ["Cxh"])
        for half in range(2):
            ptk = "pb%d" % (2 + half)
            for c4 in range(4):
                c = half * 4 + c4
                P.tr(ptr[half][:, c4, :], xh[0][:, c * 128:(c + 1) * 128], identf[:], ["Cxh", "identf"], [ptk])
            for c4 in range(4):
                c = half * 4 + c4
                P.act(hTt[:, c, :], ptr[half][:, c4, :], AF.Identity, [ptk, "modcols"], [hTtok],
                      scale=scal[k][j][:, c:c + 1], bias=bias[k][j][:, c:c + 1])

    for i in range(NT):
        j = 1 if i < CTXT else 0
        rs = slice(i * 128, (i + 1) * 128)
        P.dma(xt[:], x[rs, :], (), ["xt"])
        norm_T(i, xt[:], "xt", 0, j, hT[0], "hT0")
        for bi in range(3):
            bb = bi % 2
            P.dma(brt[bb][:], brd[bi][rs, :], (), ["brt%d" % bb])
            for c in range(4):
                P.tr(pbb[:, c, :], brt[bb][:, c * 128:(c + 1) * 128], identb[:], ["brt%d" % bb, "identb"], ["pbb"])
            P.cp(brT[bb][:], pbb[:, 0:4, :], ["pbb"], ["brT%d" % bb])
            for hf in range(2):
                pk, tk = bank()
                for c in range(8):
                    P.mm(pk[:], hT[0][:, c, :], Wgt[:, c, bi * 1024 + hf * 512:bi * 1024 + (hf + 1) * 512], c == 0, c == 7,
                         ["hT0", "Wgt"], [tk])
                P.act(G[:, hf * 512:(hf + 1) * 512], pk[:], AF.Sigmoid, [tk], ["G"])
            for hf in range(2):
                pk, tk = bank()
                for c in range(4):
                    P.mm(pk[:], brT[bb][:, c, :], Wbr[:, bi, c, hf * 512:(hf + 1) * 512], c == 0, c == 3,
                         ["brT%d" % bb, "Wbr"], [tk])
                hs_ = slice(hf * 512, (hf + 1) * 512)
                if bi == 0:
                    P.tt(m[:, hs_], pk[:], G[:, hs_], ALU.mult, [tk, "G"], ["m"])
                else:
                    P.tt(tmp[:, hs_], pk[:], G[:, hs_], ALU.mult, [tk, "G"], ["tmp"])
                    P.tt(m[:, hs_], m[:, hs_], tmp[:, hs_], ALU.add, ["m", "tmp"], ["m"], eng="gpsimd")
        for half in range(2):
            ptk = "pb%d" % (2 + half)
            for c4 in range(4):
                c = half * 4 + c4
                P.tr(ptr[half][:, c4, :], m[:, c * 128:(c + 1) * 128], identf[:], ["m", "identf"], [ptk])
            P.cp(mT[:, half * 4:half * 4 + 4, :], ptr[half][:, :, :], [ptk], ["mT"], eng="scalar")
        for hf in range(2):
            pk, tk = bank()
            hs_ = slice(hf * 512, (hf + 1) * 512)
            for c in range(8):
                P.mm(pk[:], mT[:, c, :], Wout[:, c, hs_], c == 0, c == 7, ["mT", "Wout"], [tk])
            P.tt(x1[:, hs_], pk[:], gaB[0][j][:, hs_], ALU.mult, [tk, "gaB"], ["x1"])
            P.tt(x1[:, hs_], x1[:, hs_], xt[:, hs_], ALU.add, ["x1", "xt"], ["x1"], eng="gpsimd")
        P.dma(X1[rs, :], x1[:], ["x1"], ["X1s"])
        norm_T(i, x1[:], "x1", 1, j, hT[1], "hT1")
        P.dma(H2T[:, :, rs], hT[1][:], ["hT1"], ["H2Ts"])
        pk, tk = bank()
        for c in range(8):
            P.mm(pk[:, 0:36], hT[1][:, c, :], wrb[:, c, :], c == 0, c == 7, ["hT1", "wrb"], [tk])
        P.cp(Lr[:], pk[:, 0:36], [tk], ["Lr"])
        R_ = ["Lr", "r8", "g4", "pen", "Lm", "k1", "k2", "Wt"]
        P.add("vector", lambda e: e.tensor_reduce(out=r8[:, 0:1], in_=Lr[:, 0:4], axis=AX.X, op=ALU.max), R_, R_)
        P.ts(g4[:], Lr[:, 0:4], r8[:, 0:1], ALU.is_ge, R_, R_)
        P.ts(r8[:, 1:2], r8[:, 0:1], -1.0, ALU.mult, R_, R_)
        P.act(pen[:], Lr[:, 0:4], AF.Exp, R_, R_, bias=r8[:, 1:2], accum=r8[:, 2:3])
        P.add("vector", lambda e: e.reciprocal(out=r8[:, 2:3], in_=r8[:, 2:3]), R_, R_)
        P.ts(pen[:], g4[:], -1.0, ALU.add, R_, R_, s2=1e30, op1=ALU.mult)
        P.tt(Lm[:].rearrange("p (g j) -> p g j", g=4), Lr[:, 4:36].rearrange("p (g j) -> p g j", g=4),
             pen[:, :].unsqueeze(2).to_broadcast([128, 4, 8]), ALU.add, R_, R_)
        P.add("vector", lambda e: e.tensor_reduce(out=r8[:, 3:4], in_=Lm[:], axis=AX.X, op=ALU.max), R_, R_)
        P.ts(k1[:], Lm[:], r8[:, 3:4], ALU.is_ge, R_, R_)
        P.stt(Lm[:], k1[:], -1e30, Lm[:], ALU.mult, ALU.add, R_, R_)
        P.add("vector", lambda e: e.tensor_reduce(out=r8[:, 4:5], in_=Lm[:], axis=AX.X, op=ALU.max), R_, R_)
        P.ts(k2[:], Lm[:], r8[:, 4:5], ALU.is_ge, R_, R_)
        P.tt(r8[:, 5:6], r8[:, 4:5], r8[:, 3:4], ALU.subtract, R_, R_)
        P.act(r8[:, 5:6], r8[:, 5:6], AF.Exp, R_, R_)
        P.ts(r8[:, 6:7], r8[:, 5:6], 1.0, ALU.add, R_, R_)
        P.add("vector", lambda e: e.reciprocal(out=r8[:, 6:7], in_=r8[:, 6:7]), R_, R_)
        P.tt(r8[:, 7:8], r8[:, 5:6], r8[:, 6:7], ALU.mult, R_, R_)
        P.tt(r8[:, 6:7], r8[:, 6:7], r8[:, 2:3], ALU.mult, R_, R_)
        P.tt(r8[:, 7:8], r8[:, 7:8], r8[:, 2:3], ALU.mult, R_, R_)
        P.ts(Wt[:], k1[:], r8[:, 6:7], ALU.mult, R_, R_)
        P.stt(Wt[:], k2[:], r8[:, 7:8], Wt[:], ALU.mult, ALU.add, R_, R_)
        P.dma(WR[rs, :], Wt[:], R_, ["WRs"])

    barrier(P)
    SBT = 9
    mm2 = [0]

    def bankm():
        k = mm2[0] % 7
        mm2[0] += 1
        return pbk[k], "pb%d" % k
    WE = [arena[:, p * 12288:(p + 1) * 12288].rearrange("p (m c n) -> p m c n", m=3, c=8) for p in range(2)]
    h2sb = arena[:, 24576:33792].rearrange("p (c n) -> p c n", c=8)
    AT = [arena[:, 33792 + q * 2048:33792 + (q + 1) * 2048].rearrange("p (c n) -> p c n", c=4) for q in range(2)]
    ysb = P.sb("y", [128, 8, D], F32)
    ytl = [ysb[:, t, :] for t in range(8)] + [xt[:, :]]
    Wsb = P.sb("Wsb", [128, SBT, 32], F32)
    sg = [P.sb("sg%d" % i, [128, 512], F32) for i in range(2)]
    x1r = [m, tmp]
    cntm = dict(at=0, sg=0)
    for s0 in range(0, NT, SBT):
        ns = min(SBT, NT - s0)
        ntk = ns * 128
        P.dma(h2sb[:, :, 0:ntk], H2T[:, :, s0 * 128:s0 * 128 + ntk], ["H2Ts"], ["h2sb"])
        P.dma(Wsb[:, 0:ns, :], WR[s0 * 128:s0 * 128 + ntk, :].rearrange("(t p) e -> p t e", p=128), ["WRs"], ["Wsb"])
        for e_ in range(NEXP):
            p = e_ % 2
            wtok = "WE%d" % p
            for mi, src in enumerate((wg, wu)):
                for h in range(2):
                    load_cast(WE[p][:, mi, 4 * h:4 * h + 4, :], src[e_, 512 * h:512 * h + 512, :].rearrange("(c p) n -> p c n", p=128), wtok)
            for h in range(2):
                load_cast(WE[p][:, 2, :, :].rearrange("p (fc hf) n -> p fc hf n", hf=2)[:, :, h, :],
                          wd[e_, :, h * 512:(h + 1) * 512].rearrange("(c p) n -> p c n", p=128), wtok)
            for g0 in range(0, ns, 4):
                ng = min(4, ns - g0)
                n = ng * 128
                a = cntm["at"] % 2
                cntm["at"] += 1
                for fc in range(4):
                    pg_, tg = bankm()
                    for c in range(8):
                        P.mm(pg_[:, 0:n], WE[p][:, 0, c, fc * 128:(fc + 1) * 128], h2sb[:, c, g0 * 128:g0 * 128 + n], c == 0, c == 7,
                             [wtok, "h2sb"], [tg])
                    pu_, tu = bankm()
                    for c in range(8):
                        P.mm(pu_[:, 0:n], WE[p][:, 1, c, fc * 128:(fc + 1) * 128], h2sb[:, c, g0 * 128:g0 * 128 + n], c == 0, c == 7,
                             [wtok, "h2sb"], [tu])
                    sb_ = cntm["sg"] % 2
                    cntm["sg"] += 1
                    P.act(sg[sb_][:, 0:n], pg_[:, 0:n], AF.Silu, [tg], ["sg%d" % sb_])
                    P.tt(AT[a][:, fc, 0:n], pu_[:, 0:n], sg[sb_][:, 0:n], ALU.mult, [tu, "sg%d" % sb_], ["AT%d" % a])
                for t in range(ng):
                    ti = g0 + t
                    for hf in range(2):
                        py_, ty = bankm()
                        for fc in range(4):
                            P.mm(py_[:], AT[a][:, fc, t * 128:(t + 1) * 128], WE[p][:, 2, fc * 2 + hf, :], fc == 0, fc == 3,
                                 ["AT%d" % a, wtok], [ty])
                        yv = ytl[ti][:, hf * 512:(hf + 1) * 512]
                        if e_ == 0:
                            P.ts(yv, py_[:], Wsb[:, ti, e_:e_ + 1], ALU.mult, [ty, "Wsb"], ["y%d" % ti])
                        else:
                            P.stt(yv, py_[:], Wsb[:, ti, e_:e_ + 1], yv, ALU.mult, ALU.add, [ty, "Wsb", "y%d" % ti], ["y%d" % ti])
        for t in range(ns):
            i = s0 + t
            j = 1 if i < CTXT else 0
            rs = slice(i * 128, (i + 1) * 128)
            xb = x1r[t % 2]
            xtk = "x1r%d" % (t % 2)
            P.dma(xb[:], X1[rs, :], ["X1s"], [xtk])
            P.tt(ytl[t], ytl[t], gaB[1][j][:], ALU.mult, ["y%d" % t, "gaB"], ["y%d" % t], eng="gpsimd")
            P.tt(xb[:], xb[:], ytl[t], ALU.add, [xtk, "y%d" % t], [xtk])
            P.dma(Xn[rs, :], xb[:], [xtk], ["Xn"])
            P.act(G[:], xb[:], AF.Square, [xtk], ["G", "Css"], accum=ss[0][:])
            P.act(ss[0][:], ss[0][:], AF.Sqrt, ["Css"], ["Css"], scale=1.0 / D, bias=EPS)
            P.add("vector", lambda e: e.reciprocal(out=ss[0][:], in_=ss[0][:]), ["Css"], ["Css"])
            P.stt(G[:], xb[:], ss[0][:, 0:1], fngB[:], ALU.mult, ALU.mult, [xtk, "Css", "fngB"], ["G"])
            P.dma(Yn[rs, :], G[:], ["G"], ["Yn"])
        barrier(P)
    P.final_waits("sync")
    P.emit()
    return nc


_CACHE = {}


def _rope_tables(S):
    pos = np.arange(S)
    row = (pos // 64).astype(np.float32)
    col = (pos % 64).astype(np.float32)
    inv = (10000.0 ** (-np.arange(16, dtype=np.float32) / np.float32(16))).astype(np.float32)
    ang = np.concatenate([row[:, None] * inv, col[:, None] * inv], axis=-1).astype(np.float32)
    return np.cos(ang).astype(np.float32), np.sin(ang).astype(np.float32)


def _prog(key, fn):
    if key not in _CACHE:
        _CACHE[key] = fn()
    return _CACHE[key]


def kernel(x, c, ctx, c_ctx, w_mod, b_mod, norm1_g, norm2_g, w_in, hgrn_lb_logits, hgrn_out_norm_g,
           attn_q_norm_g, attn_k_norm_g, swa_sink, w_branch_a, w_branch_b, w_branch_c, w_out,
           w_group, w_router, w_exp_gate, w_exp_up, w_exp_down, final_norm_g):
    f32 = lambda a: np.ascontiguousarray(np.asarray(a), dtype=np.float32)
    x, c, ctx, c_ctx = f32(x), f32(c), f32(ctx), f32(c_ctx)
    Bn, S, _ = x.shape
    QN = 4
    Lc = S // QN
    NT = CTXT + Lc // 128
    NL = S // 128
    depth = w_in.shape[0]
    cosL, sinL = _rope_tables(S)
    cos_c = np.ones((256, 32), np.float32)
    sin_c = np.zeros((256, 32), np.float32)
    hc = hgrn_consts()
    i = np.arange(128)
    wm = np.stack([(i[:, None] >= i[None, :]), (i[:, None] <= i[None, :])]).astype(np.float32)
    xl, xc = x, ctx
    cores = [(b, j) for b in range(Bn) for j in range(QN)]
    yout = None
    for layer in range(depth):
        xin = [np.concatenate([xc[b], xl[b, j * Lc:(j + 1) * Lc]], 0) for (b, j) in cores]
        ccs = [np.ascontiguousarray(np.stack([c[b], c_ctx], 1)) for (b, j) in cores]
        pa = _prog(("PA", NT, layer), lambda: build_PA(NT, layer))
        wmodA = f32(w_mod[layer][:, :2048])
        winA = f32(w_in[layer][:, :4096])
        ims = []
        for k, (b, j) in enumerate(cores):
            ims.append(dict(x=xin[k], cc=ccs[k], wmod=wmodA, bmod=f32(b_mod[layer][:2048]), n1g=f32(norm1_g[layer]),
                            win=winA, lbl=f32(hgrn_lb_logits), aqg=f32(attn_q_norm_g[layer]), akg=f32(attn_k_norm_g[layer]),
                            cos=np.concatenate([cos_c, cosL[j * Lc:(j + 1) * Lc]], 0),
                            sin=np.concatenate([sin_c, sinL[j * Lc:(j + 1) * Lc]], 0)))
        ra = _run(pa, ims)
        full = {}
        for nm in ("QS", "KKf", "KKb", "LFf", "LFb", "VH", "OG", "QA", "QW", "KV4"):
            full[nm] = [np.concatenate([np.asarray(ra[b * QN][nm])[:256]] +
                                       [np.asarray(ra[b * QN + j][nm])[256:] for j in range(QN)], 0) for b in range(Bn)]
        del ra
        pb = _prog(("PB", NL), lambda: build_PB(NL))
        ims = []
        for b in range(Bn):
            for hp in range(4):
                hs = slice(hp * 128, (hp + 1) * 128)
                kv = hp // 2
                ksl = lambda o: slice(o + kv * 64, o + kv * 64 + 64)
                kv4 = full["KV4"][b]
                d = dict(qs=full["QS"][b][:, hs], kk0=full["KKf"][b][:, hs], kk1=full["KKb"][b][:, hs],
                         lf0=full["LFf"][b][:, hs], lf1=full["LFb"][b][:, hs], vh=full["VH"][b][:, hs],
                         og=full["OG"][b][:, hs], hg=f32(hgrn_out_norm_g[layer]),
                         qa=full["QA"][b][:, hs], ka=np.concatenate([kv4[:, ksl(0)], kv4[:, ksl(0)]], 1), va=kv4[:, ksl(256)],
                         qw=full["QW"][b][:, hs], kw=np.concatenate([kv4[:, ksl(128)], kv4[:, ksl(128)]], 1), vw=kv4[:, ksl(384)],
                         sink=f32(swa_sink[layer][2 * hp:2 * hp + 2]), wm=wm)
                for dd in range(2):
                    for nm in ("MC", "Mend", "Mdiag", "Moff"):
                        d["%s%d" % (nm, dd)] = hc[dd][nm]
                ims.append({k_: np.ascontiguousarray(v) for k_, v in d.items()})
        del full
        rb = _run(pb, ims)
        br = {}
        for nm in ("A", "B", "C"):
            br[nm] = [np.concatenate([np.asarray(rb[b * 4 + hp][nm]) for hp in range(4)], 1) for b in range(Bn)]
        del rb
        pc = _prog(("PC", NT), lambda: build_PC(NT))
        wmodC = f32(w_mod[layer])
        shared = dict(wmod=wmodC, bmod=f32(b_mod[layer]), n1g=f32(norm1_g[layer]), n2g=f32(norm2_g[layer]),
                      fng=f32(final_norm_g), wgt=f32(w_in[layer][:, 4096:7168]), wba=f32(w_branch_a[layer]),
                      wbb=f32(w_branch_b[layer]), wbc=f32(w_branch_c[layer]), wout=f32(w_out[layer]),
                      wr=f32(np.concatenate([np.asarray(w_group[layer]), np.asarray(w_router[layer])], 1)),
                      wg=f32(w_exp_gate[layer]), wu=f32(w_exp_up[layer]), wd=f32(w_exp_down[layer]))
        ims = []
        for k, (b, j) in enumerate(cores):
            d = dict(shared)
            d["x"] = xin[k]
            d["cc"] = ccs[k]
            for nm in ("A", "B", "C"):
                d[nm] = np.ascontiguousarray(np.concatenate([br[nm][b][:256], br[nm][b][256 + j * Lc:256 + (j + 1) * Lc]], 0))
            ims.append(d)
        rc = _run(pc, ims)
        xl = np.stack([np.concatenate([np.asarray(rc[b * QN + j]["Xn"])[256:] for j in range(QN)], 0) for b in range(Bn)])
        xc = np.stack([np.asarray(rc[b * QN]["Xn"])[:256] for b in range(Bn)])
        if layer == depth - 1:
            yout = np.stack([np.concatenate([np.asarray(rc[b * QN + j]["Yn"])[256:] for j in range(QN)], 0) for b in range(Bn)])
        del rc
    return yout.astype(np.float32)
```
